# Optimizing a Trainium2 kernel written in Bass

```python
import math
import jax, jax.numpy as jnp
from jax import lax
import numpy as np

D_MODEL = 1024
BATCH = 8
SEQ = 2048
DEPTH = 1

HG_HEADS = 4
HG_DK = 128
HG_DV = 128
HG_WIDTH = HG_HEADS * HG_DV
HG_CHUNK = 16
NSA_HEADS = 8
NSA_KV_HEADS = 2
NSA_HEAD_DIM = 64
NSA_WIDTH = NSA_HEADS * NSA_HEAD_DIM
KV_WIDTH = NSA_KV_HEADS * NSA_HEAD_DIM
CMP_BLOCK = 32
CMP_STRIDE = 16
CMP_HIDDEN = 256
SLC_BLOCK = 64
SLC_TOPK = 16
SLC_Q_BLOCK = 64
WINDOW = 512
Q_BLOCK = 128
ROPE_THETA = 500000.0
ROPE_DIM = NSA_HEAD_DIM // 4
D_FF = 2816
CONV_WIDTH = 3
EPS = 1e-6
MIX_WIDTH = HG_WIDTH + NSA_WIDTH
PROJ_SPLITS = (HG_WIDTH, HG_WIDTH, HG_WIDTH, HG_WIDTH, NSA_WIDTH, KV_WIDTH, KV_WIDTH, KV_WIDTH, KV_WIDTH, KV_WIDTH, KV_WIDTH, 3 * NSA_HEADS)
PROJ_WIDTH = sum(PROJ_SPLITS)
NEG_INF = -1e30
FORCE_SCORE = 1e4

kernel_name = 'hymba_hgrn2_nsa_convffn'

F32 = jnp.float32


def rms_norm(x, gain):
    xf = x.astype(F32)
    y = xf * lax.rsqrt(jnp.mean(xf * xf, axis=-1, keepdims=True) + EPS)
    return (y * gain.astype(F32)).astype(x.dtype)


def head_rms(o):
    return o * lax.rsqrt(jnp.mean(o * o, axis=-1, keepdims=True) + EPS)


def partial_rope(x, positions):
    half = ROPE_DIM // 2
    inv_freq = ROPE_THETA ** (-jnp.arange(half, dtype=F32) * 2.0 / ROPE_DIM)
    ang = positions.astype(F32)[..., None] * inv_freq
    cos = jnp.cos(ang)[:, :, None, :]
    sin = jnp.sin(ang)[:, :, None, :]
    xf = x.astype(F32)
    x1 = xf[..., :half]
    x2 = xf[..., half:ROPE_DIM]
    out = jnp.concatenate([x1 * cos - x2 * sin, x2 * cos + x1 * sin, xf[..., ROPE_DIM:]], axis=-1)
    return out.astype(x.dtype)


def masked_softmax(s, mask):
    return jax.nn.softmax(jnp.where(mask, s, NEG_INF), axis=-1)


def hgrn2_mixer(q, f_pre, i_in, g, lb, out_gain):
    B, T, _ = q.shape
    H, C = HG_HEADS, HG_CHUNK
    N = T // C
    lb = lb.astype(F32)
    f = lb + (1.0 - lb) * jax.nn.sigmoid(f_pre.astype(F32))
    log_f = jnp.log(f)
    k = 1.0 - f

    def to_chunks(t, d):
        return t.astype(F32).reshape(B, N, C, H, d).transpose(1, 0, 3, 2, 4)

    qh = to_chunks(q, HG_DK)
    kh = to_chunks(k, HG_DK)
    vh = to_chunks(i_in, HG_DV)
    b = jnp.cumsum(to_chunks(log_f, HG_DK), axis=3)
    b_last = b[:, :, :, -1:, :]
    q_dec = qh * jnp.exp(b)
    k_inv = kh * jnp.exp(-b)
    k_dec = kh * jnp.exp(b_last - b)
    chunk_decay = jnp.exp(b_last[:, :, :, 0, :])

    causal = jnp.tril(jnp.ones((C, C), dtype=bool))
    attn = jnp.where(causal, jnp.einsum('nbhcd,nbhsd->nbhcs', q_dec, k_inv), 0.0)
    o_intra = jnp.einsum('nbhcs,nbhsv->nbhcv', attn, vh)

    def step(S, xs):
        q_d, k_d, v_c, dec = xs
        o = jnp.einsum('bhcd,bhdv->bhcv', q_d, S)
        S = S * dec[..., :, None] + jnp.einsum('bhsd,bhsv->bhdv', k_d, v_c)
        return S, o

    S0 = jnp.zeros((B, H, HG_DK, HG_DV), F32)
    _, o_inter = lax.scan(step, S0, (q_dec, k_dec, vh, chunk_decay))
    o = (o_intra + o_inter).transpose(1, 0, 3, 2, 4).reshape(B, T, H, HG_DV)
    o = head_rms(o).reshape(B, T, HG_WIDTH) * out_gain.astype(F32)
    return (o * jax.nn.silu(g.astype(F32))).astype(q.dtype)


def compress_blocks(kv, pe, w1, w2):
    B, T, G, D = kv.shape
    n_cmp = (T - CMP_BLOCK) // CMP_STRIDE + 1
    tok = jnp.arange(n_cmp)[:, None] * CMP_STRIDE + jnp.arange(CMP_BLOCK)[None, :]
    blk = kv[:, tok] + pe[:, None, :].astype(kv.dtype)
    blk = blk.transpose(0, 3, 1, 2, 4).reshape(B, G, n_cmp, CMP_BLOCK * D)
    return jax.nn.silu(blk @ w1) @ w2


def nsa_mixer(q, k_cmp, v_cmp, k_slc, v_slc, k_win, v_win, gate_pre, positions,
              pe_k, pe_v, ck_w1, ck_w2, cv_w1, cv_w2, out_gain):
    B, T, _ = q.shape
    H, G, D = NSA_HEADS, NSA_KV_HEADS, NSA_HEAD_DIM
    HPG = H // G
    dt = q.dtype
    scale = D ** -0.5
    qr = partial_rope(q.reshape(B, T, H, D), positions)
    qg = qr.reshape(B, T, G, HPG, D).transpose(0, 2, 3, 1, 4)
    kv_heads = lambda t: t.reshape(B, T, G, D)
    kc_raw = partial_rope(kv_heads(k_cmp), positions)
    ks = partial_rope(kv_heads(k_slc), positions)
    kw = partial_rope(kv_heads(k_win), positions)
    t_idx = jnp.arange(T)

    kc = compress_blocks(kc_raw, pe_k, ck_w1, ck_w2)
    vc = compress_blocks(kv_heads(v_cmp), pe_v, cv_w1, cv_w2)
    n_cmp = kc.shape[2]
    cmp_start = jnp.arange(n_cmp) * CMP_STRIDE
    cmp_end = cmp_start + CMP_BLOCK - 1
    cmp_mask = cmp_end[None, :] <= t_idx[:, None]
    s_cmp = jnp.einsum('bghtd,bgnd->bghtn', qg, kc).astype(F32) * scale
    p_cmp = jnp.where(cmp_mask, masked_softmax(s_cmp, cmp_mask), 0.0)
    o_cmp = jnp.einsum('bghtn,bgnd->bghtd', p_cmp.astype(dt), vc)

    n_sel = T // SLC_BLOCK
    sel_start = jnp.arange(n_sel) * SLC_BLOCK
    overlap = ((cmp_start[:, None] <= sel_start[None, :] + SLC_BLOCK - 1)
               & (cmp_end[:, None] >= sel_start[None, :])).astype(F32)
    p_sel = jnp.einsum('bghtn,ns->bgts', p_cmp, overlap)
    cur = t_idx // SLC_BLOCK
    blk = jnp.arange(n_sel)
    forced = (blk[None, :] == 0) | (blk[None, :] == cur[:, None]) | (blk[None, :] == cur[:, None] - 1)
    score = jnp.where(forced, FORCE_SCORE, p_sel)
    score = jnp.where(blk[None, :] <= cur[:, None], score, -jnp.inf)
    k_eff = min(SLC_TOPK, n_sel)
    _, sel_idx = lax.top_k(score, k_eff)

    k_blocks = ks.transpose(0, 2, 1, 3).reshape(B, G, n_sel, SLC_BLOCK, D)
    v_blocks = kv_heads(v_slc).transpose(0, 2, 1, 3).reshape(B, G, n_sel, SLC_BLOCK, D)
    nqb = T // SLC_Q_BLOCK
    q_sb = jnp.moveaxis(qg.reshape(B, G, HPG, nqb, SLC_Q_BLOCK, D), 3, 0)
    idx_sb = jnp.moveaxis(sel_idx.reshape(B, G, nqb, SLC_Q_BLOCK, k_eff), 2, 0)
    t_sb = t_idx.reshape(nqb, SLC_Q_BLOCK)
    bi = jnp.arange(B)[:, None, None, None]
    gi = jnp.arange(G)[None, :, None, None]

    def sel_block(args):
        qb, ib, tb = args
        kb = k_blocks[bi, gi, ib].reshape(B, G, SLC_Q_BLOCK, k_eff * SLC_BLOCK, D)
        vb = v_blocks[bi, gi, ib].reshape(B, G, SLC_Q_BLOCK, k_eff * SLC_BLOCK, D)
        tok = (ib[..., None] * SLC_BLOCK + jnp.arange(SLC_BLOCK)).reshape(B, G, SLC_Q_BLOCK, k_eff * SLC_BLOCK)
        mask = (tok <= tb[None, None, :, None])[:, :, None]
        s = jnp.einsum('bghqd,bgqjd->bghqj', qb, kb).astype(F32) * scale
        p = masked_softmax(s, mask)
        return jnp.einsum('bghqj,bgqjd->bghqd', p.astype(dt), vb)

    o_slc = lax.map(sel_block, (q_sb, idx_sb, t_sb))
    o_slc = jnp.moveaxis(o_slc, 0, 3).reshape(B, G, HPG, T, D)

    nwb = T // Q_BLOCK
    span = WINDOW + Q_BLOCK
    win_tok = jnp.arange(nwb)[:, None] * Q_BLOCK + jnp.arange(span)[None, :]
    pad = ((0, 0), (WINDOW, 0), (0, 0), (0, 0))
    kw_b = jnp.pad(kw, pad)[:, win_tok]
    vw_b = jnp.pad(kv_heads(v_win), pad)[:, win_tok]
    kpos = win_tok - WINDOW
    tq = jnp.arange(nwb)[:, None] * Q_BLOCK + jnp.arange(Q_BLOCK)[None, :]
    rel = tq[:, :, None] - kpos[:, None, :]
    win_mask = (kpos[:, None, :] >= 0) & (rel >= 0) & (rel < WINDOW)
    qw = qg.reshape(B, G, HPG, nwb, Q_BLOCK, D)
    s_win = jnp.einsum('bghwqd,bwjgd->bghwqj', qw, kw_b).astype(F32) * scale
    p_win = masked_softmax(s_win, win_mask)
    o_win = jnp.einsum('bghwqj,bwjgd->bghwqd', p_win.astype(dt), vw_b).reshape(B, G, HPG, T, D)

    gates = jax.nn.sigmoid(gate_pre.astype(F32)).reshape(B, T, 3, G, HPG)
    gates = gates.transpose(2, 0, 3, 4, 1)[..., None]
    o = gates[0] * o_cmp.astype(F32) + gates[1] * o_slc.astype(F32) + gates[2] * o_win.astype(F32)
    o = head_rms(o.transpose(0, 3, 1, 2, 4)).reshape(B, T, NSA_WIDTH) * out_gain.astype(F32)
    return o.astype(dt)


def conv_ffn(h, w_gate, w_up, conv_w, conv_b, w_down):
    gate = h @ w_gate
    gate = lax.conv_general_dilated(gate, conv_w[:, None, :].astype(gate.dtype), window_strides=(1,),
                                    padding=[(CONV_WIDTH - 1, 0)],
                                    dimension_numbers=('NWC', 'WIO', 'NWC'),
                                    feature_group_count=D_FF) + conv_b
    return (jax.nn.silu(gate) * (h @ w_up)) @ w_down


def setup_inputs(seed: int = 0) -> dict:
    key = jax.random.key(seed)
    ks = jax.random.split(key, 24)

    def nrm(k, shape, scale):
        return jax.random.normal(k, shape, F32) * scale

    cmp_in = CMP_BLOCK * NSA_HEAD_DIM
    return {
        'x': nrm(ks[0], (BATCH, SEQ, D_MODEL), 1.0),
        'positions': jax.random.randint(ks[1], (BATCH, 1), 0, 1024, dtype=jnp.int32) + jnp.arange(SEQ, dtype=jnp.int32)[None, :],
        'ln1_gain': 1.0 + nrm(ks[2], (DEPTH, D_MODEL), 0.02),
        'w_in': nrm(ks[3], (DEPTH, D_MODEL, PROJ_WIDTH), D_MODEL ** -0.5),
        'hgrn_lb_param': nrm(ks[4], (DEPTH + 1, HG_WIDTH), 0.1),
        'hgrn_out_gain': 1.0 + nrm(ks[5], (DEPTH, HG_WIDTH), 0.02),
        'cmp_pe_k': nrm(ks[6], (DEPTH, CMP_BLOCK, NSA_HEAD_DIM), 0.1),
        'cmp_pe_v': nrm(ks[7], (DEPTH, CMP_BLOCK, NSA_HEAD_DIM), 0.1),
        'cmp_k_w1': nrm(ks[8], (DEPTH, cmp_in, CMP_HIDDEN), cmp_in ** -0.5),
        'cmp_k_w2': nrm(ks[9], (DEPTH, CMP_HIDDEN, NSA_HEAD_DIM), CMP_HIDDEN ** -0.5),
        'cmp_v_w1': nrm(ks[10], (DEPTH, cmp_in, CMP_HIDDEN), cmp_in ** -0.5),
        'cmp_v_w2': nrm(ks[11], (DEPTH, CMP_HIDDEN, NSA_HEAD_DIM), CMP_HIDDEN ** -0.5),
        'nsa_out_gain': 1.0 + nrm(ks[12], (DEPTH, NSA_WIDTH), 0.02),
        'w_o': nrm(ks[13], (DEPTH, MIX_WIDTH, D_MODEL), MIX_WIDTH ** -0.5),
        'ln2_gain': 1.0 + nrm(ks[14], (DEPTH, D_MODEL), 0.02),
        'ffn_w_gate': nrm(ks[15], (DEPTH, D_MODEL, D_FF), D_MODEL ** -0.5),
        'ffn_w_up': nrm(ks[16], (DEPTH, D_MODEL, D_FF), D_MODEL ** -0.5),
        'ffn_conv_w': nrm(ks[17], (DEPTH, CONV_WIDTH, D_FF), CONV_WIDTH ** -0.5),
        'ffn_conv_b': nrm(ks[18], (DEPTH, D_FF), 0.02),
        'ffn_w_down': nrm(ks[19], (DEPTH, D_FF, D_MODEL), D_FF ** -0.5),
        'final_gain': 1.0 + nrm(ks[20], (D_MODEL,), 0.02),
    }


def reference(x, positions, ln1_gain, w_in, hgrn_lb_param, hgrn_out_gain, cmp_pe_k, cmp_pe_v,
              cmp_k_w1, cmp_k_w2, cmp_v_w1, cmp_v_w2, nsa_out_gain, w_o, ln2_gain,
              ffn_w_gate, ffn_w_up, ffn_conv_w, ffn_conv_b, ffn_w_down, final_gain):
    lower_bounds = jnp.cumsum(jax.nn.softmax(hgrn_lb_param.astype(F32), axis=0), axis=0)
    offsets = np.cumsum(PROJ_SPLITS)[:-1].tolist()
    for l in range(DEPTH):
        h = rms_norm(x, ln1_gain[l])
        proj = h @ w_in[l]
        (hq, hf, hi, hg, nq, kc, vc, ksl, vsl, kwn, vwn, ngate) = jnp.split(proj, offsets, axis=-1)
        o_h = hgrn2_mixer(hq, hf, hi, hg, lower_bounds[l], hgrn_out_gain[l])
        o_n = nsa_mixer(nq, kc, vc, ksl, vsl, kwn, vwn, ngate, positions,
                        cmp_pe_k[l], cmp_pe_v[l], cmp_k_w1[l], cmp_k_w2[l], cmp_v_w1[l], cmp_v_w2[l],
                        nsa_out_gain[l])
        x = x + jnp.concatenate([o_h, o_n], axis=-1) @ w_o[l]
        h = rms_norm(x, ln2_gain[l])
        x = x + conv_ffn(h, ffn_w_gate[l], ffn_w_up[l], ffn_conv_w[l], ffn_conv_b[l], ffn_w_down[l])
    return rms_norm(x, final_gain)
```

```python
import numpy as np
from contextlib import ExitStack
import concourse.bass as bass
import concourse.mybir as mybir
from concourse.bass_utils import run_bass_kernel_spmd

F32 = mybir.dt.float32
BF16 = mybir.dt.bfloat16
I32 = mybir.dt.int32
AF = mybir.ActivationFunctionType
ALU = mybir.AluOpType

T = 2048
D = 1024
NT = 16
DFF = 2816
NF = 22
EPS = 1e-6
PI = float(np.pi)
NCORES = 8
FG = 4


class Res:
    __slots__ = ("w", "r")

    def __init__(self):
        self.w = None
        self.r = []


class Eng:
    def __init__(self, name):
        self.name = name
        self.q = []
        self.count = 0
        self.waited = {}
        self.pending = False


class FW:
    def __init__(self, n_dma_sems=12):
        self.pe = Eng("tensor")
        self.act = Eng("scalar")
        self.dve = Eng("vector")
        self.pool = Eng("gpsimd")
        self.sp = Eng("sync")
        self.engs = [self.pe, self.act, self.dve, self.pool, self.sp]
        self.n_dma_sems = n_dma_sems
        self.dma_state = {q: dict(next=0, issued=[0] * n_dma_sems) for q in ("sync", "gpsimd")}
        self._rec = None

    def rec_begin(self):
        self._rec = []

    def rec_end(self):
        r = self._rec
        self._rec = None
        return r

    def replay_lanes(self, lanes, stagger=0):
        if stagger:
            lanes = [[None] * (k_ * stagger) + list(l) for k_, l in enumerate(lanes)]
        idx = [0] * len(lanes)
        live = True
        while live:
            live = False
            for k, lane in enumerate(lanes):
                if idx[k] < len(lane):
                    ent = lane[idx[k]]
                    idx[k] += 1
                    live = True
                    if ent is None:
                        continue
                    if ent[0] == "op":
                        self.op(ent[1], ent[2], ent[3], ent[4], ent[5])
                    else:
                        self.dma(ent[1], ent[2], ent[3], ent[4])

    def _wait(self, eng, ev):
        if ev is None:
            return
        key, val = ev
        if eng is self.pe and key == "tensor":
            return
        if eng.waited.get(key, 0) >= val:
            return
        eng.waited[key] = val
        eng.q.append(("wait", key, val))

    def _deps(self, eng, reads, writes):
        for r in reads:
            self._wait(eng, r.w)
        for w in writes:
            self._wait(eng, w.w)
            for ev in w.r:
                self._wait(eng, ev)

    def op(self, eng, fn, reads=(), writes=(), signal=True):
        if self._rec is not None:
            self._rec.append(("op", eng, fn, list(reads), list(writes), signal))
            return None
        self._deps(eng, reads, writes)
        if signal:
            eng.count += 1
            ev = (eng.name, eng.count)
            eng.pending = False
        else:
            ev = (eng.name, eng.count + 1)
            eng.pending = True
        eng.q.append(("op", fn, signal))
        for r in reads:
            r.r.append(ev)
        for w in writes:
            w.w = ev
            w.r = []
        return ev

    def dma(self, qeng, fn, reads=(), writes=()):
        if self._rec is not None:
            self._rec.append(("dma", qeng, fn, list(reads), list(writes)))
            return None
        st = self.dma_state[qeng.name]
        k = st["next"]
        st["next"] = (k + 1) % self.n_dma_sems
        key = "dma_%s_%d" % (qeng.name, k)
        if st["issued"][k] > 0:
            self._wait(qeng, (key, 16 * st["issued"][k]))
        self._deps(qeng, reads, writes)
        st["issued"][k] += 1
        ev = (key, 16 * st["issued"][k])
        qeng.q.append(("dma", fn, key))
        for r in reads:
            r.r.append(ev)
        for w in writes:
            w.w = ev
            w.r = []
        return ev

    def barrier(self):
        evs = []
        for e in self.engs:
            assert not e.pending
            if e.count > 0:
                evs.append((e.name, e.count))
        for q, st in self.dma_state.items():
            for k in range(self.n_dma_sems):
                if st["issued"][k] > 0:
                    evs.append(("dma_%s_%d" % (q, k), 16 * st["issued"][k]))
        for e in self.engs:
            for ev in evs:
                self._wait(e, ev)

    def sem_keys(self):
        keys = [e.name for e in self.engs]
        for q in self.dma_state:
            for k in range(self.n_dma_sems):
                keys.append("dma_%s_%d" % (q, k))
        return keys

    def runner(self, sems):
        def run(eng, h):
            own = sems[eng.name]
            pend = []
            for item in eng.q:
                if item[0] == "wait":
                    pend.append((sems[item[1]], item[2]))
                    continue
                for (s, v) in pend:
                    h.wait_ge(s, v)
                pend = []
                ins = item[1](h)
                if item[0] == "op":
                    if item[2]:
                        ins.then_inc(own, 1)
                else:
                    ins.then_inc(sems[item[2]], 16)
            for (s, v) in pend:
                h.wait_ge(s, v)
        return run


def f_mm(out, lhsT, rhs, start=True, stop=True):
    return lambda e: e.matmul(out, lhsT=lhsT, rhs=rhs, start=start, stop=stop)


def f_tr(out, in_, ident):
    return lambda e: e.transpose(out=out, in_=in_, identity=ident)


def f_act(out, in_, func, **kw):
    return lambda e: e.activation(out=out, in_=in_, func=func, **kw)


def f_tt(out, in0, in1, op):
    return lambda e: e.tensor_tensor(out=out, in0=in0, in1=in1, op=op)


def f_ts(out, in0, s1, s2, op0, op1=None):
    if op1 is None:
        return lambda e: e.tensor_scalar(out=out, in0=in0, scalar1=s1, scalar2=None, op0=op0)
    return lambda e: e.tensor_scalar(out=out, in0=in0, scalar1=s1, scalar2=s2, op0=op0, op1=op1)


def f_stt(out, in0, scalar, in1, op0, op1):
    return lambda e: e.scalar_tensor_tensor(out=out, in0=in0, scalar=scalar, in1=in1, op0=op0, op1=op1)


def f_copy(out, in_):
    return lambda e: e.tensor_copy(out=out, in_=in_)


def f_dma(out, in_):
    return lambda e: e.dma_start(out=out, in_=in_)


def f_memset(ap, v):
    return lambda e: e.memset(ap, v)


class Arena:
    def __init__(self, ap, n):
        self.ap = ap
        self.n = n
        self.off = 0

    def reset(self):
        self.off = 0

    def _take(self, nf):
        assert self.off + nf <= self.n, "arena overflow %d + %d > %d" % (self.off, nf, self.n)
        a = self.ap[:, self.off:self.off + nf]
        self.off += nf
        return a

    @staticmethod
    def _shape(a, shape):
        if shape[0] != 128:
            a = a[0:shape[0], :]
        if len(shape) == 2:
            return a
        if len(shape) == 3:
            return a.rearrange("p (a b) -> p a b", b=shape[2])
        return a.rearrange("p (a b c) -> p a b c", b=shape[2], c=shape[3])

    def f32(self, shape):
        n = int(np.prod(shape[1:]))
        return self._shape(self._take(n), shape)

    def i32(self, shape):
        n = int(np.prod(shape[1:]))
        return self._shape(self._take(n).bitcast(I32), shape)

    def bf16(self, shape):
        n = int(np.prod(shape[1:]))
        nf = (n + 1) // 2
        a = self._take(nf).bitcast(BF16)
        if 2 * nf != n:
            a = a[:, 0:n]
        return self._shape(a, shape)


class _Stop(Exception):
    pass


def build_nc(debug=False, stop=None):
    nc = bass.Bass("TRN2", target_bir_lowering=False)
    fw = FW()

    def din(name, shape, dt=F32):
        return nc.dram_tensor(name, list(shape), dt, kind="ExternalInput").ap()

    x_d = din("x", [T, D])
    pos_d = din("pos", [1, T], I32)
    wfm_d = din("w_fm", [128, 16, 1024])
    wti_d = din("w_tm_i", [128, 8 * 512])
    wtg_d = din("w_tm_g", [128, 8 * 512])
    wtn_d = din("w_tm_n", [128, 8 * 280])
    wo_d = din("w_o", [128, 8 * 1024])
    wg_d = din("w_gate", [128, NF, 1024])
    wu_d = din("w_up", [128, NF, 1024])
    wd_d = din("w_down", [128, NF, 1024])
    w1k_d = din("w1k", [128, 32 * 256])
    w1v_d = din("w1v", [128, 32 * 256])
    w2k_d = din("w2k", [128, 2 * 64])
    w2v_d = din("w2v", [128, 2 * 64])
    pek_d = din("pek", [128, 32])
    pev_d = din("pev", [128, 32])
    g1_d = din("g1", [128, 8])
    g2_d = din("g2", [128, 8])
    lbp_d = din("lbp", [128, 8])
    hgain_d = din("hgain", [1, 512])
    ngain_d = din("ngain", [1, 512])
    fgain_d = din("fgain", [1, 1024])
    cw_d = din("convw", [128, NF * 3])
    cb_d = din("convb", [128, NF])
    c_ident = din("c_ident", [128, 128])
    c_perm = din("c_perm", [128, 128])
    c_invf = din("c_invf", [128, 1])
    c_tri0 = din("c_tri0", [128, 512])
    c_tri4 = din("c_tri4", [128, 512])
    c_eblk = din("c_eblk", [128, 16 * 128])
    c_cmpm = din("c_cmpm", [128, T])
    c_keep = din("c_keep", [128, 8 * 32])
    c_add = din("c_add", [128, 8 * 32])
    c_ovl = din("c_ovl", [128, 32])
    out_d = nc.dram_tensor("out", [T, D], F32, kind="ExternalOutput").ap()
    dbg = {}
    if debug:
        dbg["mix"] = nc.dram_tensor("dbg_mix", [128, 8 * T], F32, kind="ExternalOutput").ap()
        dbg["x1"] = nc.dram_tensor("dbg_x1", [T, D], F32, kind="ExternalOutput").ap()

    with ExitStack() as es:
        def sb(name, shape, dt):
            return es.enter_context(nc.sbuf_tensor("s_" + name, shape, dt))

        X1 = sb("X1", [128, NT * D], F32)
        HT = sb("HT", [128, 8, T], BF16)
        MIXT = sb("MIXT", [128, 8 * T // 2], F32)
        MIXTb = MIXT[:, :].bitcast(BF16).rearrange("p (c t) -> p c t", t=T)
        identb = sb("identb", [128, 128], BF16)
        permf = sb("permf", [128, 128], F32)
        invf = sb("invf", [128, 1], F32)
        tri0 = sb("tri0", [128, 512], BF16)
        tri4 = sb("tri4", [128, 512], BF16)
        eblk = sb("eblk", [128, 16, 128], BF16)
        cmpm = sb("cmpm", [128, T], BF16)
        keep_t = sb("keep_t", [128, 8, 32], F32)
        add_t = sb("add_t", [128, 8, 32], F32)
        g1 = sb("g1s", [128, 8], F32)
        g2 = sb("g2s", [128, 8], F32)
        lbp = sb("lbps", [128, 8], F32)
        lb = sb("lb", [128, 4], F32)
        oml = sb("oml", [128, 4], F32)
        hgain = sb("hgain", [128, 512], F32)
        ngain = sb("ngain", [128, 512], F32)
        fgain = sb("fgain", [128, 1024], F32)
        convw = sb("convw", [128, NF, 3], F32)
        convb = sb("convb", [128, NF], F32)
        stat = sb("stat", [128, 64], F32)
        WKN = 13912
        WK = sb("WK", [128, WKN], F32)
        pbs = [es.enter_context(nc.psum_tensor("pb%d" % k, [128, 512], F32)) for k in range(8)]
        pbR = [Res() for _ in range(8)]
        sems = {k: es.enter_context(nc.semaphore(k)) for k in fw.sem_keys()}

        cR = Res()
        bank_i = [0]
        lane_banks = [None]
        lane_pos = {}

        def bank():
            if lane_banks[0] is not None:
                ids = lane_banks[0]
                key = tuple(ids)
                p = lane_pos.get(key, 0)
                lane_pos[key] = (p + 1) % len(ids)
                k = ids[p]
                return pbs[k], pbR[k]
            k = bank_i[0]
            bank_i[0] = (k + 1) % 8
            return pbs[k], pbR[k]

        wk = Arena(WK[:, :], WKN)
        xa = Arena(X1[:, :], NT * D)
        ma = Arena(MIXT[:, :], 8 * T // 2)


        def mmg(outap, outres, pairs, reads):
            n = len(pairs)
            for k, (l, r) in enumerate(pairs):
                fw.op(fw.pe, f_mm(outap, l, r, start=(k == 0), stop=(k == n - 1)), reads=reads, writes=[outres],
                      signal=(k == n - 1))

        wk.reset()
        xt = [wk.f32([128, D]) for _ in range(4)]
        xtR = [Res() for _ in range(4)]
        x_tiles = x_d.rearrange("(n p) d -> n p d", p=128)
        for i in range(4):
            fw.dma(fw.sp, f_dma(xt[i][:, :], x_tiles[i]), writes=[xtR[i]])
        ev_id = fw.dma(fw.pool, f_dma(identb[:], c_ident), writes=[Res()])
        ev_g1 = fw.dma(fw.sp, f_dma(g1[:], g1_d), writes=[Res()])
        epsT = sb("epsT", [128, 1], F32)
        ev_eps = fw.op(fw.pool, f_memset(epsT[:], EPS), writes=[Res()])
        oneT = sb("oneT", [128, 1], F32)
        fw.op(fw.pool, f_memset(oneT[:], 1.0), writes=[Res()])
        fw._wait(fw.pe, ev_id)
        fw._wait(fw.dve, ev_g1)
        fw._wait(fw.act, ev_eps)
        xa.reset()
        _v_tm_pre = xa.bf16([128, NT, 512])
        wtm_pre = [xa.bf16([128, 8, 512]) for _ in range(2)]
        wtmR_pre = [Res(), Res()]
        fw.dma(fw.pool, f_dma(wtm_pre[0][:].rearrange("p a b -> p (a b)").rearrange("p (s e) -> p s e", e=2048),
                              wti_d.rearrange("p (s e) -> p s e", e=2048)), writes=[wtmR_pre[0]])
        mhi_pre = Arena(MIXT[:, 4096:8192], 4096)
        wfm_pre = [mhi_pre.bf16([128, 8, 128]) for _ in range(3)]
        wfmR_pre = [Res() for _ in range(4)]
        fw.dma(fw.pool, f_dma(wfm_pre[0][:].rearrange("p a b -> p (a b)"), wfm_d[:, 0, :]), writes=[wfmR_pre[0]])
        fw.dma(fw.pool, f_dma(wfm_pre[1][:].rearrange("p a b -> p (a b)"), wfm_d[:, 4, :]), writes=[wfmR_pre[1]])
        fw.dma(fw.pool, f_dma(wtm_pre[1][:].rearrange("p a b -> p (a b)").rearrange("p (s e) -> p s e", e=2048),
                              wtg_d.rearrange("p (s e) -> p s e", e=2048)), writes=[wtmR_pre[1]])
        for (dst, src) in [(permf[:], c_perm), (invf[:], c_invf), (keep_t[:].rearrange("p a b -> p (a b)"), c_keep),
                           (add_t[:].rearrange("p a b -> p (a b)"), c_add), (g2[:], g2_d), (lbp[:], lbp_d),
                           (hgain[:], hgain_d.broadcast_to([128, 512])), (ngain[:], ngain_d.broadcast_to([128, 512])),
                           (fgain[:], fgain_d.broadcast_to([128, 1024])),
                           (convw[:].rearrange("p a b -> p (a b)"), cw_d), (convb[:], cb_d)]:
            fw.dma(fw.sp, f_dma(dst, src), writes=[Res()])
        for (dst, src) in [(tri0[:], c_tri0), (tri4[:], c_tri4),
                           (eblk[:].rearrange("p a b -> p (a b)"), c_eblk), (cmpm[:], c_cmpm)]:
            fw.dma(fw.pool, f_dma(dst, src), writes=[Res()])

        HT_R = [Res() for _ in range(NT)]
        MX_R = [Res() for _ in range(NT)]
        X1_R = [Res() for _ in range(NT)]

        def rstd_from_ss(ss_ap, n_feat, out_ap, res):
            fw.op(fw.act, f_act(out_ap, ss_ap, AF.Sqrt, scale=1.0 / n_feat, bias=epsT[:, 0:1]), reads=[res, cR], writes=[res])
            fw.op(fw.dve, lambda e: e.reciprocal(out_ap, out_ap), reads=[res], writes=[res])

        def norm_to_HT(src_tile_ap, src_res, i, gains, wka):
            st = wka["st"][i % 4]
            stR = wka["stR"][i % 4]
            junk = wka["junk"]
            fw.op(fw.act, f_act(junk[:, :], src_tile_ap, AF.Square, accum_out=st[:, 0:1]), reads=[src_res], writes=[wka["junkR"], stR])
            rstd_from_ss(st[:, 0:1], D, st[:, 1:2], stR)
            xb = wka["xb"][i % 4]
            xbR = wka["xbR"][i % 4]
            fw.op(fw.dve, f_ts(xb[:, :], src_tile_ap, st[:, 1:2], None, ALU.mult), reads=[src_res, stR], writes=[xbR])
            pt, ptR = bank()
            ptb = pt[:, :].bitcast(BF16)
            for c in range(8):
                fw.op(fw.pe, f_tr(ptb[:, c * 128:(c + 1) * 128], xb[:, c * 128:(c + 1) * 128], identb[:]), reads=[xbR, cR], writes=[ptR],
                      signal=(c == 7))
            fw.op(fw.dve, f_tt(HT[:, :, i * 128:(i + 1) * 128], ptb.rearrange("p (c t) -> p c t", t=128),
                               gains[:, 0:8].unsqueeze(2).broadcast_to([128, 8, 128]), ALU.mult),
                  reads=[ptR, cR], writes=[HT_R[i]])

        def chk(name):
            if stop == name:
                fw.barrier()
                raise _Stop()
        try:
            wka = dict(st=[stat[:, 2 * q_:2 * q_ + 2] for q_ in range(4)], stR=[Res() for _ in range(4)], junk=wk.bf16([128, D]), junkR=Res(),
                       xb=[wk.bf16([128, D]) for _ in range(4)], xbR=[Res() for _ in range(4)])
            lanes = [[], [], [], []]
            for i in range(NT):
                lane_banks[0] = [2 * (i % 4), 2 * (i % 4) + 1]
                fw.rec_begin()
                if i >= 4:
                    fw.dma(fw.sp, f_dma(xt[i % 4][:, :], x_tiles[i]), writes=[xtR[i % 4]])
                norm_to_HT(xt[i % 4][:, :], xtR[i % 4], i, g1, wka)
                lanes[i % 4] += fw.rec_end()
            lane_banks[0] = None
            fw.replay_lanes(lanes, stagger=4)
            fw.barrier()
            if stop == "P1":
                raise _Stop()

            wk.reset()
            xa.reset()
            lbR = Res()
            fw.op(fw.dve, f_tt(lb[:], lbp[:, 0:4], lbp[:, 4:8], ALU.subtract), writes=[lbR])
            fw.op(fw.act, f_act(lb[:], lb[:], AF.Sigmoid), reads=[lbR], writes=[lbR])
            fw.op(fw.dve, f_ts(oml[:], lb[:], -1.0, 1.0, ALU.mult, ALU.add), reads=[lbR], writes=[lbR])
            fw.op(fw.dve, f_ts(hgain[:, :], hgain[:, :], -1.0, None, ALU.mult), reads=[cR], writes=[cR])
            mhi = Arena(MIXT[:, 4096:8192], 4096)
            v_tm = xa.bf16([128, NT, 512])
            v_R = [Res() for _ in range(NT)]
            wtm = [xa.bf16([128, 8, 512]) for _ in range(2)]
            wtmR = wtmR_pre
            H2 = T // 4
            tq = [xa.f32([128, H2]) for _ in range(4)]
            tf = [xa.f32([128, H2]) for _ in range(4)]
            tb_ = [xa.f32([128, H2]) for _ in range(4)]
            te = [xa.f32([128, H2]) for _ in range(4)]
            tqR, tfR, tbR, teR = [[Res() for _ in range(4)] for _ in range(4)]
            qdT = wk.bf16([128, 4, T])
            kiT = wk.bf16([128, 4, T])
            kd_tm = wk.bf16([128, 4, NT, 128])
            rmask = wk.f32([128, H2])
            qdR = [Res() for _ in range(4)]
            kiR = [Res() for _ in range(4)]
            kdtR = [Res() for _ in range(4)]
            wfm = [mhi.bf16([128, 8, 128]) for _ in range(3)] + [wk.bf16([128, 8, 128])]
            wfmR = wfmR_pre
            kdT = [mhi.bf16([128, H2]) for _ in range(4)]
            kdR = [Res() for _ in range(4)]
            Sf = mhi.f32([128, 4, 128])
            SfR = [Res() for _ in range(4)]
            dec = mhi.f32([128, 4, NT])
            decR = Res()
            hst = mhi.f32([128, 16])
            fw.op(fw.pool, f_memset(rmask[:, :], 1.0), writes=[cR])
            fw.op(fw.pool, f_memset(rmask[:, :].rearrange("p (n c) -> p n c", c=128)[:, :, 0:1], 0.0), writes=[cR])
            def load_fm(chunk, ws):
                fw.dma(fw.pool, f_dma(wfm[ws][:].rearrange("p a b -> p (a b)"), wfm_d[:, chunk, :]), writes=[wfmR[ws]])
            for i in range(NT):
                pb, pR = bank()
                mmg(pb[:, :], pR, [(HT[:, c, i * 128:(i + 1) * 128], wtm[0][:, c, :]) for c in range(8)], [HT_R[i], wtmR[0]])
                fw.op(fw.act, f_act(v_tm[:, i, :], pb[:, :], AF.Copy), reads=[pR], writes=[v_R[i]])
            def fm_unit(h, u):
                s = u
                c0_ = u * H2
                cur_q, cur_f = 2 * (h % 2), 2 * (h % 2) + 1
                tbk = u
                pb, pR = bank()
                mmg(pb[:, :], pR, [(wfm[cur_f][:, c, :], HT[:, c, tbk * 512:(tbk + 1) * 512]) for c in range(8)],
                    HT_R[4 * tbk:4 * tbk + 4] + [wfmR[cur_f]])
                fw.op(fw.act, f_act(tf[s][:, :], pb[:, :], AF.Exp, scale=-1.0), reads=[pR], writes=[tfR[s]])
                fw.op(fw.act, f_act(tf[s][:, :], tf[s][:, :], AF.Ln, bias=oneT[:, 0:1]), reads=[tfR[s]], writes=[tfR[s]])
                fw.op(fw.act, f_act(tf[s][:, :], tf[s][:, :], AF.Exp, scale=-1.0), reads=[tfR[s]], writes=[tfR[s]])
                fw.op(fw.dve, f_ts(tf[s][:, :], tf[s][:, :], oml[:, h:h + 1], lb[:, h:h + 1], ALU.mult, ALU.add), reads=[tfR[s], lbR], writes=[tfR[s]])
                fw.op(fw.act, f_act(tb_[s][:, :], tf[s][:, :], AF.Ln), reads=[tfR[s]], writes=[tbR[s]])
                fw.op(fw.dve, (lambda s_: lambda e: e.tensor_tensor_scan(out=te[s_][:, :], data0=rmask[:, :], data1=tb_[s_][:, :], initial=0.0,
                                                                         op0=ALU.mult, op1=ALU.add))(s), reads=[tbR[s], cR], writes=[teR[s]])
                fw.op(fw.act, f_act(tb_[s][:, :], te[s][:, :], AF.Exp), reads=[teR[s]], writes=[tbR[s]])
                pq, pqR = bank()
                mmg(pq[:, :], pqR, [(wfm[cur_q][:, c, :], HT[:, c, tbk * 512:(tbk + 1) * 512]) for c in range(8)],
                    HT_R[4 * tbk:4 * tbk + 4] + [wfmR[cur_q]])
                fw.op(fw.dve, f_tt(qdT[:, h, c0_:c0_ + H2], pq[:, :], tb_[s][:, :], ALU.mult), reads=[pqR, tbR[s]], writes=[qdR[h]])
                fw.op(fw.act, f_act(tq[s][:, :], te[s][:, :], AF.Exp, scale=-1.0), reads=[teR[s]], writes=[tqR[s]])
                te3 = te[s][:, :].rearrange("p (n c) -> p n c", c=128)
                fw.op(fw.act, f_act(dec[:, h, 4 * u:4 * u + 4], te3[:, :, 127], AF.Exp), reads=[teR[s]], writes=[decR])
                fw.op(fw.dve, f_stt(kiT[:, h, c0_:c0_ + H2], tf[s][:, :], 1.0, tq[s][:, :], ALU.subtract, ALU.mult), reads=[tfR[s], tqR[s]], writes=[kiR[h]])
                fw.op(fw.dve, f_tt(kdT[s][:, :].rearrange("p (n c) -> p n c", c=128), kiT[:, h, c0_:c0_ + H2].rearrange("p (n c) -> p n c", c=128),
                                   dec[:, h, 4 * u:4 * u + 4].unsqueeze(2).broadcast_to([128, 4, 128]), ALU.mult), reads=[kiR[h], decR], writes=[kdR[s]])
                pt, ptR = bank()
                ptb = pt[:, :].bitcast(BF16)
                for k_ in range(4):
                    fw.op(fw.pe, f_tr(ptb[:, k_ * 128:(k_ + 1) * 128], kdT[s][:, k_ * 128:(k_ + 1) * 128], identb[:]), reads=[kdR[s], cR], writes=[ptR],
                          signal=(k_ == 3))
                fw.op(fw.dve, f_copy(kd_tm[:, h, 4 * u:4 * u + 4, :], ptb[:, 0:512].rearrange("p (k d) -> p k d", d=128)),
                      reads=[ptR], writes=[kdtR[h]])

            lanes = [[], [], [], []]
            fm_tails = [[], [], [], []]
            for h in range(4):
                pre = []
                if h < 3:
                    fw.rec_begin()
                    load_fm(h + 1, 2 * ((h + 1) % 2))
                    load_fm(4 + h + 1, 2 * ((h + 1) % 2) + 1)
                    pre = fw.rec_end()
                for u in range(4):
                    lane_banks[0] = [2 * u, 2 * u + 1]
                    fw.rec_begin()
                    fm_unit(h, u)
                    body = fw.rec_end()
                    if h == 0 and u > 0:
                        lanes[u] += [None] * (u * (len(body) // 4))
                    tail_ = body[-5:]
                    body = body[:-5]
                    mid = len(body) // 2
                    fill = pre if u == 0 else [None] * len(pre)
                    lanes[u] += body[:10] + fm_tails[u] + body[10:mid] + fill + body[mid:]
                    fm_tails[u] = tail_
            lane_banks[0] = None
            for u in range(4):
                lanes[u] += fm_tails[u]
            fw.replay_lanes(lanes)
            fw.barrier()
            wtn = MIXT[:, 4096:4096 + 1120].bitcast(BF16).rearrange("p (a b) -> p a b", b=280)
            wtnR = Res()
            fw.dma(fw.pool, f_dma(wtn[:, :, :], wtn_d.rearrange("p (a b) -> p a b", b=280)), writes=[wtnR])
            xb_ = Arena(X1[:, 8192:16384], 8192)
            atm = [xb_.bf16([128, 512]) for _ in range(2)]
            atmR = [Res(), Res()]
            sg = [xb_.f32([128, 512]) for _ in range(2)]
            sgR = [Res(), Res()]
            sq = [xb_.f32([128, 512]) for _ in range(2)]
            sqR = [Res(), Res()]
            t1 = [xb_.f32([128, 512]) for _ in range(2)]
            t1R = [Res(), Res()]
            mxh = [xb_.bf16([128, 512]) for _ in range(2)]
            mxhR = [Res(), Res()]
            Ssn = xb_.bf16([128, NT, 4, 128])
            SsnR = [Res() for _ in range(NT)]
            hstR = [Res(), Res()]
            fw.op(fw.pool, f_memset(Ssn[:, 0, :, :].rearrange("p a b -> p (a b)"), 0.0), writes=[SsnR[0]])
            fw.op(fw.pool, f_memset(Sf[:, :, :].rearrange("p a b -> p (a b)"), 0.0), writes=SfR)

            def s_step(m):
                pu, puR = bank()
                for h in range(4):
                    fw.op(fw.pe, f_mm(pu[:, h * 128:(h + 1) * 128], kd_tm[:, h, m, :], v_tm[:, m, h * 128:(h + 1) * 128]), reads=[kdtR[h], v_R[m]],
                          writes=[puR], signal=(h == 3))
                for h in range(4):
                    fw.op(fw.dve, f_stt(Sf[:, h, :], Sf[:, h, :], dec[:, h, m:m + 1], pu[:, h * 128:(h + 1) * 128], ALU.mult, ALU.add),
                          reads=[SfR[h], decR, puR], writes=[SfR[h]])
                fw.op(fw.pool, f_copy(Ssn[:, m + 1, :, :], Sf[:, :, :]), reads=SfR, writes=[SsnR[m + 1]])

            def o_tile(n):
                s = n % 2
                pa, paR = bank()
                for h in range(4):
                    fw.op(fw.pe, f_mm(pa[:, h * 128:(h + 1) * 128], kiT[:, h, n * 128:(n + 1) * 128], qdT[:, h, n * 128:(n + 1) * 128]), reads=[kiR[h], qdR[h]],
                          writes=[paR], signal=(h == 3))
                fw.op(fw.dve, f_tt(atm[s][:, :], pa[:, :], tri0[:, :], ALU.mult), reads=[paR, cR], writes=[atmR[s]])
                pg, pgR = bank()
                mmg(pg[:, :], pgR, [(HT[:, c, n * 128:(n + 1) * 128], wtm[1][:, c, :]) for c in range(8)], [HT_R[n], wtmR[1]])
                fw.op(fw.act, f_act(sg[s][:, :], pg[:, :], AF.Silu), reads=[pgR], writes=[sgR[s]])
                po, poR = bank()
                for h in range(4):
                    hs = slice(h * 128, (h + 1) * 128)
                    fw.op(fw.pe, f_mm(po[:, hs], atm[s][:, hs], v_tm[:, n, hs], start=True, stop=False), reads=[atmR[s], v_R[n]], writes=[poR], signal=False)
                    fw.op(fw.pe, f_mm(po[:, hs], qdT[:, h, n * 128:(n + 1) * 128], Ssn[:, n, h, :], start=False, stop=True), reads=[qdR[h], SsnR[n]], writes=[poR],
                          signal=(h == 3))
                fw.op(fw.act, f_act(sq[s][:, :], po[:, :], AF.Square), reads=[poR], writes=[sqR[s]])
                fw.op(fw.dve, lambda e, s_=s: e.tensor_reduce(out=hst[:, 8 * s_:8 * s_ + 4], in_=sq[s_][:, :].rearrange("p (h v) -> p h v", v=128),
                                                            axis=mybir.AxisListType.X, op=ALU.add), reads=[sqR[s]], writes=[hstR[s]])
                fw.op(fw.act, f_act(hst[:, 8 * s + 4:8 * s + 8], hst[:, 8 * s:8 * s + 4], AF.Sqrt, scale=1.0 / 128, bias=epsT[:, 0:1]), reads=[hstR[s], cR], writes=[hstR[s]])
                fw.op(fw.dve, lambda e, s_=s: e.reciprocal(hst[:, 8 * s_ + 4:8 * s_ + 8], hst[:, 8 * s_ + 4:8 * s_ + 8]), reads=[hstR[s]], writes=[hstR[s]])
                fw.op(fw.dve, f_tt(t1[s][:, :].rearrange("p (h v) -> p h v", v=128), po[:, :].rearrange("p (h v) -> p h v", v=128),
                                   hst[:, 8 * s + 4:8 * s + 8].unsqueeze(2).broadcast_to([128, 4, 128]), ALU.mult), reads=[poR, hstR[s]], writes=[t1R[s]])
                fw.op(fw.pool, f_tt(sg[s][:, :], sg[s][:, :], hgain[:, :], ALU.mult), reads=[sgR[s], cR], writes=[sgR[s]])
                fw.op(fw.dve, f_tt(mxh[s][:, :], t1[s][:, :], sg[s][:, :], ALU.mult), reads=[t1R[s], sgR[s]], writes=[mxhR[s]])
                pt, ptR = bank()
                ptb = pt[:, :].bitcast(BF16)
                for h in range(4):
                    fw.op(fw.pe, f_tr(ptb[:, h * 128:(h + 1) * 128], mxh[s][:, h * 128:(h + 1) * 128], identb[:]), reads=[mxhR[s], cR], writes=[ptR], signal=(h == 3))
                fw.op(fw.act, f_act(MIXTb[:, 0:4, n * 128:(n + 1) * 128], ptb[:, 0:512].rearrange("p (c t) -> p c t", t=128), AF.Copy), reads=[ptR], writes=[MX_R[n]])

            lanes = [[], [], []]
            lane_banks[0] = [0, 7]
            fw.rec_begin()
            for m in range(NT - 1):
                s_step(m)
            lanes[0] = fw.rec_end()
            tails = {1: [], 2: []}
            for n in range(NT):
                lane_banks[0] = [1, 2, 3] if n % 2 == 0 else [4, 5, 6]
                fw.rec_begin()
                o_tile(n)
                it_ = fw.rec_end()
                ln_ = 1 + n % 2
                body_, tail_ = it_[:-5], it_[-5:]
                lanes[ln_] += body_[:8] + tails[ln_] + body_[8:]
                tails[ln_] = tail_
            for ln_ in (1, 2):
                lanes[ln_] += tails[ln_]
            lane_banks[0] = None
            lanes[2] = [None] * 20 + lanes[2]
            fw.replay_lanes(lanes)
            fw.barrier()
            if stop == "P2":
                raise _Stop()

            wk.reset()
            xa.reset()
            qT = wk.bf16([128, 4, T])
            kcT = wk.bf16([128, T])
            vcT = wk.bf16([128, T])
            Vs = wk.bf16([128, NT, 2, 66])
            Vw = wk.bf16([128, NT, 2, 66])
            gates = wk.f32([128, NT, 24])
            qTR, kcR, vcR, ksR, kwR, VsR, VwR, gtR = [Res() for _ in range(8)]
            ksZ = [wk.bf16([128, T]) for _ in range(2)]
            kwZ = [wk.bf16([128, T]) for _ in range(2)]
            kzR = Res()
            for g_ in range(2):
                fw.op(fw.pool, f_memset(ksZ[g_][:, :], 0.0), writes=[kzR])
                fw.op(fw.pool, f_memset(kwZ[g_][:, :], 0.0), writes=[kzR])
            cosT = xa.f32([128, T])
            sinT = xa.f32([128, T])
            posi = xa.i32([128, T])
            posf = xa.f32([128, T])
            yy = xa.f32([128, T])
            wfm = [xa.bf16([128, 8, 128]) for _ in range(3)]
            wfmR = [Res() for _ in range(3)]
            qf = [xa.f32([128, 512]) for _ in range(2)]
            qfR = [Res(), Res()]
            qb = [wk.bf16([128, 512]) for _ in range(2)]
            qbR = [Res(), Res()]
            permb = wk.bf16([128, 128])
            fw.op(fw.dve, f_copy(permb[:, :], permf[:, :]), reads=[cR], writes=[cR])
            r1 = [xa.f32([128, 512]) for _ in range(2)]
            r1R = [Res(), Res()]
            r2 = [xa.f32([128, 512]) for _ in range(2)]
            r2R = [Res(), Res()]
            ropeR = Res()
            w1kb = MIXT[:, 4096:8192].bitcast(BF16).rearrange("p (l h) -> p l h", h=256)
            w1kR = Res()
            fw.dma(fw.sp, f_dma(posi[:, :], pos_d.broadcast_to([128, T])), writes=[ropeR])
            fw.op(fw.dve, f_copy(posf[:, :], posi[:, :]), reads=[ropeR], writes=[ropeR])
            fw.op(fw.dve, f_ts(yy[:, :], posf[:, :], invf[:, 0:1], None, ALU.mult), reads=[ropeR, cR], writes=[ropeR])

            def frac_sin(dst, src, add):
                if add != 0.0:
                    fw.op(fw.dve, f_ts(dst, src, add, None, ALU.add), reads=[ropeR], writes=[ropeR])
                    src = dst
                fw.op(fw.dve, f_copy(posi[:, :], src), reads=[ropeR], writes=[ropeR])
                fw.op(fw.dve, f_copy(posf[:, :], posi[:, :]), reads=[ropeR], writes=[ropeR])
                fw.op(fw.dve, f_tt(dst, src, posf[:, :], ALU.subtract), reads=[ropeR], writes=[ropeR])
                fw.op(fw.dve, f_stt(posf[:, :], dst, 0.5, dst, ALU.is_gt, ALU.subtract), reads=[ropeR], writes=[ropeR])
                fw.op(fw.dve, f_stt(dst, posf[:, :], 0.5, posf[:, :], ALU.is_gt, ALU.subtract), reads=[ropeR], writes=[ropeR])
                fw.op(fw.act, f_act(dst, dst, AF.Sin, scale=2 * PI), reads=[ropeR], writes=[ropeR])


            ndma = [0]

            p3lanes = [[], []]

            def fm_proj2(chunk, consume):
                ws = ndma[0] % 3
                ndma[0] += 1
                fw.rec_begin()
                fw.dma(fw.pool, f_dma(wfm[ws][:].rearrange("p a b -> p (a b)"), wfm_d[:, chunk, :]), writes=[wfmR[ws]])
                pre = fw.rec_end()
                p3lanes[0] += pre
                p3lanes[1] += [None] * len(pre)
                for tb in range(4):
                    lane_banks[0] = [4 * (tb % 2) + b_ for b_ in range(4)]
                    fw.rec_begin()
                    pb, pR = bank()
                    mmg(pb[:, :], pR, [(wfm[ws][:, c, :], HT[:, c, tb * 512:(tb + 1) * 512]) for c in range(8)],
                        HT_R[4 * tb:4 * tb + 4] + [wfmR[ws]])
                    consume(tb, pb, pR)
                    p3lanes[tb % 2] += fw.rec_end()
                lane_banks[0] = None

            rope_cnt = [0]

            def rope_consume(dst2d, dstR):
                def consume(tb, pb, pR):
                    s = tb % 2
                    sl = slice(tb * 512, (tb + 1) * 512)
                    fw.op(fw.act, f_act(qf[s][:, :], pb[:, :], AF.Copy), reads=[pR], writes=[qfR[s]])
                    fw.op(fw.act, f_act(qb[s][:, :], pb[:, :], AF.Copy), reads=[pR], writes=[qbR[s]])
                    pr, prR = bank()
                    fw.op(fw.pe, f_mm(pr[:, :], permb[:, :], qb[s][:, :]), reads=[qbR[s], cR], writes=[prR])
                    fw.op(fw.dve, f_tt(r1[s][:, :], qf[s][:, :], cosT[:, sl], ALU.mult), reads=[qfR[s], ropeR], writes=[r1R[s]])
                    fw.op(fw.dve, f_tt(r2[s][:, :], pr[:, :], sinT[:, sl], ALU.mult), reads=[prR, ropeR], writes=[r2R[s]])
                    if isinstance(dst2d, list):
                        for g_ in range(2):
                            rw = slice(g_ * 64, (g_ + 1) * 64)
                            fw.op(fw.dve, f_tt(dst2d[g_][rw, sl], r1[s][rw, :], r2[s][rw, :], ALU.add), reads=[r1R[s], r2R[s]], writes=[dstR])
                    else:
                        fw.op(fw.dve, f_tt(dst2d[:, sl], r1[s][:, :], r2[s][:, :], ALU.add), reads=[r1R[s], r2R[s]], writes=[dstR])
                return consume

            fw.op(fw.dve, f_memset(Vs[:, :, :, :].rearrange('p a b c -> p (a b c)'), 1.0), writes=[VsR])
            fw.op(fw.dve, f_memset(Vw[:, :, :, :].rearrange('p a b c -> p (a b c)'), 1.0), writes=[VwR])
            for i in range(NT):
                pb, pR = bank()
                mmg(pb[:, 0:280], pR, [(HT[:, c, i * 128:(i + 1) * 128], wtn[:, c, :]) for c in range(8)], [HT_R[i], wtnR])
                fw.op(fw.act, f_act(Vs[:, i, :, 0:64], pb[:, 0:128].rearrange("p (g d) -> p g d", d=64), AF.Copy), reads=[pR], writes=[VsR])
                fw.op(fw.act, f_act(Vw[:, i, :, 0:64], pb[:, 128:256].rearrange("p (g d) -> p g d", d=64), AF.Copy), reads=[pR], writes=[VwR])
                fw.op(fw.act, f_act(gates[:, i, :], pb[:, 256:280], AF.Sigmoid), reads=[pR], writes=[gtR])
            frac_sin(sinT[:, :], yy[:, :], 0.0)
            frac_sin(cosT[:, :], yy[:, :], 0.25)
            fm_proj2(13, lambda tb, pb, pR: fw.op(fw.act, f_act(vcT[:, tb * 512:(tb + 1) * 512], pb[:, :], AF.Copy), reads=[pR], writes=[vcR]))
            for c in range(4):
                fm_proj2(8 + c, rope_consume(qT[:, c, :], qTR))
            fm_proj2(12, rope_consume(kcT, kcR))
            fm_proj2(14, rope_consume(ksZ, kzR))
            fm_proj2(15, rope_consume(kwZ, kzR))
            p3lanes[1] = [None] * 8 + p3lanes[1]
            fw.replay_lanes(p3lanes)
            fw.dma(fw.pool, f_dma(w1kb.rearrange("p a b -> p (a b)").rearrange("p (s e) -> p s e", e=2048),
                                  w1k_d.rearrange("p (s e) -> p s e", e=2048)), writes=[w1kR, wtnR])
            chk('P3a3')
            fw.barrier()
            if stop == "P3a":
                raise _Stop()

            xa.reset()
            w1b = xa.bf16([128, 32, 256])
            w1R = Res()
            w2b = xa.bf16([128, 2, 64])
            w2R = Res()
            peb = xa.bf16([128, 32])
            pef = xa.f32([128, 32])
            peR = Res()
            hidT = xa.bf16([128, 2, 2, 128])
            hidR = Res()
            cvec = xa.f32([128, 2])
            cvR = Res()
            kcc = wk.bf16([128, 2, 128])
            kccR = Res()
            vcx = wk.bf16([128, 2, 98])
            vcxR = Res()
            fw.op(fw.pool, f_memset(vcx[:, :, :].rearrange('p a b -> p (a b)'), 1.0), writes=[vcxR])
            fw.op(fw.pool, f_memset(vcx[:, :, 0:64], 0.0), writes=[vcxR])
            ovl_f = xa.f32([128, 32])
            fw.dma(fw.sp, f_dma(ovl_f[:, :], c_ovl), writes=[vcxR])
            for g in range(2):
                fw.op(fw.dve, f_copy(vcx[:, g, 65:97], ovl_f[:, :]), reads=[vcxR], writes=[vcxR])
            fw.op(fw.pool, f_memset(kcc[:, :, :].rearrange("p a b -> p (a b)"), 0.0), writes=[kccR])

            w1v_buf = w1b
            fw.dma(fw.pool, f_dma(w1v_buf[:].rearrange("p a b -> p (a b)").rearrange("p (s e) -> p s e", e=2048),
                                  w1v_d.rearrange("p (s e) -> p s e", e=2048)), writes=[w1R])
            for kv in range(2):
                srcT, srcR = (kcT, kcR) if kv == 0 else (vcT, vcR)
                w1b = w1kb if kv == 0 else w1v_buf
                if kv == 0:
                    w1R_save = w1R
                    w1R = w1kR
                else:
                    w1R = w1R_save
                fw.dma(fw.pool, f_dma(w2b[:].rearrange("p a b -> p (a b)"), w2k_d if kv == 0 else w2v_d), writes=[w2R])
                fw.dma(fw.sp, f_dma(pef[:, :], pek_d if kv == 0 else pev_d), writes=[peR])
                fw.op(fw.dve, f_copy(peb[:, :], pef[:, :]), reads=[peR], writes=[peR])
                src3 = srcT[:, :].rearrange("p (n l) -> p n l", l=16)
                for hc in range(2):
                    pc, pcR = bank()
                    mmg(pc[:, 0:1], pcR, [(w1b[0:64, l, hc * 128:(hc + 1) * 128], peb[0:64, l:l + 1]) for l in range(32)], [w1R, peR])
                    fw.op(fw.dve, f_copy(cvec[:, hc:hc + 1], pc[:, 0:1]), reads=[pcR], writes=[cvR])
                    phs = [bank(), bank()]
                    for l in range(32):
                        for g in range(2):
                            ph, phR = phs[g]
                            rows = slice(g * 64, (g + 1) * 64)
                            rhs = src3[rows, 0:127, l] if l < 16 else src3[rows, 1:128, l - 16]
                            fw.op(fw.pe, f_mm(ph[:, 0:127], w1b[rows, l, hc * 128:(hc + 1) * 128], rhs, start=(l == 0), stop=(l == 31)),
                                  reads=[w1R, srcR], writes=[phR], signal=(l == 31))
                    for g in range(2):
                        ph, phR = phs[g]
                        fw.op(fw.act, f_act(hidT[:, g, hc, 0:127], ph[:, 0:127], AF.Silu, bias=cvec[:, hc:hc + 1]), reads=[phR, cvR], writes=[hidR])
                for g in range(2):
                    po, poR = bank()
                    if kv == 0:
                        mmg(po[g * 64:(g + 1) * 64, 0:127], poR, [(w2b[:, hc, :], hidT[:, g, hc, 0:127]) for hc in range(2)], [w2R, hidR])
                        fw.op(fw.act, f_act(kcc[g * 64:(g + 1) * 64, g, 0:127], po[g * 64:(g + 1) * 64, 0:127], AF.Copy), reads=[poR], writes=[kccR])
                    else:
                        mmg(po[0:127, 0:64], poR, [(hidT[:, g, hc, 0:127], w2b[:, hc, :]) for hc in range(2)], [w2R, hidR])
                        fw.op(fw.act, f_act(vcx[0:127, g, 0:64], po[0:127, 0:64], AF.Copy), reads=[poR], writes=[vcxR])
            fw.barrier()
            if stop == "P3b":
                raise _Stop()

            xa.reset()
            NE = 18
            ering = [[xa.bf16([128, 512]) for _ in range(NE)] for _ in range(2)]
            eR = [[Res() for _ in range(NE)] for _ in range(2)]
            e_i = [0, 0]

            def eslot(g):
                k = e_i[g]
                e_i[g] = (k + 1) % NE
                return ering[g][k], eR[g][k]
            psel = xa.f32([128, 2, 32])
            pselR = [Res(), Res()]
            sc = [xa.f32([128, 32]) for _ in range(2)]
            sc2 = [xa.f32([128, 32]) for _ in range(2)]
            m8a = [xa.f32([128, 8]) for _ in range(2)]
            m8b = [xa.f32([128, 8]) for _ in range(2)]
            selb = [xa.bf16([128, 32]) for _ in range(2)]
            selbT = [[xa.bf16([128, 4, 128]) for _ in range(2)] for _ in range(2)]
            selbTR = [[Res(), Res()] for _ in range(2)]
            for g_ in range(2):
                for p_ in range(2):
                    fw.op(fw.pool, f_memset(selbT[g_][p_][:, :, :].rearrange("p a b -> p (a b)"), 0.0), writes=[selbTR[g_][p_]])
            tkR = [Res(), Res()]
            acc = [xa.f32([128, 8, 64]) for _ in range(3)]
            accR = [[Res(), Res()] for _ in range(3)]
            coef = xa.f32([128, 3, 8])
            rs = xa.f32([128, 8])
            coefR = [Res(), Res()]
            nst = xa.f32([128, 16])
            nstR = Res()
            pos_ = [[xa.f32([128, 4, 98]) for _ in range(3)] for _ in range(2)]
            posR = [[Res() for _ in range(3)] for _ in range(2)]
            sqn = xa.f32([128, 8, 64])
            njR = Res()
            mxn = xa.bf16([128, 512])
            mxnR = Res()
            SCALE = 0.125

            def q4(g, i):
                return qT[:, :, i * 128:(i + 1) * 128]

            def f_recip(out, in_):
                return lambda e: e.reciprocal(out, in_)

            def f_max8(out, in_):
                return lambda e: e.max(out=out, in_=in_)

            def f_mrep(out, rep_, vals):
                return lambda e: e.match_replace(out=out, in_to_replace=rep_, in_values=vals, imm_value=-1e30)

            def finish_branch(b, g, i, po, poR, first):
                hs = slice(g * 4, g * 4 + 4)
                ab = acc[i % 3]
                aR = accR[i % 3][g]
                gi_ = gates[:, i, :]
                fw.op(fw.dve, f_ts(rs[:, hs], po[:, :, 64], 1e-30, None, ALU.max), reads=[poR], writes=[coefR[g]])
                fw.op(fw.dve, f_recip(rs[:, hs], rs[:, hs]), reads=[coefR[g]], writes=[coefR[g]])
                fw.op(fw.dve, f_tt(coef[:, b, hs], rs[:, hs], gi_[:, b * 8 + g * 4:b * 8 + g * 4 + 4], ALU.mult), reads=[coefR[g], gtR], writes=[coefR[g]])
                for hp in range(4):
                    h = g * 4 + hp
                    if first:
                        fw.op(fw.dve, f_ts(ab[:, h, :], po[:, hp, 0:64], coef[:, b, h:h + 1], None, ALU.mult), reads=[poR, coefR[g]], writes=[aR])
                    else:
                        fw.op(fw.dve, f_stt(ab[:, h, :], po[:, hp, 0:64], coef[:, b, h:h + 1], ab[:, h, :], ALU.mult, ALU.add),
                              reads=[poR, coefR[g], aR], writes=[aR])

            def cmp_part(i, g):
                ps_, psR = bank()
                ps3 = ps_[:, :].rearrange("p (h q) -> p h q", q=128)
                fw.op(fw.pe, f_mm(ps3, kcc[:, g, :], q4(g, i), start=True, stop=False), reads=[kccR, qTR], writes=[psR], signal=False)
                fw.op(fw.pe, f_mm(ps3, identb[:, :], cmpm[:, i * 128:(i + 1) * 128].unsqueeze(1).broadcast_to([128, 4, 128]), start=False, stop=True),
                      reads=[cR], writes=[psR])
                ec, ecR = eslot(g)
                fw.op(fw.act, f_act(ec[:, :], ps_[:, :], AF.Exp, scale=SCALE), reads=[psR], writes=[ecR])
                po, poR = bank()
                po3 = po[:, :].rearrange("p (h w) -> p h w", w=128)
                for hp in range(4):
                    fw.op(fw.pe, f_mm(po3[:, hp, 0:97], ec[:, hp * 128:(hp + 1) * 128], vcx[:, g, 0:97]), reads=[ecR, vcxR], writes=[poR])
                pst = pos_[g][0]
                fw.op(fw.dve, f_copy(pst[:, :, 0:97], po3[:, :, 0:97]), reads=[poR], writes=[posR[g][0]])
                po3 = pst
                poR = posR[g][0]
                finish_branch(0, g, i, po3, poR, True)
                if i >= 8:
                    for hp in range(4):
                        h = g * 4 + hp
                        if hp == 0:
                            fw.op(fw.dve, f_ts(psel[:, g, :], po3[:, hp, 65:97], rs[:, h:h + 1], None, ALU.mult), reads=[poR, coefR[g]], writes=[pselR[g]])
                        else:
                            fw.op(fw.dve, f_stt(psel[:, g, :], po3[:, hp, 65:97], rs[:, h:h + 1], psel[:, g, :], ALU.mult, ALU.add),
                                  reads=[poR, coefR[g], pselR[g]], writes=[pselR[g]])
                    fw.op(fw.dve, f_tt(sc[g][:, :], psel[:, g, :], keep_t[:, i - 8, :], ALU.mult), reads=[pselR[g], cR], writes=[tkR[g]])
                    fw.op(fw.dve, f_tt(sc[g][:, :], sc[g][:, :], add_t[:, i - 8, :], ALU.add), reads=[tkR[g], cR], writes=[tkR[g]])
                    fw.op(fw.dve, f_max8(m8a[g][:, :], sc[g][:, :]), reads=[tkR[g]], writes=[tkR[g]])
                    fw.op(fw.dve, f_mrep(sc2[g][:, :], m8a[g][:, :], sc[g][:, :]), reads=[tkR[g]], writes=[tkR[g]])
                    fw.op(fw.dve, f_max8(m8b[g][:, :], sc2[g][:, :]), reads=[tkR[g]], writes=[tkR[g]])
                    fw.op(fw.dve, f_ts(sc2[g][:, :], sc[g][:, :], m8b[g][:, 7:8], None, ALU.is_ge), reads=[tkR[g]], writes=[tkR[g]])
                    fw.op(fw.dve, f_ts(selb[g][:, :], sc2[g][:, :], -1.0, 30000.0, ALU.add, ALU.mult), reads=[tkR[g]], writes=[tkR[g]])
                    pt, ptR = bank()
                    ptb = pt[:, :].bitcast(BF16)
                    fw.op(fw.pe, f_tr(ptb[0:32, 0:128], selb[g][:, :], identb[:]), reads=[tkR[g], cR], writes=[ptR])
                    fw.op(fw.act, f_act(selbT[g][i % 2][0:32, :, :], ptb[0:32, 0:128].unsqueeze(1).broadcast_to([32, 4, 128]), AF.Copy),
                          reads=[ptR], writes=[selbTR[g][i % 2]])
            def make_branch(i, g):
                def branch(bidx, js, Kt, KR, Vt, VR, sel):
                    po, poR = bank()
                    po3 = po[:, :].rearrange("p (h w) -> p h w", w=128)
                    n = len(js)
                    ets = []

                    def pv(t):
                        ej, ejR, j = ets[t]
                        for hp in range(4):
                            last = (t == n - 1 and hp == 3)
                            fw.op(fw.pe, lambda e, o=po3[:, hp, 0:65], l=ej[:, hp * 128:(hp + 1) * 128], r=Vt[:, j, g, 0:65], st=(t == 0 and hp == 0), sp=last:
                                  e.matmul(o, lhsT=l, rhs=r, start=st, stop=sp, skip_group_check=True),
                                  reads=[ejR, VR], writes=[poR], signal=(hp == 3))
                    for t, j in enumerate(js):
                        while True:
                            ps_, psR = bank()
                            if ps_ is not po:
                                break
                        ps3 = ps_[:, :].rearrange("p (h q) -> p h q", q=128)
                        if sel:
                            fw.op(fw.pe, f_mm(ps3, Kt[g][:, j * 128:(j + 1) * 128], q4(g, i), start=True, stop=False),
                                  reads=[KR, qTR], writes=[psR], signal=False)
                            fw.op(fw.pe, f_mm(ps3, eblk[:, j, :], selbT[g][i % 2][:, :, :], start=False, stop=True), reads=[cR, selbTR[g][i % 2]], writes=[psR])
                        else:
                            fw.op(fw.pe, f_mm(ps3, Kt[g][:, j * 128:(j + 1) * 128], q4(g, i)), reads=[KR, qTR], writes=[psR])
                        ej, ejR = eslot(g)
                        fw.op(fw.act, f_act(ej[:, :], ps_[:, :], AF.Exp, scale=SCALE), reads=[psR], writes=[ejR])
                        if j == i:
                            fw.op(fw.dve, f_tt(ej[:, :], ej[:, :], tri0[:, :], ALU.mult), reads=[ejR, cR], writes=[ejR])
                        elif bidx == 2 and j == i - 4:
                            fw.op(fw.dve, f_tt(ej[:, :], ej[:, :], tri4[:, :], ALU.mult), reads=[ejR, cR], writes=[ejR])
                        ets.append((ej, ejR, j))
                        if t >= 2:
                            pv(t - 2)
                    for t in range(max(0, n - 2), n):
                        pv(t)
                    pst = pos_[g][bidx]
                    fw.op(fw.dve, f_copy(pst[:, :, 0:65], po3[:, :, 0:65]), reads=[poR], writes=[posR[g][bidx]])
                    finish_branch(bidx, g, i, pst, posR[g][bidx], False)

                return branch

            def win_part(i, g):
                make_branch(i, g)(2, list(range(max(0, i - 4), i + 1)), kwZ, kzR, Vw, VwR, False)

            def slc_part(i, g):
                make_branch(i, g)(1, list(range(i + 1)), ksZ, kzR, Vs, VsR, i >= 8)

            def combine(i):
                ab = acc[i % 3]
                aRs = accR[i % 3]
                fw.op(fw.dve, f_tt(sqn[:, :, :], ab[:, :, :], ab[:, :, :], ALU.mult), reads=aRs, writes=[njR])
                fw.op(fw.dve, lambda e: e.tensor_reduce(out=nst[:, 0:8], in_=sqn[:, :, :], axis=mybir.AxisListType.X, op=ALU.add), reads=[njR], writes=[nstR])
                fw.op(fw.act, f_act(nst[:, 8:16], nst[:, 0:8], AF.Ln, scale=1.0 / 64, bias=epsT[:, 0:1]), reads=[nstR, cR], writes=[nstR])
                fw.op(fw.act, f_act(nst[:, 8:16], nst[:, 8:16], AF.Exp, scale=-0.5), reads=[nstR], writes=[nstR])
                fw.op(fw.dve, f_tt(ab[:, :, :], ab[:, :, :], nst[:, 8:16].unsqueeze(2).broadcast_to([128, 8, 64]), ALU.mult),
                      reads=aRs + [nstR], writes=aRs)
                fw.op(fw.dve, f_tt(mxn[:, :], ab[:, :, :].rearrange("p h d -> p (h d)"), ngain[:, :], ALU.mult), reads=aRs + [cR], writes=[mxnR])
                pt, ptR = bank()
                ptb = pt[:, :].bitcast(BF16)
                for c in range(4):
                    fw.op(fw.pe, f_tr(ptb[:, c * 128:(c + 1) * 128], mxn[:, c * 128:(c + 1) * 128], identb[:]), reads=[mxnR, cR], writes=[ptR],
                          signal=(c == 3))
                fw.op(fw.act, f_act(MIXTb[:, 4:8, i * 128:(i + 1) * 128], ptb[:, 0:512].rearrange("p (c t) -> p c t", t=128), AF.Copy),
                      reads=[ptR], writes=[MX_R[i]])

            lanes = [[], [], []]
            lbanks = [[0, 1, 2, 3], [4, 5, 6]]

            def rec_part(fn, i, g):
                lane_banks[0] = lbanks[g]
                fw.rec_begin()
                fn(i, g)
                return fw.rec_end()
            for g in range(2):
                lanes[g] += rec_part(cmp_part, 0, g)
            lanes[2] += [None] * len(lanes[0])
            prev_cb = []
            for i in range(NT):
                n0 = len(lanes[0])
                for g in range(2):
                    lanes[g] += rec_part(win_part, i, g)
                    tail = []
                    if i + 1 < NT:
                        c_ = rec_part(cmp_part, i + 1, g)
                        if i + 1 >= 8:
                            tail = c_[-2:]
                            c_ = c_[:-2]
                        lanes[g] += c_
                    lanes[g] += rec_part(slc_part, i, g) + tail
                assert len(lanes[0]) == len(lanes[1])
                ntile = len(lanes[0]) - n0
                sp = []
                for e_ in prev_cb:
                    sp += [e_, None, None, None]
                assert len(sp) <= ntile, (len(sp), ntile)
                lanes[2] += sp + [None] * (ntile - len(sp))
                lane_banks[0] = [7]
                fw.rec_begin()
                combine(i)
                prev_cb = fw.rec_end()
            lanes[2] += prev_cb
            lane_banks[0] = None
            fw.replay_lanes(lanes)
            fw.barrier()
            if stop == "P3c":
                raise _Stop()

            if debug:
                wk.reset()
                dtmp = wk.f32([128, 2048])
                dR = Res()
                for c in range(8):
                    fw.op(fw.dve, f_copy(dtmp[:, :], MIXTb[:, c, :]), reads=[dR], writes=[dR])
                    fw.dma(fw.sp, f_dma(dbg["mix"][:, c * T:(c + 1) * T], dtmp[:, :]), reads=[dR], writes=[])
                    fw.barrier()

            wk.reset()
            wob = wk.bf16([128, 8, 1024])
            woR = Res()
            fw.dma(fw.pool, f_dma(wob[:].rearrange("p a b -> p (a b)").rearrange("p (s e) -> p s e", e=2048),
                                  wo_d.rearrange("p (s e) -> p s e", e=2048)), writes=[woR])
            wka = dict(st=[stat[:, 2 * q_:2 * q_ + 2] for q_ in range(4)], stR=[Res() for _ in range(4)], junk=wk.bf16([128, D]), junkR=Res(),
                       xb=[wk.bf16([128, D]) for _ in range(4)], xbR=[Res() for _ in range(4)])
            HT_R = [Res() for _ in range(NT)]
            for i in range(NT):
                fw.dma(fw.sp, f_dma(X1[:, i * D:(i + 1) * D], x_tiles[i]), writes=[X1_R[i]])
            lanes = [[], [], [], []]
            p4_tails = [[], [], [], []]
            for i in range(NT):
                xi = X1[:, i * D:(i + 1) * D]
                lane_banks[0] = [2 * (i % 4), 2 * (i % 4) + 1]
                fw.rec_begin()
                for hf in range(2):
                    pb, pR = bank()
                    mmg(pb[:, :], pR, [(MIXTb[:, c, i * 128:(i + 1) * 128], wob[:, c, hf * 512:(hf + 1) * 512]) for c in range(8)], [MX_R[i], woR])
                    fw.op(fw.dve, f_tt(xi[:, hf * 512:(hf + 1) * 512], xi[:, hf * 512:(hf + 1) * 512], pb[:, :], ALU.add), reads=[pR, X1_R[i]], writes=[X1_R[i]])
                norm_to_HT(xi, X1_R[i], i, g2, wka)
                it_ = fw.rec_end()
                body_, tail_ = it_[:-9], it_[-9:]
                lanes[i % 4] += body_[:9] + p4_tails[i % 4] + body_[9:]
                p4_tails[i % 4] = tail_
            for q_ in range(4):
                lanes[q_] += p4_tails[q_]
            lane_banks[0] = None
            fw.replay_lanes(lanes, stagger=8)
            fw.barrier()
            if stop == "P4":
                raise _Stop()
            if debug:
                for i in range(NT):
                    fw.dma(fw.sp, f_dma(dbg["x1"][i * 128:(i + 1) * 128, :], X1[:, i * D:(i + 1) * D]), reads=[X1_R[i]], writes=[])
                fw.barrier()

            wk.reset()
            ma.reset()
            actb = [ma.bf16([128, FG, T]) for _ in range(2)]
            actR = [[Res() for _ in range(FG)] for _ in range(2)]
            NGU, NWD = 3, 12
            wgu = [wk.bf16([128, 2, 8, 128]) for _ in range(NGU)]
            wguR = [Res() for _ in range(NGU)]
            wdn = [wk.bf16([128, 1024]) for _ in range(NWD)]
            wdnR = [Res() for _ in range(NWD)]
            gsb = [wk.f32([128, 514]) for _ in range(2)]
            gsbR = [Res(), Res()]
            c0 = [wk.f32([128, 512]) for _ in range(2)]
            c0R = [Res(), Res()]
            c1 = [wk.f32([128, 512]) for _ in range(2)]
            c1R = [Res(), Res()]
            sl_ = [wk.f32([128, 512]) for _ in range(2)]
            slR = [Res(), Res()]
            groups = [list(range(s, min(s + FG, NF))) for s in range(0, NF, FG)]
            blk = [0]

            def ffn_down(gi):
                fl = groups[gi]
                ab = actb[gi % 2]
                for i in range(NT):
                    for hf in range(2):
                        pb, pR = bank()
                        mmg(pb[:, :], pR, [(ab[:, k, i * 128:(i + 1) * 128], wdn[f % NWD][:, hf * 512:(hf + 1) * 512]) for k, f in enumerate(fl)],
                            [actR[gi % 2][k] for k in range(len(fl))] + [wdnR[f % NWD] for f in fl])
                        xi = X1[:, i * D + hf * 512:i * D + (hf + 1) * 512]
                        fw.op(fw.dve, f_tt(xi, xi, pb[:, :], ALU.add), reads=[pR, X1_R[i]], writes=[X1_R[i]])

            def ffn_load(f):
                if f >= NF:
                    return
                ws = f % NGU
                fw.dma(fw.pool, f_dma(wgu[ws][:, 0, :, :].rearrange("p a b -> p (a b)"), wg_d[:, f, :]), writes=[wguR[ws]])
                fw.dma(fw.pool, f_dma(wgu[ws][:, 1, :, :].rearrange("p a b -> p (a b)"), wu_d[:, f, :]), writes=[wguR[ws]])
                fw.dma(fw.pool, f_dma(wdn[f % NWD][:, :], wd_d[:, f, :]), writes=[wdnR[f % NWD]])

            pending = []

            def flush_tail():
                while pending:
                    s_, ab_, k_, sl_c, pu_, puR_ = pending.pop(0)
                    fw.op(fw.act, f_act(sl_[s_][:, :], c0[s_][:, :], AF.Silu), reads=[c0R[s_]], writes=[slR[s_]])
                    fw.op(fw.dve, f_tt(actb[ab_][:, k_, sl_c], sl_[s_][:, :], pu_[:, :], ALU.mult), reads=[slR[s_], puR_], writes=[actR[ab_][k_]])

            PF = 2
            for f in range(PF):
                ffn_load(f)
            for gi, fl in enumerate(groups):
                for k, f in enumerate(fl):
                    ws = f % NGU
                    ffn_load(f + PF)
                    for tb in range(4):
                        s = blk[0] % 2
                        blk[0] += 1
                        sl = slice(tb * 512, (tb + 1) * 512)
                        pg, pgR = bank()
                        mmg(pg[:, :], pgR, [(wgu[ws][:, 0, c, :], HT[:, c, sl]) for c in range(8)], HT_R[4 * tb:4 * tb + 4] + [wguR[ws]])
                        pu, puR = bank()
                        mmg(pu[:, :], puR, [(wgu[ws][:, 1, c, :], HT[:, c, sl]) for c in range(8)], HT_R[4 * tb:4 * tb + 4] + [wguR[ws]])
                        if tb == 0:
                            fw.op(fw.dve, f_memset(gsb[s][:, 0:2], 0.0), writes=[gsbR[s]])
                        else:
                            fw.op(fw.act, f_act(gsb[s][:, 0:2], gsb[1 - s][:, 512:514], AF.Copy), reads=[gsbR[1 - s]], writes=[gsbR[s]])
                        fw.op(fw.act, f_act(gsb[s][:, 2:514], pg[:, :], AF.Copy), reads=[pgR], writes=[gsbR[s]])
                        fw.op(fw.act, f_act(c0[s][:, :], pg[:, :], AF.Identity, scale=convw[:, f, 2:3], bias=convb[:, f:f + 1]), reads=[pgR, cR], writes=[c0R[s]])
                        fw.op(fw.dve, f_stt(c1[s][:, :], gsb[s][:, 1:513], convw[:, f, 1:2], c0[s][:, :], ALU.mult, ALU.add),
                              reads=[gsbR[s], c0R[s], cR], writes=[c1R[s]])
                        fw.op(fw.dve, f_stt(c0[s][:, :], gsb[s][:, 0:512], convw[:, f, 0:1], c1[s][:, :], ALU.mult, ALU.add),
                              reads=[gsbR[s], c1R[s], cR], writes=[c0R[s]])
                        flush_tail()
                        pending.append((s, gi % 2, k, sl, pu, puR))
                flush_tail()
                if gi >= 1:
                    ffn_down(gi - 1)
            ffn_down(len(groups) - 1)

            outR = Res()
            out_tiles = out_d.rearrange("(n p) d -> n p d", p=128)
            fjunk = wk.bf16([128, D]) if False else c0[0]
            lanes = [[], [], [], []]
            p6R = [Res() for _ in range(4)]
            for i in range(NT):
                xi = X1[:, i * D:(i + 1) * D]
                s = i % 2
                st = stat[:, 8 + 2 * (i % 4):10 + 2 * (i % 4)]
                stR = p6R[i % 4]
                fw.rec_begin()
                fw.op(fw.act, f_act(sl_[s][:, :].bitcast(BF16), xi, AF.Square, accum_out=st[:, 0:1]), reads=[X1_R[i]], writes=[slR[s], stR])
                rstd_from_ss(st[:, 0:1], D, st[:, 1:2], stR)
                fw.op(fw.dve, f_stt(xi, xi, st[:, 1:2], fgain[:, :], ALU.mult, ALU.mult), reads=[X1_R[i], stR, cR], writes=[X1_R[i]])
                fw.dma(fw.sp, f_dma(out_tiles[i], xi), reads=[X1_R[i]], writes=[])
                lanes[i % 4] += fw.rec_end()
            fw.replay_lanes(lanes, stagger=1)
            fw.barrier()
            if stop == "P6":
                raise _Stop()


        except _Stop:
            fw.barrier()
        run = fw.runner(sems)
        with nc.Block() as block:
            @block.tensor
            def _(e):
                run(fw.pe, e)

            @block.scalar
            def _(e):
                run(fw.act, e)

            @block.vector
            def _(e):
                run(fw.dve, e)

            @block.gpsimd
            def _(e):
                run(fw.pool, e)

            @block.sync
            def _(e):
                run(fw.sp, e)
    return nc


def _consts():
    c = {}
    c["c_ident"] = np.eye(128, dtype=np.float32)
    P = np.zeros((128, 128), np.float32)
    for po in range(128):
        j = po % 64
        if j < 8:
            P[po + 8, po] = -1.0
        elif j < 16:
            P[po - 8, po] = 1.0
    c["c_perm"] = P
    invf = np.zeros((128, 1), np.float32)
    half = 8
    inv_freq = (500000.0 ** (-np.arange(half, dtype=np.float32) * 2.0 / 16)).astype(np.float32)
    for p in range(128):
        j = p % 64
        if j < 16:
            invf[p, 0] = inv_freq[j % 8] / (2 * np.pi)
    c["c_invf"] = invf
    pk = np.arange(128)[:, None]
    pq = np.arange(128)[None, :]
    tri0 = (pq >= pk).astype(np.float32)
    c["c_tri0"] = np.tile(tri0, (1, 4))
    c["c_tri4"] = np.tile(1.0 - tri0, (1, 4))
    eb = np.zeros((128, 16, 128), np.float32)
    for j in range(16):
        for p in range(128):
            eb[2 * j + p // 64, j, p] = 1.0
    c["c_eblk"] = eb.reshape(128, 16 * 128)
    n = np.arange(128)[:, None]
    t = np.arange(T)[None, :]
    cm = ((16 * n + 31) <= t).astype(np.float32)
    cm[127, :] = 0
    c["c_cmpm"] = (cm - 1.0) * 30000.0
    keep = np.zeros((128, 8, 32), np.float32)
    add = np.zeros((128, 8, 32), np.float32)
    for i in range(8, 16):
        for p in range(128):
            cur = 2 * i + (1 if p >= 64 else 0)
            for s in range(32):
                valid = s <= cur
                forced = (s == 0) or (s == cur) or (s == cur - 1)
                if not valid:
                    add[p, i - 8, s] = -1.0
                elif forced:
                    add[p, i - 8, s] = 1e4
                else:
                    keep[p, i - 8, s] = 1.0
    c["c_keep"] = keep.reshape(128, 256)
    c["c_add"] = add.reshape(128, 256)
    ovl = np.zeros((128, 32), np.float32)
    for nn in range(127):
        for s in range(32):
            if (16 * nn <= 64 * s + 63) and (16 * nn + 31 >= 64 * s):
                ovl[nn, s] = 1.0
    c["c_ovl"] = ovl
    return c


def _prep_shared(inp):
    f = lambda a: np.ascontiguousarray(a, dtype=np.float32)
    w_in = np.asarray(inp["w_in"][0])
    w3 = w_in.reshape(8, 128, -1)

    def fm_chunk(cols):
        return w3[:, :, cols].transpose(1, 0, 2)
    chunks = []
    for h in range(4):
        chunks.append(fm_chunk(np.arange(h * 128, (h + 1) * 128)))
    for h in range(4):
        chunks.append(fm_chunk(np.arange(512 + h * 128, 512 + (h + 1) * 128)))
    for c in range(4):
        cols = np.concatenate([2048 + c * 64 + np.arange(64), 2048 + (4 + c) * 64 + np.arange(64)])
        chunks.append(fm_chunk(cols))
    for base in (2560, 2688, 2816, 3072):
        chunks.append(fm_chunk(np.arange(base, base + 128)))
    sh = {}
    sh["w_fm"] = f(np.stack(chunks, axis=1).reshape(128, 16, 1024))
    sh["w_tm_i"] = f(w3[:, :, 1024:1536].transpose(1, 0, 2).reshape(128, -1))
    sh["w_tm_g"] = f(w3[:, :, 1536:2048].transpose(1, 0, 2).reshape(128, -1))
    ncols = np.concatenate([np.arange(2944, 3072), np.arange(3200, 3328), np.arange(3328, 3352)])
    sh["w_tm_n"] = f(w3[:, :, ncols].transpose(1, 0, 2).reshape(128, -1))
    sh["w_o"] = f(np.asarray(inp["w_o"][0]).reshape(8, 128, 1024).transpose(1, 0, 2).reshape(128, -1))
    for nm, key in (("w_gate", "ffn_w_gate"), ("w_up", "ffn_w_up")):
        w = np.asarray(inp[key][0]).reshape(8, 128, NF, 128)
        sh[nm] = f(w.transpose(1, 2, 0, 3).reshape(128, NF, 1024))
    sh["w_down"] = f(np.asarray(inp["ffn_w_down"][0]).reshape(NF, 128, 1024).transpose(1, 0, 2))
    for nm, key in (("w1k", "cmp_k_w1"), ("w1v", "cmp_v_w1")):
        w = np.asarray(inp[key][0]).reshape(32, 64, 256).transpose(1, 0, 2)
        sh[nm] = f(np.concatenate([w, w], axis=0).reshape(128, -1))
    for nm, key in (("w2k", "cmp_k_w2"), ("w2v", "cmp_v_w2")):
        sh[nm] = f(np.asarray(inp[key][0]).reshape(2, 128, 64).transpose(1, 0, 2).reshape(128, -1))
    for nm, key in (("pek", "cmp_pe_k"), ("pev", "cmp_pe_v")):
        pe = np.asarray(inp[key][0]).T
        sh[nm] = f(np.concatenate([pe, pe], axis=0))
    sh["g1"] = f(np.asarray(inp["ln1_gain"][0]).reshape(8, 128).T)
    sh["g2"] = f(np.asarray(inp["ln2_gain"][0]).reshape(8, 128).T)
    lbp = np.asarray(inp["hgrn_lb_param"])
    sh["lbp"] = f(lbp.reshape(2, 4, 128).transpose(2, 0, 1).reshape(128, 8))
    sh["hgain"] = f(np.asarray(inp["hgrn_out_gain"][0]).reshape(1, 512))
    sh["ngain"] = f(np.asarray(inp["nsa_out_gain"][0]).reshape(1, 512))
    sh["fgain"] = f(np.asarray(inp["final_gain"]).reshape(1, 1024))
    cw = np.asarray(inp["ffn_conv_w"][0])
    sh["convw"] = f(cw.reshape(3, NF, 128).transpose(2, 1, 0).reshape(128, NF * 3))
    sh["convb"] = f(np.asarray(inp["ffn_conv_b"][0]).reshape(NF, 128).T)
    sh.update(_consts())
    return sh


_NC_CACHE = {}


def kernel(**inputs):
    debug = bool(inputs.pop("_debug", False))
    cores = inputs.pop("_cores", None)
    sh = _prep_shared(inputs)
    x = np.asarray(inputs["x"], dtype=np.float32)
    pos = np.asarray(inputs["positions"]).astype(np.int32)
    core_ids = list(range(NCORES)) if cores is None else list(cores)
    if debug not in _NC_CACHE:
        _NC_CACHE[debug] = build_nc(debug)
    nc = _NC_CACHE[debug]
    in_maps = []
    for b in core_ids:
        m = dict(sh)
        m["x"] = np.ascontiguousarray(x[b])
        m["pos"] = np.ascontiguousarray(pos[b:b + 1])
        in_maps.append(m)
    res = run_bass_kernel_spmd(nc, in_maps, core_ids=core_ids)
    if debug:
        return res
    out = np.stack([np.asarray(r["out"], dtype=np.float32) for r in res.results], axis=0)
    return out
```

```python
import numpy as np
from contextlib import ExitStack
import concourse.bass as bass
import concourse.mybir as mybir
from concourse.bass_utils import run_bass_kernel_spmd

F32 = mybir.dt.float32
BF16 = mybir.dt.bfloat16
I32 = mybir.dt.int32
AF = mybir.ActivationFunctionType
ALU = mybir.AluOpType

T = 2048
D = 1024
NT = 16
DFF = 2816
NF = 22
EPS = 1e-6
PI = float(np.pi)
NCORES = 8
FG = 4


class Res:
    __slots__ = ("w", "r")

    def __init__(self):
        self.w = None
        self.r = []


class Eng:
    def __init__(self, name):
        self.name = name
        self.q = []
        self.count = 0
        self.waited = {}
        self.pending = False


class FW:
    def __init__(self, n_dma_sems=12):
        self.pe = Eng("tensor")
        self.act = Eng("scalar")
        self.dve = Eng("vector")
        self.pool = Eng("gpsimd")
        self.sp = Eng("sync")
        self.engs = [self.pe, self.act, self.dve, self.pool, self.sp]
        self.n_dma_sems = n_dma_sems
        self.dma_state = {q: dict(next=0, issued=[0] * n_dma_sems) for q in ("sync", "gpsimd")}
        self._rec = None

    def rec_begin(self):
        self._rec = []

    def rec_end(self):
        r = self._rec
        self._rec = None
        return r

    def replay_lanes(self, lanes, stagger=0):
        if stagger:
            lanes = [[None] * (k_ * stagger) + list(l) for k_, l in enumerate(lanes)]
        idx = [0] * len(lanes)
        live = True
        while live:
            live = False
            for k, lane in enumerate(lanes):
                if idx[k] < len(lane):
                    ent = lane[idx[k]]
                    idx[k] += 1
                    live = True
                    if ent is None:
                        continue
                    if ent[0] == "op":
                        self.op(ent[1], ent[2], ent[3], ent[4], ent[5])
                    else:
                        self.dma(ent[1], ent[2], ent[3], ent[4])

    def _wait(self, eng, ev):
        if ev is None:
            return
        key, val = ev
        if eng is self.pe and key == "tensor":
            return
        if eng.waited.get(key, 0) >= val:
            return
        eng.waited[key] = val
        eng.q.append(("wait", key, val))

    def _deps(self, eng, reads, writes):
        for r in reads:
            self._wait(eng, r.w)
        for w in writes:
            self._wait(eng, w.w)
            for ev in w.r:
                self._wait(eng, ev)

    def op(self, eng, fn, reads=(), writes=(), signal=True):
        if self._rec is not None:
            self._rec.append(("op", eng, fn, list(reads), list(writes), signal))
            return None
        self._deps(eng, reads, writes)
        if signal:
            eng.count += 1
            ev = (eng.name, eng.count)
            eng.pending = False
        else:
            ev = (eng.name, eng.count + 1)
            eng.pending = True
        eng.q.append(("op", fn, signal))
        for r in reads:
            r.r.append(ev)
        for w in writes:
            w.w = ev
            w.r = []
        return ev

    def dma(self, qeng, fn, reads=(), writes=()):
        if self._rec is not None:
            self._rec.append(("dma", qeng, fn, list(reads), list(writes)))
            return None
        st = self.dma_state[qeng.name]
        k = st["next"]
        st["next"] = (k + 1) % self.n_dma_sems
        key = "dma_%s_%d" % (qeng.name, k)
        if st["issued"][k] > 0:
            self._wait(qeng, (key, 16 * st["issued"][k]))
        self._deps(qeng, reads, writes)
        st["issued"][k] += 1
        ev = (key, 16 * st["issued"][k])
        qeng.q.append(("dma", fn, key))
        for r in reads:
            r.r.append(ev)
        for w in writes:
            w.w = ev
            w.r = []
        return ev

    def barrier(self):
        evs = []
        for e in self.engs:
            assert not e.pending
            if e.count > 0:
                evs.append((e.name, e.count))
        for q, st in self.dma_state.items():
            for k in range(self.n_dma_sems):
                if st["issued"][k] > 0:
                    evs.append(("dma_%s_%d" % (q, k), 16 * st["issued"][k]))
        for e in self.engs:
            for ev in evs:
                self._wait(e, ev)

    def sem_keys(self):
        keys = [e.name for e in self.engs]
        for q in self.dma_state:
            for k in range(self.n_dma_sems):
                keys.append("dma_%s_%d" % (q, k))
        return keys

    def runner(self, sems):
        def run(eng, h):
            own = sems[eng.name]
            pend = []
            for item in eng.q:
                if item[0] == "wait":
                    pend.append((sems[item[1]], item[2]))
                    continue
                for (s, v) in pend:
                    h.wait_ge(s, v)
                pend = []
                ins = item[1](h)
                if item[0] == "op":
                    if item[2]:
                        ins.then_inc(own, 1)
                else:
                    ins.then_inc(sems[item[2]], 16)
            for (s, v) in pend:
                h.wait_ge(s, v)
        return run


def f_mm(out, lhsT, rhs, start=True, stop=True):
    return lambda e: e.matmul(out, lhsT=lhsT, rhs=rhs, start=start, stop=stop)


def f_tr(out, in_, ident):
    return lambda e: e.transpose(out=out, in_=in_, identity=ident)


def f_act(out, in_, func, **kw):
    return lambda e: e.activation(out=out, in_=in_, func=func, **kw)


def f_tt(out, in0, in1, op):
    return lambda e: e.tensor_tensor(out=out, in0=in0, in1=in1, op=op)


def f_ts(out, in0, s1, s2, op0, op1=None):
    if op1 is None:
        return lambda e: e.tensor_scalar(out=out, in0=in0, scalar1=s1, scalar2=None, op0=op0)
    return lambda e: e.tensor_scalar(out=out, in0=in0, scalar1=s1, scalar2=s2, op0=op0, op1=op1)


def f_stt(out, in0, scalar, in1, op0, op1):
    return lambda e: e.scalar_tensor_tensor(out=out, in0=in0, scalar=scalar, in1=in1, op0=op0, op1=op1)


def f_copy(out, in_):
    return lambda e: e.tensor_copy(out=out, in_=in_)


def f_dma(out, in_):
    return lambda e: e.dma_start(out=out, in_=in_)


def f_memset(ap, v):
    return lambda e: e.memset(ap, v)


class Arena:
    def __init__(self, ap, n):
        self.ap = ap
        self.n = n
        self.off = 0

    def reset(self):
        self.off = 0

    def _take(self, nf):
        assert self.off + nf <= self.n, "arena overflow %d + %d > %d" % (self.off, nf, self.n)
        a = self.ap[:, self.off:self.off + nf]
        self.off += nf
        return a

    @staticmethod
    def _shape(a, shape):
        if shape[0] != 128:
            a = a[0:shape[0], :]
        if len(shape) == 2:
            return a
        if len(shape) == 3:
            return a.rearrange("p (a b) -> p a b", b=shape[2])
        return a.rearrange("p (a b c) -> p a b c", b=shape[2], c=shape[3])

    def f32(self, shape):
        n = int(np.prod(shape[1:]))
        return self._shape(self._take(n), shape)

    def i32(self, shape):
        n = int(np.prod(shape[1:]))
        return self._shape(self._take(n).bitcast(I32), shape)

    def bf16(self, shape):
        n = int(np.prod(shape[1:]))
        nf = (n + 1) // 2
        a = self._take(nf).bitcast(BF16)
        if 2 * nf != n:
            a = a[:, 0:n]
        return self._shape(a, shape)


class _Stop(Exception):
    pass


def build_nc(debug=False, stop=None):
    nc = bass.Bass("TRN2", target_bir_lowering=False)
    fw = FW()

    def din(name, shape, dt=F32):
        return nc.dram_tensor(name, list(shape), dt, kind="ExternalInput").ap()

    x_d = din("x", [T, D])
    pos_d = din("pos", [1, T], I32)
    wfm_d = din("w_fm", [128, 16, 1024])
    wti_d = din("w_tm_i", [128, 8 * 512])
    wtg_d = din("w_tm_g", [128, 8 * 512])
    wtn_d = din("w_tm_n", [128, 8 * 280])
    wo_d = din("w_o", [128, 8 * 1024])
    wg_d = din("w_gate", [128, NF, 1024])
    wu_d = din("w_up", [128, NF, 1024])
    wd_d = din("w_down", [128, NF, 1024])
    w1k_d = din("w1k", [128, 32 * 256])
    w1v_d = din("w1v", [128, 32 * 256])
    w2k_d = din("w2k", [128, 2 * 64])
    w2v_d = din("w2v", [128, 2 * 64])
    pek_d = din("pek", [128, 32])
    pev_d = din("pev", [128, 32])
    g1_d = din("g1", [128, 8])
    g2_d = din("g2", [128, 8])
    lbp_d = din("lbp", [128, 8])
    hgain_d = din("hgain", [1, 512])
    ngain_d = din("ngain", [1, 512])
    fgain_d = din("fgain", [1, 1024])
    cw_d = din("convw", [128, NF * 3])
    cb_d = din("convb", [128, NF])
    c_ident = din("c_ident", [128, 128])
    c_perm = din("c_perm", [128, 128])
    c_invf = din("c_invf", [128, 1])
    c_tri0 = din("c_tri0", [128, 512])
    c_tri4 = din("c_tri4", [128, 512])
    c_eblk = din("c_eblk", [128, 16 * 128])
    c_cmpm = din("c_cmpm", [128, T])
    c_keep = din("c_keep", [128, 8 * 32])
    c_add = din("c_add", [128, 8 * 32])
    c_ovl = din("c_ovl", [128, 32])
    out_d = nc.dram_tensor("out", [T, D], F32, kind="ExternalOutput").ap()
    dbg = {}
    if debug:
        dbg["mix"] = nc.dram_tensor("dbg_mix", [128, 8 * T], F32, kind="ExternalOutput").ap()
        dbg["x1"] = nc.dram_tensor("dbg_x1", [T, D], F32, kind="ExternalOutput").ap()

    with ExitStack() as es:
        def sb(name, shape, dt):
            return es.enter_context(nc.sbuf_tensor("s_" + name, shape, dt))

        X1 = sb("X1", [128, NT * D], F32)
        HT = sb("HT", [128, 8, T], BF16)
        MIXT = sb("MIXT", [128, 8 * T // 2], F32)
        MIXTb = MIXT[:, :].bitcast(BF16).rearrange("p (c t) -> p c t", t=T)
        identb = sb("identb", [128, 128], BF16)
        permf = sb("permf", [128, 128], F32)
        invf = sb("invf", [128, 1], F32)
        tri0 = sb("tri0", [128, 512], BF16)
        tri4 = sb("tri4", [128, 512], BF16)
        eblk = sb("eblk", [128, 16, 128], BF16)
        cmpm = sb("cmpm", [128, T], BF16)
        keep_t = sb("keep_t", [128, 8, 32], F32)
        add_t = sb("add_t", [128, 8, 32], F32)
        g1 = sb("g1s", [128, 8], F32)
        g2 = sb("g2s", [128, 8], F32)
        lbp = sb("lbps", [128, 8], F32)
        lb = sb("lb", [128, 4], F32)
        oml = sb("oml", [128, 4], F32)
        hgain = sb("hgain", [128, 512], F32)
        ngain = sb("ngain", [128, 512], F32)
        fgain = sb("fgain", [128, 1024], F32)
        convw = sb("convw", [128, NF, 3], F32)
        convb = sb("convb", [128, NF], F32)
        stat = sb("stat", [128, 64], F32)
        WKN = 13912
        WK = sb("WK", [128, WKN], F32)
        pbs = [es.enter_context(nc.psum_tensor("pb%d" % k, [128, 512], F32)) for k in range(8)]
        pbR = [Res() for _ in range(8)]
        sems = {k: es.enter_context(nc.semaphore(k)) for k in fw.sem_keys()}

        cR = Res()
        bank_i = [0]
        lane_banks = [None]
        lane_pos = {}

        def bank():
            if lane_banks[0] is not None:
                ids = lane_banks[0]
                key = tuple(ids)
                p = lane_pos.get(key, 0)
                lane_pos[key] = (p + 1) % len(ids)
                k = ids[p]
                return pbs[k], pbR[k]
            k = bank_i[0]
            bank_i[0] = (k + 1) % 8
            return pbs[k], pbR[k]

        wk = Arena(WK[:, :], WKN)
        xa = Arena(X1[:, :], NT * D)
        ma = Arena(MIXT[:, :], 8 * T // 2)


        def mmg(outap, outres, pairs, reads):
            n = len(pairs)
            for k, (l, r) in enumerate(pairs):
                fw.op(fw.pe, f_mm(outap, l, r, start=(k == 0), stop=(k == n - 1)), reads=reads, writes=[outres],
                      signal=(k == n - 1))

        wk.reset()
        xt = [wk.f32([128, D]) for _ in range(4)]
        xtR = [Res() for _ in range(4)]
        x_tiles = x_d.rearrange("(n p) d -> n p d", p=128)
        for i in range(4):
            fw.dma(fw.sp, f_dma(xt[i][:, :], x_tiles[i]), writes=[xtR[i]])
        ev_id = fw.dma(fw.pool, f_dma(identb[:], c_ident), writes=[Res()])
        ev_g1 = fw.dma(fw.sp, f_dma(g1[:], g1_d), writes=[Res()])
        epsT = sb("epsT", [128, 1], F32)
        ev_eps = fw.op(fw.pool, f_memset(epsT[:], EPS), writes=[Res()])
        oneT = sb("oneT", [128, 1], F32)
        fw.op(fw.pool, f_memset(oneT[:], 1.0), writes=[Res()])
        fw._wait(fw.pe, ev_id)
        fw._wait(fw.dve, ev_g1)
        fw._wait(fw.act, ev_eps)
        xa.reset()
        _v_tm_pre = xa.bf16([128, NT, 512])
        wtm_pre = [xa.bf16([128, 8, 512]) for _ in range(2)]
        wtmR_pre = [Res(), Res()]
        fw.dma(fw.pool, f_dma(wtm_pre[0][:].rearrange("p a b -> p (a b)").rearrange("p (s e) -> p s e", e=2048),
                              wti_d.rearrange("p (s e) -> p s e", e=2048)), writes=[wtmR_pre[0]])
        mhi_pre = Arena(MIXT[:, 4096:8192], 4096)
        wfm_pre = [mhi_pre.bf16([128, 8, 128]) for _ in range(3)]
        wfmR_pre = [Res() for _ in range(4)]
        fw.dma(fw.pool, f_dma(wfm_pre[0][:].rearrange("p a b -> p (a b)"), wfm_d[:, 0, :]), writes=[wfmR_pre[0]])
        fw.dma(fw.pool, f_dma(wfm_pre[1][:].rearrange("p a b -> p (a b)"), wfm_d[:, 4, :]), writes=[wfmR_pre[1]])
        fw.dma(fw.pool, f_dma(wtm_pre[1][:].rearrange("p a b -> p (a b)").rearrange("p (s e) -> p s e", e=2048),
                              wtg_d.rearrange("p (s e) -> p s e", e=2048)), writes=[wtmR_pre[1]])
        for (dst, src) in [(permf[:], c_perm), (invf[:], c_invf), (keep_t[:].rearrange("p a b -> p (a b)"), c_keep),
                           (add_t[:].rearrange("p a b -> p (a b)"), c_add), (g2[:], g2_d), (lbp[:], lbp_d),
                           (hgain[:], hgain_d.broadcast_to([128, 512])), (ngain[:], ngain_d.broadcast_to([128, 512])),
                           (fgain[:], fgain_d.broadcast_to([128, 1024])),
                           (convw[:].rearrange("p a b -> p (a b)"), cw_d), (convb[:], cb_d)]:
            fw.dma(fw.sp, f_dma(dst, src), writes=[Res()])
        for (dst, src) in [(tri0[:], c_tri0), (tri4[:], c_tri4),
                           (eblk[:].rearrange("p a b -> p (a b)"), c_eblk), (cmpm[:], c_cmpm)]:
            fw.dma(fw.pool, f_dma(dst, src), writes=[Res()])

        HT_R = [Res() for _ in range(NT)]
        MX_R = [Res() for _ in range(NT)]
        X1_R = [Res() for _ in range(NT)]

        def rstd_from_ss(ss_ap, n_feat, out_ap, res):
            fw.op(fw.act, f_act(out_ap, ss_ap, AF.Sqrt, scale=1.0 / n_feat, bias=epsT[:, 0:1]), reads=[res, cR], writes=[res])
            fw.op(fw.dve, lambda e: e.reciprocal(out_ap, out_ap), reads=[res], writes=[res])

        def norm_to_HT(src_tile_ap, src_res, i, gains, wka):
            st = wka["st"][i % 4]
            stR = wka["stR"][i % 4]
            junk = wka["junk"]
            fw.op(fw.act, f_act(junk[:, :], src_tile_ap, AF.Square, accum_out=st[:, 0:1]), reads=[src_res], writes=[wka["junkR"], stR])
            rstd_from_ss(st[:, 0:1], D, st[:, 1:2], stR)
            xb = wka["xb"][i % 4]
            xbR = wka["xbR"][i % 4]
            fw.op(fw.dve, f_ts(xb[:, :], src_tile_ap, st[:, 1:2], None, ALU.mult), reads=[src_res, stR], writes=[xbR])
            pt, ptR = bank()
            ptb = pt[:, :].bitcast(BF16)
            for c in range(8):
                fw.op(fw.pe, f_tr(ptb[:, c * 128:(c + 1) * 128], xb[:, c * 128:(c + 1) * 128], identb[:]), reads=[xbR, cR], writes=[ptR],
                      signal=(c == 7))
            fw.op(fw.dve, f_tt(HT[:, :, i * 128:(i + 1) * 128], ptb.rearrange("p (c t) -> p c t", t=128),
                               gains[:, 0:8].unsqueeze(2).broadcast_to([128, 8, 128]), ALU.mult),
                  reads=[ptR, cR], writes=[HT_R[i]])

        def chk(name):
            if stop == name:
                fw.barrier()
                raise _Stop()
        try:
            wka = dict(st=[stat[:, 2 * q_:2 * q_ + 2] for q_ in range(4)], stR=[Res() for _ in range(4)], junk=wk.bf16([128, D]), junkR=Res(),
                       xb=[wk.bf16([128, D]) for _ in range(4)], xbR=[Res() for _ in range(4)])
            lanes = [[], [], [], []]
            for i in range(NT):
                lane_banks[0] = [2 * (i % 4), 2 * (i % 4) + 1]
                fw.rec_begin()
                if i >= 4:
                    fw.dma(fw.sp, f_dma(xt[i % 4][:, :], x_tiles[i]), writes=[xtR[i % 4]])
                norm_to_HT(xt[i % 4][:, :], xtR[i % 4], i, g1, wka)
                lanes[i % 4] += fw.rec_end()
            lane_banks[0] = None
            fw.replay_lanes(lanes, stagger=4)
            fw.barrier()
            if stop == "P1":
                raise _Stop()

            wk.reset()
            xa.reset()
            lbR = Res()
            fw.op(fw.dve, f_tt(lb[:], lbp[:, 0:4], lbp[:, 4:8], ALU.subtract), writes=[lbR])
            fw.op(fw.act, f_act(lb[:], lb[:], AF.Sigmoid), reads=[lbR], writes=[lbR])
            fw.op(fw.dve, f_ts(oml[:], lb[:], -1.0, 1.0, ALU.mult, ALU.add), reads=[lbR], writes=[lbR])
            fw.op(fw.dve, f_ts(hgain[:, :], hgain[:, :], -1.0, None, ALU.mult), reads=[cR], writes=[cR])
            mhi = Arena(MIXT[:, 4096:8192], 4096)
            v_tm = xa.bf16([128, NT, 512])
            v_R = [Res() for _ in range(NT)]
            wtm = [xa.bf16([128, 8, 512]) for _ in range(2)]
            wtmR = wtmR_pre
            H2 = T // 4
            tq = [xa.f32([128, H2]) for _ in range(4)]
            tf = [xa.f32([128, H2]) for _ in range(4)]
            tb_ = [xa.f32([128, H2]) for _ in range(4)]
            te = [xa.f32([128, H2]) for _ in range(4)]
            tqR, tfR, tbR, teR = [[Res() for _ in range(4)] for _ in range(4)]
            qdT = wk.bf16([128, 4, T])
            kiT = wk.bf16([128, 4, T])
            kd_tm = wk.bf16([128, 4, NT, 128])
            rmask = wk.f32([128, H2])
            qdR = [Res() for _ in range(4)]
            kiR = [Res() for _ in range(4)]
            kdtR = [Res() for _ in range(4)]
            wfm = [mhi.bf16([128, 8, 128]) for _ in range(3)] + [wk.bf16([128, 8, 128])]
            wfmR = wfmR_pre
            kdT = [mhi.bf16([128, H2]) for _ in range(4)]
            kdR = [Res() for _ in range(4)]
            Sf = mhi.f32([128, 4, 128])
            SfR = [Res() for _ in range(4)]
            dec = mhi.f32([128, 4, NT])
            decR = Res()
            hst = mhi.f32([128, 16])
            fw.op(fw.pool, f_memset(rmask[:, :], 1.0), writes=[cR])
            fw.op(fw.pool, f_memset(rmask[:, :].rearrange("p (n c) -> p n c", c=128)[:, :, 0:1], 0.0), writes=[cR])
            def load_fm(chunk, ws):
                fw.dma(fw.pool, f_dma(wfm[ws][:].rearrange("p a b -> p (a b)"), wfm_d[:, chunk, :]), writes=[wfmR[ws]])
            for i in range(NT):
                pb, pR = bank()
                mmg(pb[:, :], pR, [(HT[:, c, i * 128:(i + 1) * 128], wtm[0][:, c, :]) for c in range(8)], [HT_R[i], wtmR[0]])
                fw.op(fw.act, f_act(v_tm[:, i, :], pb[:, :], AF.Copy), reads=[pR], writes=[v_R[i]])
            def fm_unit(h, u):
                s = u
                c0_ = u * H2
                cur_q, cur_f = 2 * (h % 2), 2 * (h % 2) + 1
                tbk = u
                pb, pR = bank()
                mmg(pb[:, :], pR, [(wfm[cur_f][:, c, :], HT[:, c, tbk * 512:(tbk + 1) * 512]) for c in range(8)],
                    HT_R[4 * tbk:4 * tbk + 4] + [wfmR[cur_f]])
                fw.op(fw.act, f_act(tf[s][:, :], pb[:, :], AF.Exp, scale=-1.0), reads=[pR], writes=[tfR[s]])
                fw.op(fw.act, f_act(tf[s][:, :], tf[s][:, :], AF.Ln, bias=oneT[:, 0:1]), reads=[tfR[s]], writes=[tfR[s]])
                fw.op(fw.act, f_act(tf[s][:, :], tf[s][:, :], AF.Exp, scale=-1.0), reads=[tfR[s]], writes=[tfR[s]])
                fw.op(fw.dve, f_ts(tf[s][:, :], tf[s][:, :], oml[:, h:h + 1], lb[:, h:h + 1], ALU.mult, ALU.add), reads=[tfR[s], lbR], writes=[tfR[s]])
                fw.op(fw.act, f_act(tb_[s][:, :], tf[s][:, :], AF.Ln), reads=[tfR[s]], writes=[tbR[s]])
                fw.op(fw.dve, (lambda s_: lambda e: e.tensor_tensor_scan(out=te[s_][:, :], data0=rmask[:, :], data1=tb_[s_][:, :], initial=0.0,
                                                                         op0=ALU.mult, op1=ALU.add))(s), reads=[tbR[s], cR], writes=[teR[s]])
                fw.op(fw.act, f_act(tb_[s][:, :], te[s][:, :], AF.Exp), reads=[teR[s]], writes=[tbR[s]])
                pq, pqR = bank()
                mmg(pq[:, :], pqR, [(wfm[cur_q][:, c, :], HT[:, c, tbk * 512:(tbk + 1) * 512]) for c in range(8)],
                    HT_R[4 * tbk:4 * tbk + 4] + [wfmR[cur_q]])
                fw.op(fw.dve, f_tt(qdT[:, h, c0_:c0_ + H2], pq[:, :], tb_[s][:, :], ALU.mult), reads=[pqR, tbR[s]], writes=[qdR[h]])
                fw.op(fw.act, f_act(tq[s][:, :], te[s][:, :], AF.Exp, scale=-1.0), reads=[teR[s]], writes=[tqR[s]])
                te3 = te[s][:, :].rearrange("p (n c) -> p n c", c=128)
                fw.op(fw.act, f_act(dec[:, h, 4 * u:4 * u + 4], te3[:, :, 127], AF.Exp), reads=[teR[s]], writes=[decR])
                fw.op(fw.dve, f_stt(kiT[:, h, c0_:c0_ + H2], tf[s][:, :], 1.0, tq[s][:, :], ALU.subtract, ALU.mult), reads=[tfR[s], tqR[s]], writes=[kiR[h]])
                fw.op(fw.dve, f_tt(kdT[s][:, :].rearrange("p (n c) -> p n c", c=128), kiT[:, h, c0_:c0_ + H2].rearrange("p (n c) -> p n c", c=128),
                                   dec[:, h, 4 * u:4 * u + 4].unsqueeze(2).broadcast_to([128, 4, 128]), ALU.mult), reads=[kiR[h], decR], writes=[kdR[s]])
                pt, ptR = bank()
                ptb = pt[:, :].bitcast(BF16)
                for k_ in range(4):
                    fw.op(fw.pe, f_tr(ptb[:, k_ * 128:(k_ + 1) * 128], kdT[s][:, k_ * 128:(k_ + 1) * 128], identb[:]), reads=[kdR[s], cR], writes=[ptR],
                          signal=(k_ == 3))
                fw.op(fw.dve, f_copy(kd_tm[:, h, 4 * u:4 * u + 4, :], ptb[:, 0:512].rearrange("p (k d) -> p k d", d=128)),
                      reads=[ptR], writes=[kdtR[h]])

            lanes = [[], [], [], []]
            fm_tails = [[], [], [], []]
            for h in range(4):
                pre = []
                if h < 3:
                    fw.rec_begin()
                    load_fm(h + 1, 2 * ((h + 1) % 2))
                    load_fm(4 + h + 1, 2 * ((h + 1) % 2) + 1)
                    pre = fw.rec_end()
                for u in range(4):
                    lane_banks[0] = [2 * u, 2 * u + 1]
                    fw.rec_begin()
                    fm_unit(h, u)
                    body = fw.rec_end()
                    if h == 0 and u > 0:
                        lanes[u] += [None] * (u * (len(body) // 4))
                    tail_ = body[-5:]
                    body = body[:-5]
                    mid = len(body) // 2
                    fill = pre if u == 0 else [None] * len(pre)
                    lanes[u] += body[:10] + fm_tails[u] + body[10:mid] + fill + body[mid:]
                    fm_tails[u] = tail_
            lane_banks[0] = None
            for u in range(4):
                lanes[u] += fm_tails[u]
            fw.replay_lanes(lanes)
            fw.barrier()
            wtn = MIXT[:, 4096:4096 + 1120].bitcast(BF16).rearrange("p (a b) -> p a b", b=280)
            wtnR = Res()
            fw.dma(fw.pool, f_dma(wtn[:, :, :], wtn_d.rearrange("p (a b) -> p a b", b=280)), writes=[wtnR])
            xb_ = Arena(X1[:, 8192:16384], 8192)
            atm = [xb_.bf16([128, 512]) for _ in range(2)]
            atmR = [Res(), Res()]
            sg = [xb_.f32([128, 512]) for _ in range(2)]
            sgR = [Res(), Res()]
            sq = [xb_.f32([128, 512]) for _ in range(2)]
            sqR = [Res(), Res()]
            t1 = [xb_.f32([128, 512]) for _ in range(2)]
            t1R = [Res(), Res()]
            mxh = [xb_.bf16([128, 512]) for _ in range(2)]
            mxhR = [Res(), Res()]
            Ssn = xb_.bf16([128, NT, 4, 128])
            SsnR = [Res() for _ in range(NT)]
            hstR = [Res(), Res()]
            fw.op(fw.pool, f_memset(Ssn[:, 0, :, :].rearrange("p a b -> p (a b)"), 0.0), writes=[SsnR[0]])
            fw.op(fw.pool, f_memset(Sf[:, :, :].rearrange("p a b -> p (a b)"), 0.0), writes=SfR)

            def s_step(m):
                pu, puR = bank()
                for h in range(4):
                    fw.op(fw.pe, f_mm(pu[:, h * 128:(h + 1) * 128], kd_tm[:, h, m, :], v_tm[:, m, h * 128:(h + 1) * 128]), reads=[kdtR[h], v_R[m]],
                          writes=[puR], signal=(h == 3))
                for h in range(4):
                    fw.op(fw.dve, f_stt(Sf[:, h, :], Sf[:, h, :], dec[:, h, m:m + 1], pu[:, h * 128:(h + 1) * 128], ALU.mult, ALU.add),
                          reads=[SfR[h], decR, puR], writes=[SfR[h]])
                fw.op(fw.pool, f_copy(Ssn[:, m + 1, :, :], Sf[:, :, :]), reads=SfR, writes=[SsnR[m + 1]])

            def o_tile(n):
                s = n % 2
                pa, paR = bank()
                for h in range(4):
                    fw.op(fw.pe, f_mm(pa[:, h * 128:(h + 1) * 128], kiT[:, h, n * 128:(n + 1) * 128], qdT[:, h, n * 128:(n + 1) * 128]), reads=[kiR[h], qdR[h]],
                          writes=[paR], signal=(h == 3))
                fw.op(fw.dve, f_tt(atm[s][:, :], pa[:, :], tri0[:, :], ALU.mult), reads=[paR, cR], writes=[atmR[s]])
                pg, pgR = bank()
                mmg(pg[:, :], pgR, [(HT[:, c, n * 128:(n + 1) * 128], wtm[1][:, c, :]) for c in range(8)], [HT_R[n], wtmR[1]])
                fw.op(fw.act, f_act(sg[s][:, :], pg[:, :], AF.Silu), reads=[pgR], writes=[sgR[s]])
                po, poR = bank()
                for h in range(4):
                    hs = slice(h * 128, (h + 1) * 128)
                    fw.op(fw.pe, f_mm(po[:, hs], atm[s][:, hs], v_tm[:, n, hs], start=True, stop=False), reads=[atmR[s], v_R[n]], writes=[poR], signal=False)
                    fw.op(fw.pe, f_mm(po[:, hs], qdT[:, h, n * 128:(n + 1) * 128], Ssn[:, n, h, :], start=False, stop=True), reads=[qdR[h], SsnR[n]], writes=[poR],
                          signal=(h == 3))
                fw.op(fw.act, f_act(sq[s][:, :], po[:, :], AF.Square), reads=[poR], writes=[sqR[s]])
                fw.op(fw.dve, lambda e, s_=s: e.tensor_reduce(out=hst[:, 8 * s_:8 * s_ + 4], in_=sq[s_][:, :].rearrange("p (h v) -> p h v", v=128),
                                                            axis=mybir.AxisListType.X, op=ALU.add), reads=[sqR[s]], writes=[hstR[s]])
                fw.op(fw.act, f_act(hst[:, 8 * s + 4:8 * s + 8], hst[:, 8 * s:8 * s + 4], AF.Sqrt, scale=1.0 / 128, bias=epsT[:, 0:1]), reads=[hstR[s], cR], writes=[hstR[s]])
                fw.op(fw.dve, lambda e, s_=s: e.reciprocal(hst[:, 8 * s_ + 4:8 * s_ + 8], hst[:, 8 * s_ + 4:8 * s_ + 8]), reads=[hstR[s]], writes=[hstR[s]])
                fw.op(fw.dve, f_tt(t1[s][:, :].rearrange("p (h v) -> p h v", v=128), po[:, :].rearrange("p (h v) -> p h v", v=128),
                                   hst[:, 8 * s + 4:8 * s + 8].unsqueeze(2).broadcast_to([128, 4, 128]), ALU.mult), reads=[poR, hstR[s]], writes=[t1R[s]])
                fw.op(fw.pool, f_tt(sg[s][:, :], sg[s][:, :], hgain[:, :], ALU.mult), reads=[sgR[s], cR], writes=[sgR[s]])
                fw.op(fw.dve, f_tt(mxh[s][:, :], t1[s][:, :], sg[s][:, :], ALU.mult), reads=[t1R[s], sgR[s]], writes=[mxhR[s]])
                pt, ptR = bank()
                ptb = pt[:, :].bitcast(BF16)
                for h in range(4):
                    fw.op(fw.pe, f_tr(ptb[:, h * 128:(h + 1) * 128], mxh[s][:, h * 128:(h + 1) * 128], identb[:]), reads=[mxhR[s], cR], writes=[ptR], signal=(h == 3))
                fw.op(fw.act, f_act(MIXTb[:, 0:4, n * 128:(n + 1) * 128], ptb[:, 0:512].rearrange("p (c t) -> p c t", t=128), AF.Copy), reads=[ptR], writes=[MX_R[n]])

            lanes = [[], [], []]
            lane_banks[0] = [0, 7]
            fw.rec_begin()
            for m in range(NT - 1):
                s_step(m)
            lanes[0] = fw.rec_end()
            tails = {1: [], 2: []}
            for n in range(NT):
                lane_banks[0] = [1, 2, 3] if n % 2 == 0 else [4, 5, 6]
                fw.rec_begin()
                o_tile(n)
                it_ = fw.rec_end()
                ln_ = 1 + n % 2
                body_, tail_ = it_[:-5], it_[-5:]
                lanes[ln_] += body_[:8] + tails[ln_] + body_[8:]
                tails[ln_] = tail_
            for ln_ in (1, 2):
                lanes[ln_] += tails[ln_]
            lane_banks[0] = None
            lanes[2] = [None] * 20 + lanes[2]
            fw.replay_lanes(lanes)
            fw.barrier()
            if stop == "P2":
                raise _Stop()

            wk.reset()
            xa.reset()
            qT = wk.bf16([128, 4, T])
            kcT = wk.bf16([128, T])
            vcT = wk.bf16([128, T])
            Vs = wk.bf16([128, NT, 2, 66])
            Vw = wk.bf16([128, NT, 2, 66])
            gates = wk.f32([128, NT, 24])
            qTR, kcR, vcR, ksR, kwR, VsR, VwR, gtR = [Res() for _ in range(8)]
            ksZ = [wk.bf16([128, T]) for _ in range(2)]
            kwZ = [wk.bf16([128, T]) for _ in range(2)]
            kzR = Res()
            for g_ in range(2):
                fw.op(fw.pool, f_memset(ksZ[g_][:, :], 0.0), writes=[kzR])
                fw.op(fw.pool, f_memset(kwZ[g_][:, :], 0.0), writes=[kzR])
            cosT = xa.f32([128, T])
            sinT = xa.f32([128, T])
            posi = xa.i32([128, T])
            posf = xa.f32([128, T])
            yy = xa.f32([128, T])
            wfm = [xa.bf16([128, 8, 128]) for _ in range(3)]
            wfmR = [Res() for _ in range(3)]
            qf = [xa.f32([128, 512]) for _ in range(2)]
            qfR = [Res(), Res()]
            qb = [wk.bf16([128, 512]) for _ in range(2)]
            qbR = [Res(), Res()]
            permb = wk.bf16([128, 128])
            fw.op(fw.dve, f_copy(permb[:, :], permf[:, :]), reads=[cR], writes=[cR])
            r1 = [xa.f32([128, 512]) for _ in range(2)]
            r1R = [Res(), Res()]
            r2 = [xa.f32([128, 512]) for _ in range(2)]
            r2R = [Res(), Res()]
            ropeR = Res()
            w1kb = MIXT[:, 4096:8192].bitcast(BF16).rearrange("p (l h) -> p l h", h=256)
            w1kR = Res()
            fw.dma(fw.sp, f_dma(posi[:, :], pos_d.broadcast_to([128, T])), writes=[ropeR])
            fw.op(fw.dve, f_copy(posf[:, :], posi[:, :]), reads=[ropeR], writes=[ropeR])
            fw.op(fw.dve, f_ts(yy[:, :], posf[:, :], invf[:, 0:1], None, ALU.mult), reads=[ropeR, cR], writes=[ropeR])

            def frac_sin(dst, src, add):
                if add != 0.0:
                    fw.op(fw.dve, f_ts(dst, src, add, None, ALU.add), reads=[ropeR], writes=[ropeR])
                    src = dst
                fw.op(fw.dve, f_copy(posi[:, :], src), reads=[ropeR], writes=[ropeR])
                fw.op(fw.dve, f_copy(posf[:, :], posi[:, :]), reads=[ropeR], writes=[ropeR])
                fw.op(fw.dve, f_tt(dst, src, posf[:, :], ALU.subtract), reads=[ropeR], writes=[ropeR])
                fw.op(fw.dve, f_stt(posf[:, :], dst, 0.5, dst, ALU.is_gt, ALU.subtract), reads=[ropeR], writes=[ropeR])
                fw.op(fw.dve, f_stt(dst, posf[:, :], 0.5, posf[:, :], ALU.is_gt, ALU.subtract), reads=[ropeR], writes=[ropeR])
                fw.op(fw.act, f_act(dst, dst, AF.Sin, scale=2 * PI), reads=[ropeR], writes=[ropeR])


            ndma = [0]

            p3lanes = [[], []]

            def fm_proj2(chunk, consume):
                ws = ndma[0] % 3
                ndma[0] += 1
                fw.rec_begin()
                fw.dma(fw.pool, f_dma(wfm[ws][:].rearrange("p a b -> p (a b)"), wfm_d[:, chunk, :]), writes=[wfmR[ws]])
                pre = fw.rec_end()
                p3lanes[0] += pre
                p3lanes[1] += [None] * len(pre)
                for tb in range(4):
                    lane_banks[0] = [4 * (tb % 2) + b_ for b_ in range(4)]
                    fw.rec_begin()
                    pb, pR = bank()
                    mmg(pb[:, :], pR, [(wfm[ws][:, c, :], HT[:, c, tb * 512:(tb + 1) * 512]) for c in range(8)],
                        HT_R[4 * tb:4 * tb + 4] + [wfmR[ws]])
                    consume(tb, pb, pR)
                    p3lanes[tb % 2] += fw.rec_end()
                lane_banks[0] = None

            rope_cnt = [0]

            def rope_consume(dst2d, dstR):
                def consume(tb, pb, pR):
                    s = tb % 2
                    sl = slice(tb * 512, (tb + 1) * 512)
                    fw.op(fw.act, f_act(qf[s][:, :], pb[:, :], AF.Copy), reads=[pR], writes=[qfR[s]])
                    fw.op(fw.act, f_act(qb[s][:, :], pb[:, :], AF.Copy), reads=[pR], writes=[qbR[s]])
                    pr, prR = bank()
                    fw.op(fw.pe, f_mm(pr[:, :], permb[:, :], qb[s][:, :]), reads=[qbR[s], cR], writes=[prR])
                    fw.op(fw.dve, f_tt(r1[s][:, :], qf[s][:, :], cosT[:, sl], ALU.mult), reads=[qfR[s], ropeR], writes=[r1R[s]])
                    fw.op(fw.dve, f_tt(r2[s][:, :], pr[:, :], sinT[:, sl], ALU.mult), reads=[prR, ropeR], writes=[r2R[s]])
                    if isinstance(dst2d, list):
                        for g_ in range(2):
                            rw = slice(g_ * 64, (g_ + 1) * 64)
                            fw.op(fw.dve, f_tt(dst2d[g_][rw, sl], r1[s][rw, :], r2[s][rw, :], ALU.add), reads=[r1R[s], r2R[s]], writes=[dstR])
                    else:
                        fw.op(fw.dve, f_tt(dst2d[:, sl], r1[s][:, :], r2[s][:, :], ALU.add), reads=[r1R[s], r2R[s]], writes=[dstR])
                return consume

            fw.op(fw.dve, f_memset(Vs[:, :, :, :].rearrange('p a b c -> p (a b c)'), 1.0), writes=[VsR])
            fw.op(fw.dve, f_memset(Vw[:, :, :, :].rearrange('p a b c -> p (a b c)'), 1.0), writes=[VwR])
            for i in range(NT):
                pb, pR = bank()
                mmg(pb[:, 0:280], pR, [(HT[:, c, i * 128:(i + 1) * 128], wtn[:, c, :]) for c in range(8)], [HT_R[i], wtnR])
                fw.op(fw.act, f_act(Vs[:, i, :, 0:64], pb[:, 0:128].rearrange("p (g d) -> p g d", d=64), AF.Copy), reads=[pR], writes=[VsR])
                fw.op(fw.act, f_act(Vw[:, i, :, 0:64], pb[:, 128:256].rearrange("p (g d) -> p g d", d=64), AF.Copy), reads=[pR], writes=[VwR])
                fw.op(fw.act, f_act(gates[:, i, :], pb[:, 256:280], AF.Sigmoid), reads=[pR], writes=[gtR])
            frac_sin(sinT[:, :], yy[:, :], 0.0)
            frac_sin(cosT[:, :], yy[:, :], 0.25)
            fm_proj2(13, lambda tb, pb, pR: fw.op(fw.act, f_act(vcT[:, tb * 512:(tb + 1) * 512], pb[:, :], AF.Copy), reads=[pR], writes=[vcR]))
            for c in range(4):
                fm_proj2(8 + c, rope_consume(qT[:, c, :], qTR))
            fm_proj2(12, rope_consume(kcT, kcR))
            fm_proj2(14, rope_consume(ksZ, kzR))
            fm_proj2(15, rope_consume(kwZ, kzR))
            p3lanes[1] = [None] * 8 + p3lanes[1]
            fw.replay_lanes(p3lanes)
            fw.dma(fw.pool, f_dma(w1kb.rearrange("p a b -> p (a b)").rearrange("p (s e) -> p s e", e=2048),
                                  w1k_d.rearrange("p (s e) -> p s e", e=2048)), writes=[w1kR, wtnR])
            chk('P3a3')
            fw.barrier()
            if stop == "P3a":
                raise _Stop()

            xa.reset()
            w1b = xa.bf16([128, 32, 256])
            w1R = Res()
            w2b = xa.bf16([128, 2, 64])
            w2R = Res()
            peb = xa.bf16([128, 32])
            pef = xa.f32([128, 32])
            peR = Res()
            hidT = xa.bf16([128, 2, 2, 128])
            hidR = Res()
            cvec = xa.f32([128, 2])
            cvR = Res()
            kcc = wk.bf16([128, 2, 128])
            kccR = Res()
            vcx = wk.bf16([128, 2, 98])
            vcxR = Res()
            fw.op(fw.pool, f_memset(vcx[:, :, :].rearrange('p a b -> p (a b)'), 1.0), writes=[vcxR])
            fw.op(fw.pool, f_memset(vcx[:, :, 0:64], 0.0), writes=[vcxR])
            ovl_f = xa.f32([128, 32])
            fw.dma(fw.sp, f_dma(ovl_f[:, :], c_ovl), writes=[vcxR])
            for g in range(2):
                fw.op(fw.dve, f_copy(vcx[:, g, 65:97], ovl_f[:, :]), reads=[vcxR], writes=[vcxR])
            fw.op(fw.pool, f_memset(kcc[:, :, :].rearrange("p a b -> p (a b)"), 0.0), writes=[kccR])

            w1v_buf = w1b
            fw.dma(fw.pool, f_dma(w1v_buf[:].rearrange("p a b -> p (a b)").rearrange("p (s e) -> p s e", e=2048),
                                  w1v_d.rearrange("p (s e) -> p s e", e=2048)), writes=[w1R])
            for kv in range(2):
                srcT, srcR = (kcT, kcR) if kv == 0 else (vcT, vcR)
                w1b = w1kb if kv == 0 else w1v_buf
                if kv == 0:
                    w1R_save = w1R
                    w1R = w1kR
                else:
                    w1R = w1R_save
                fw.dma(fw.pool, f_dma(w2b[:].rearrange("p a b -> p (a b)"), w2k_d if kv == 0 else w2v_d), writes=[w2R])
                fw.dma(fw.sp, f_dma(pef[:, :], pek_d if kv == 0 else pev_d), writes=[peR])
                fw.op(fw.dve, f_copy(peb[:, :], pef[:, :]), reads=[peR], writes=[peR])
                src3 = srcT[:, :].rearrange("p (n l) -> p n l", l=16)
                for hc in range(2):
                    pc, pcR = bank()
                    mmg(pc[:, 0:1], pcR, [(w1b[0:64, l, hc * 128:(hc + 1) * 128], peb[0:64, l:l + 1]) for l in range(32)], [w1R, peR])
                    fw.op(fw.dve, f_copy(cvec[:, hc:hc + 1], pc[:, 0:1]), reads=[pcR], writes=[cvR])
                    phs = [bank(), bank()]
                    for l in range(32):
                        for g in range(2):
                            ph, phR = phs[g]
                            rows = slice(g * 64, (g + 1) * 64)
                            rhs = src3[rows, 0:127, l] if l < 16 else src3[rows, 1:128, l - 16]
                            fw.op(fw.pe, f_mm(ph[:, 0:127], w1b[rows, l, hc * 128:(hc + 1) * 128], rhs, start=(l == 0), stop=(l == 31)),
                                  reads=[w1R, srcR], writes=[phR], signal=(l == 31))
                    for g in range(2):
                        ph, phR = phs[g]
                        fw.op(fw.act, f_act(hidT[:, g, hc, 0:127], ph[:, 0:127], AF.Silu, bias=cvec[:, hc:hc + 1]), reads=[phR, cvR], writes=[hidR])
                for g in range(2):
                    po, poR = bank()
                    if kv == 0:
                        mmg(po[g * 64:(g + 1) * 64, 0:127], poR, [(w2b[:, hc, :], hidT[:, g, hc, 0:127]) for hc in range(2)], [w2R, hidR])
                        fw.op(fw.act, f_act(kcc[g * 64:(g + 1) * 64, g, 0:127], po[g * 64:(g + 1) * 64, 0:127], AF.Copy), reads=[poR], writes=[kccR])
                    else:
                        mmg(po[0:127, 0:64], poR, [(hidT[:, g, hc, 0:127], w2b[:, hc, :]) for hc in range(2)], [w2R, hidR])
                        fw.op(fw.act, f_act(vcx[0:127, g, 0:64], po[0:127, 0:64], AF.Copy), reads=[poR], writes=[vcxR])
            fw.barrier()
            if stop == "P3b":
                raise _Stop()

            xa.reset()
            wob_pre = HT[:, 0:4, :].rearrange("p a b -> p (a b)")
            wobpR = Res()
            fw.dma(fw.pool, f_dma(wob_pre.rearrange("p (s e) -> p s e", e=2048), wo_d.rearrange("p (s e) -> p s e", e=2048)), writes=[wobpR])
            NE = 18
            ering = [[xa.bf16([128, 512]) for _ in range(NE)] for _ in range(2)]
            eR = [[Res() for _ in range(NE)] for _ in range(2)]
            e_i = [0, 0]

            def eslot(g):
                k = e_i[g]
                e_i[g] = (k + 1) % NE
                return ering[g][k], eR[g][k]
            psel = xa.f32([128, 2, 32])
            pselR = [Res(), Res()]
            sc = [xa.f32([128, 32]) for _ in range(2)]
            sc2 = [xa.f32([128, 32]) for _ in range(2)]
            m8a = [xa.f32([128, 8]) for _ in range(2)]
            m8b = [xa.f32([128, 8]) for _ in range(2)]
            selb = [xa.bf16([128, 32]) for _ in range(2)]
            selbT = [[xa.bf16([128, 4, 128]) for _ in range(2)] for _ in range(2)]
            selbTR = [[Res(), Res()] for _ in range(2)]
            for g_ in range(2):
                for p_ in range(2):
                    fw.op(fw.pool, f_memset(selbT[g_][p_][:, :, :].rearrange("p a b -> p (a b)"), 0.0), writes=[selbTR[g_][p_]])
            tkR = [Res(), Res()]
            acc = [xa.f32([128, 8, 64]) for _ in range(3)]
            accR = [[Res(), Res()] for _ in range(3)]
            coef = xa.f32([128, 3, 8])
            rs = xa.f32([128, 8])
            coefR = [Res(), Res()]
            nst = xa.f32([128, 16])
            nstR = Res()
            pos_ = [[xa.f32([128, 4, 98]) for _ in range(3)] for _ in range(2)]
            posR = [[Res() for _ in range(3)] for _ in range(2)]
            sqn = xa.f32([128, 8, 64])
            njR = Res()
            mxn = xa.bf16([128, 512])
            mxnR = Res()
            SCALE = 0.125

            def q4(g, i):
                return qT[:, :, i * 128:(i + 1) * 128]

            def f_recip(out, in_):
                return lambda e: e.reciprocal(out, in_)

            def f_max8(out, in_):
                return lambda e: e.max(out=out, in_=in_)

            def f_mrep(out, rep_, vals):
                return lambda e: e.match_replace(out=out, in_to_replace=rep_, in_values=vals, imm_value=-1e30)

            def finish_branch(b, g, i, po, poR, first):
                hs = slice(g * 4, g * 4 + 4)
                ab = acc[i % 3]
                aR = accR[i % 3][g]
                gi_ = gates[:, i, :]
                fw.op(fw.dve, f_ts(rs[:, hs], po[:, :, 64], 1e-30, None, ALU.max), reads=[poR], writes=[coefR[g]])
                fw.op(fw.dve, f_recip(rs[:, hs], rs[:, hs]), reads=[coefR[g]], writes=[coefR[g]])
                fw.op(fw.dve, f_tt(coef[:, b, hs], rs[:, hs], gi_[:, b * 8 + g * 4:b * 8 + g * 4 + 4], ALU.mult), reads=[coefR[g], gtR], writes=[coefR[g]])
                for hp in range(4):
                    h = g * 4 + hp
                    if first:
                        fw.op(fw.dve, f_ts(ab[:, h, :], po[:, hp, 0:64], coef[:, b, h:h + 1], None, ALU.mult), reads=[poR, coefR[g]], writes=[aR])
                    else:
                        fw.op(fw.dve, f_stt(ab[:, h, :], po[:, hp, 0:64], coef[:, b, h:h + 1], ab[:, h, :], ALU.mult, ALU.add),
                              reads=[poR, coefR[g], aR], writes=[aR])

            def cmp_part(i, g):
                ps_, psR = bank()
                ps3 = ps_[:, :].rearrange("p (h q) -> p h q", q=128)
                fw.op(fw.pe, f_mm(ps3, kcc[:, g, :], q4(g, i), start=True, stop=False), reads=[kccR, qTR], writes=[psR], signal=False)
                fw.op(fw.pe, f_mm(ps3, identb[:, :], cmpm[:, i * 128:(i + 1) * 128].unsqueeze(1).broadcast_to([128, 4, 128]), start=False, stop=True),
                      reads=[cR], writes=[psR])
                ec, ecR = eslot(g)
                fw.op(fw.act, f_act(ec[:, :], ps_[:, :], AF.Exp, scale=SCALE), reads=[psR], writes=[ecR])
                po, poR = bank()
                po3 = po[:, :].rearrange("p (h w) -> p h w", w=128)
                for hp in range(4):
                    fw.op(fw.pe, f_mm(po3[:, hp, 0:97], ec[:, hp * 128:(hp + 1) * 128], vcx[:, g, 0:97]), reads=[ecR, vcxR], writes=[poR])
                pst = pos_[g][0]
                fw.op(fw.dve, f_copy(pst[:, :, 0:97], po3[:, :, 0:97]), reads=[poR], writes=[posR[g][0]])
                po3 = pst
                poR = posR[g][0]
                finish_branch(0, g, i, po3, poR, True)
                if i >= 8:
                    for hp in range(4):
                        h = g * 4 + hp
                        if hp == 0:
                            fw.op(fw.dve, f_ts(psel[:, g, :], po3[:, hp, 65:97], rs[:, h:h + 1], None, ALU.mult), reads=[poR, coefR[g]], writes=[pselR[g]])
                        else:
                            fw.op(fw.dve, f_stt(psel[:, g, :], po3[:, hp, 65:97], rs[:, h:h + 1], psel[:, g, :], ALU.mult, ALU.add),
                                  reads=[poR, coefR[g], pselR[g]], writes=[pselR[g]])
                    fw.op(fw.dve, f_tt(sc[g][:, :], psel[:, g, :], keep_t[:, i - 8, :], ALU.mult), reads=[pselR[g], cR], writes=[tkR[g]])
                    fw.op(fw.dve, f_tt(sc[g][:, :], sc[g][:, :], add_t[:, i - 8, :], ALU.add), reads=[tkR[g], cR], writes=[tkR[g]])
                    fw.op(fw.dve, f_max8(m8a[g][:, :], sc[g][:, :]), reads=[tkR[g]], writes=[tkR[g]])
                    fw.op(fw.dve, f_mrep(sc2[g][:, :], m8a[g][:, :], sc[g][:, :]), reads=[tkR[g]], writes=[tkR[g]])
                    fw.op(fw.dve, f_max8(m8b[g][:, :], sc2[g][:, :]), reads=[tkR[g]], writes=[tkR[g]])
                    fw.op(fw.dve, f_ts(sc2[g][:, :], sc[g][:, :], m8b[g][:, 7:8], None, ALU.is_ge), reads=[tkR[g]], writes=[tkR[g]])
                    fw.op(fw.dve, f_ts(selb[g][:, :], sc2[g][:, :], -1.0, 30000.0, ALU.add, ALU.mult), reads=[tkR[g]], writes=[tkR[g]])
                    pt, ptR = bank()
                    ptb = pt[:, :].bitcast(BF16)
                    fw.op(fw.pe, f_tr(ptb[0:32, 0:128], selb[g][:, :], identb[:]), reads=[tkR[g], cR], writes=[ptR])
                    fw.op(fw.act, f_act(selbT[g][i % 2][0:32, :, :], ptb[0:32, 0:128].unsqueeze(1).broadcast_to([32, 4, 128]), AF.Copy),
                          reads=[ptR], writes=[selbTR[g][i % 2]])
            def make_branch(i, g):
                def branch(bidx, js, Kt, KR, Vt, VR, sel):
                    po, poR = bank()
                    po3 = po[:, :].rearrange("p (h w) -> p h w", w=128)
                    n = len(js)
                    ets = []

                    def pv(t):
                        ej, ejR, j = ets[t]
                        for hp in range(4):
                            last = (t == n - 1 and hp == 3)
                            fw.op(fw.pe, lambda e, o=po3[:, hp, 0:65], l=ej[:, hp * 128:(hp + 1) * 128], r=Vt[:, j, g, 0:65], st=(t == 0 and hp == 0), sp=last:
                                  e.matmul(o, lhsT=l, rhs=r, start=st, stop=sp, skip_group_check=True),
                                  reads=[ejR, VR], writes=[poR], signal=(hp == 3))
                    for t, j in enumerate(js):
                        while True:
                            ps_, psR = bank()
                            if ps_ is not po:
                                break
                        ps3 = ps_[:, :].rearrange("p (h q) -> p h q", q=128)
                        if sel:
                            fw.op(fw.pe, f_mm(ps3, Kt[g][:, j * 128:(j + 1) * 128], q4(g, i), start=True, stop=False),
                                  reads=[KR, qTR], writes=[psR], signal=False)
                            fw.op(fw.pe, f_mm(ps3, eblk[:, j, :], selbT[g][i % 2][:, :, :], start=False, stop=True), reads=[cR, selbTR[g][i % 2]], writes=[psR])
                        else:
                            fw.op(fw.pe, f_mm(ps3, Kt[g][:, j * 128:(j + 1) * 128], q4(g, i)), reads=[KR, qTR], writes=[psR])
                        ej, ejR = eslot(g)
                        fw.op(fw.act, f_act(ej[:, :], ps_[:, :], AF.Exp, scale=SCALE), reads=[psR], writes=[ejR])
                        if j == i:
                            fw.op(fw.dve, f_tt(ej[:, :], ej[:, :], tri0[:, :], ALU.mult), reads=[ejR, cR], writes=[ejR])
                        elif bidx == 2 and j == i - 4:
                            fw.op(fw.dve, f_tt(ej[:, :], ej[:, :], tri4[:, :], ALU.mult), reads=[ejR, cR], writes=[ejR])
                        ets.append((ej, ejR, j))
                        if t >= 2:
                            pv(t - 2)
                    for t in range(max(0, n - 2), n):
                        pv(t)
                    pst = pos_[g][bidx]
                    fw.op(fw.dve, f_copy(pst[:, :, 0:65], po3[:, :, 0:65]), reads=[poR], writes=[posR[g][bidx]])
                    finish_branch(bidx, g, i, pst, posR[g][bidx], False)

                return branch

            def win_part(i, g):
                make_branch(i, g)(2, list(range(max(0, i - 4), i + 1)), kwZ, kzR, Vw, VwR, False)

            def slc_part(i, g):
                make_branch(i, g)(1, list(range(i + 1)), ksZ, kzR, Vs, VsR, i >= 8)

            def combine(i):
                ab = acc[i % 3]
                aRs = accR[i % 3]
                fw.op(fw.dve, f_tt(sqn[:, :, :], ab[:, :, :], ab[:, :, :], ALU.mult), reads=aRs, writes=[njR])
                fw.op(fw.dve, lambda e: e.tensor_reduce(out=nst[:, 0:8], in_=sqn[:, :, :], axis=mybir.AxisListType.X, op=ALU.add), reads=[njR], writes=[nstR])
                fw.op(fw.act, f_act(nst[:, 8:16], nst[:, 0:8], AF.Ln, scale=1.0 / 64, bias=epsT[:, 0:1]), reads=[nstR, cR], writes=[nstR])
                fw.op(fw.act, f_act(nst[:, 8:16], nst[:, 8:16], AF.Exp, scale=-0.5), reads=[nstR], writes=[nstR])
                fw.op(fw.dve, f_tt(ab[:, :, :], ab[:, :, :], nst[:, 8:16].unsqueeze(2).broadcast_to([128, 8, 64]), ALU.mult),
                      reads=aRs + [nstR], writes=aRs)
                fw.op(fw.dve, f_tt(mxn[:, :], ab[:, :, :].rearrange("p h d -> p (h d)"), ngain[:, :], ALU.mult), reads=aRs + [cR], writes=[mxnR])
                pt, ptR = bank()
                ptb = pt[:, :].bitcast(BF16)
                for c in range(4):
                    fw.op(fw.pe, f_tr(ptb[:, c * 128:(c + 1) * 128], mxn[:, c * 128:(c + 1) * 128], identb[:]), reads=[mxnR, cR], writes=[ptR],
                          signal=(c == 3))
                fw.op(fw.act, f_act(MIXTb[:, 4:8, i * 128:(i + 1) * 128], ptb[:, 0:512].rearrange("p (c t) -> p c t", t=128), AF.Copy),
                      reads=[ptR], writes=[MX_R[i]])

            lanes = [[], [], []]
            lbanks = [[0, 1, 2, 3], [4, 5, 6]]

            def rec_part(fn, i, g):
                lane_banks[0] = lbanks[g]
                fw.rec_begin()
                fn(i, g)
                return fw.rec_end()
            for g in range(2):
                lanes[g] += rec_part(cmp_part, 0, g)
            lanes[2] += [None] * len(lanes[0])
            prev_cb = []
            for i in range(NT):
                n0 = len(lanes[0])
                for g in range(2):
                    lanes[g] += rec_part(win_part, i, g)
                    tail = []
                    if i + 1 < NT:
                        c_ = rec_part(cmp_part, i + 1, g)
                        if i + 1 >= 8:
                            tail = c_[-2:]
                            c_ = c_[:-2]
                        lanes[g] += c_
                    lanes[g] += rec_part(slc_part, i, g) + tail
                assert len(lanes[0]) == len(lanes[1])
                ntile = len(lanes[0]) - n0
                sp = []
                for e_ in prev_cb:
                    sp += [e_, None, None, None]
                assert len(sp) <= ntile, (len(sp), ntile)
                lanes[2] += sp + [None] * (ntile - len(sp))
                lane_banks[0] = [7]
                fw.rec_begin()
                combine(i)
                prev_cb = fw.rec_end()
            lanes[2] += prev_cb
            lane_banks[0] = None
            fw.replay_lanes(lanes)
            fw.barrier()
            if stop == "P3c":
                raise _Stop()

            if debug:
                wk.reset()
                dtmp = wk.f32([128, 2048])
                dR = Res()
                for c in range(8):
                    fw.op(fw.dve, f_copy(dtmp[:, :], MIXTb[:, c, :]), reads=[dR], writes=[dR])
                    fw.dma(fw.sp, f_dma(dbg["mix"][:, c * T:(c + 1) * T], dtmp[:, :]), reads=[dR], writes=[])
                    fw.barrier()

            wk.reset()
            wob = wk.bf16([128, 8, 1024])
            woR = Res()
            wob2 = wob[:].rearrange("p a b -> p (a b)")
            ev_a = fw.op(fw.act, f_act(wob2[:, 0:4096], wob_pre[:, 0:4096], AF.Copy), reads=[wobpR], writes=[woR])
            ev_b = fw.op(fw.dve, f_copy(wob2[:, 4096:8192], wob_pre[:, 4096:8192]), reads=[wobpR], writes=[woR])
            wka = dict(st=[stat[:, 2 * q_:2 * q_ + 2] for q_ in range(4)], stR=[Res() for _ in range(4)], junk=wk.bf16([128, D]), junkR=Res(),
                       xb=[wk.bf16([128, D]) for _ in range(4)], xbR=[Res() for _ in range(4)])
            HT_R = [Res() for _ in range(NT)]
            for r_ in HT_R:
                r_.r = [ev_a, ev_b]
            for i in range(NT):
                fw.dma(fw.sp, f_dma(X1[:, i * D:(i + 1) * D], x_tiles[i]), writes=[X1_R[i]])
            lanes = [[], [], [], []]
            p4_tails = [[], [], [], []]
            for i in range(NT):
                xi = X1[:, i * D:(i + 1) * D]
                lane_banks[0] = [2 * (i % 4), 2 * (i % 4) + 1]
                fw.rec_begin()
                for hf in range(2):
                    pb, pR = bank()
                    mmg(pb[:, :], pR, [(MIXTb[:, c, i * 128:(i + 1) * 128], wob[:, c, hf * 512:(hf + 1) * 512]) for c in range(8)], [MX_R[i], woR])
                    fw.op(fw.dve, f_tt(xi[:, hf * 512:(hf + 1) * 512], xi[:, hf * 512:(hf + 1) * 512], pb[:, :], ALU.add), reads=[pR, X1_R[i]], writes=[X1_R[i]])
                norm_to_HT(xi, X1_R[i], i, g2, wka)
                it_ = fw.rec_end()
                body_, tail_ = it_[:-9], it_[-9:]
                lanes[i % 4] += body_[:9] + p4_tails[i % 4] + body_[9:]
                p4_tails[i % 4] = tail_
            for q_ in range(4):
                lanes[q_] += p4_tails[q_]
            lane_banks[0] = None
            fw.replay_lanes(lanes, stagger=8)
            fw.barrier()
            if stop == "P4":
                raise _Stop()
            if debug:
                for i in range(NT):
                    fw.dma(fw.sp, f_dma(dbg["x1"][i * 128:(i + 1) * 128, :], X1[:, i * D:(i + 1) * D]), reads=[X1_R[i]], writes=[])
                fw.barrier()

            wk.reset()
            ma.reset()
            actb = [ma.bf16([128, FG, T]) for _ in range(2)]
            actR = [[Res() for _ in range(FG)] for _ in range(2)]
            NGU, NWD = 3, 12
            wgu = [wk.bf16([128, 2, 8, 128]) for _ in range(NGU)]
            wguR = [Res() for _ in range(NGU)]
            wdn = [wk.bf16([128, 1024]) for _ in range(NWD)]
            wdnR = [Res() for _ in range(NWD)]
            gsb = [wk.f32([128, 514]) for _ in range(2)]
            gsbR = [Res(), Res()]
            c0 = [wk.f32([128, 512]) for _ in range(2)]
            c0R = [Res(), Res()]
            c1 = [wk.f32([128, 512]) for _ in range(2)]
            c1R = [Res(), Res()]
            sl_ = [wk.f32([128, 512]) for _ in range(2)]
            slR = [Res(), Res()]
            groups = [list(range(s, min(s + FG, NF))) for s in range(0, NF, FG)]
            blk = [0]

            def ffn_down(gi):
                fl = groups[gi]
                ab = actb[gi % 2]
                for i in range(NT):
                    for hf in range(2):
                        pb, pR = bank()
                        mmg(pb[:, :], pR, [(ab[:, k, i * 128:(i + 1) * 128], wdn[f % NWD][:, hf * 512:(hf + 1) * 512]) for k, f in enumerate(fl)],
                            [actR[gi % 2][k] for k in range(len(fl))] + [wdnR[f % NWD] for f in fl])
                        xi = X1[:, i * D + hf * 512:i * D + (hf + 1) * 512]
                        fw.op(fw.dve, f_tt(xi, xi, pb[:, :], ALU.add), reads=[pR, X1_R[i]], writes=[X1_R[i]])

            def ffn_load(f):
                if f >= NF:
                    return
                ws = f % NGU
                fw.dma(fw.pool, f_dma(wgu[ws][:, 0, :, :].rearrange("p a b -> p (a b)"), wg_d[:, f, :]), writes=[wguR[ws]])
                fw.dma(fw.pool, f_dma(wgu[ws][:, 1, :, :].rearrange("p a b -> p (a b)"), wu_d[:, f, :]), writes=[wguR[ws]])
                fw.dma(fw.pool, f_dma(wdn[f % NWD][:, :], wd_d[:, f, :]), writes=[wdnR[f % NWD]])

            pending = []

            def flush_tail():
                while pending:
                    s_, ab_, k_, sl_c, pu_, puR_ = pending.pop(0)
                    fw.op(fw.act, f_act(sl_[s_][:, :], c0[s_][:, :], AF.Silu), reads=[c0R[s_]], writes=[slR[s_]])
                    fw.op(fw.dve, f_tt(actb[ab_][:, k_, sl_c], sl_[s_][:, :], pu_[:, :], ALU.mult), reads=[slR[s_], puR_], writes=[actR[ab_][k_]])

            PF = 2
            for f in range(PF):
                ffn_load(f)
            for gi, fl in enumerate(groups):
                for k, f in enumerate(fl):
                    ws = f % NGU
                    ffn_load(f + PF)
                    for tb in range(4):
                        s = blk[0] % 2
                        blk[0] += 1
                        sl = slice(tb * 512, (tb + 1) * 512)
                        pg, pgR = bank()
                        mmg(pg[:, :], pgR, [(wgu[ws][:, 0, c, :], HT[:, c, sl]) for c in range(8)], HT_R[4 * tb:4 * tb + 4] + [wguR[ws]])
                        pu, puR = bank()
                        mmg(pu[:, :], puR, [(wgu[ws][:, 1, c, :], HT[:, c, sl]) for c in range(8)], HT_R[4 * tb:4 * tb + 4] + [wguR[ws]])
                        if tb == 0:
                            fw.op(fw.dve, f_memset(gsb[s][:, 0:2], 0.0), writes=[gsbR[s]])
                        else:
                            fw.op(fw.act, f_act(gsb[s][:, 0:2], gsb[1 - s][:, 512:514], AF.Copy), reads=[gsbR[1 - s]], writes=[gsbR[s]])
                        fw.op(fw.act, f_act(gsb[s][:, 2:514], pg[:, :], AF.Copy), reads=[pgR], writes=[gsbR[s]])
                        fw.op(fw.act, f_act(c0[s][:, :], pg[:, :], AF.Identity, scale=convw[:, f, 2:3], bias=convb[:, f:f + 1]), reads=[pgR, cR], writes=[c0R[s]])
                        fw.op(fw.dve, f_stt(c1[s][:, :], gsb[s][:, 1:513], convw[:, f, 1:2], c0[s][:, :], ALU.mult, ALU.add),
                              reads=[gsbR[s], c0R[s], cR], writes=[c1R[s]])
                        fw.op(fw.dve, f_stt(c0[s][:, :], gsb[s][:, 0:512], convw[:, f, 0:1], c1[s][:, :], ALU.mult, ALU.add),
                              reads=[gsbR[s], c1R[s], cR], writes=[c0R[s]])
                        flush_tail()
                        pending.append((s, gi % 2, k, sl, pu, puR))
                flush_tail()
                if gi >= 1:
                    ffn_down(gi - 1)
            ffn_down(len(groups) - 1)

            outR = Res()
            out_tiles = out_d.rearrange("(n p) d -> n p d", p=128)
            fjunk = wk.bf16([128, D]) if False else c0[0]
            lanes = [[], [], [], []]
            p6R = [Res() for _ in range(4)]
            for i in range(NT):
                xi = X1[:, i * D:(i + 1) * D]
                s = i % 2
                st = stat[:, 8 + 2 * (i % 4):10 + 2 * (i % 4)]
                stR = p6R[i % 4]
                fw.rec_begin()
                fw.op(fw.act, f_act(sl_[s][:, :].bitcast(BF16), xi, AF.Square, accum_out=st[:, 0:1]), reads=[X1_R[i]], writes=[slR[s], stR])
                rstd_from_ss(st[:, 0:1], D, st[:, 1:2], stR)
                fw.op(fw.dve, f_stt(xi, xi, st[:, 1:2], fgain[:, :], ALU.mult, ALU.mult), reads=[X1_R[i], stR, cR], writes=[X1_R[i]])
                fw.dma(fw.sp, f_dma(out_tiles[i], xi), reads=[X1_R[i]], writes=[])
                lanes[i % 4] += fw.rec_end()
            fw.replay_lanes(lanes, stagger=1)
            fw.barrier()
            if stop == "P6":
                raise _Stop()


        except _Stop:
            fw.barrier()
        run = fw.runner(sems)
        with nc.Block() as block:
            @block.tensor
            def _(e):
                run(fw.pe, e)

            @block.scalar
            def _(e):
                run(fw.act, e)

            @block.vector
            def _(e):
                run(fw.dve, e)

            @block.gpsimd
            def _(e):
                run(fw.pool, e)

            @block.sync
            def _(e):
                run(fw.sp, e)
    return nc


def _consts():
    c = {}
    c["c_ident"] = np.eye(128, dtype=np.float32)
    P = np.zeros((128, 128), np.float32)
    for po in range(128):
        j = po % 64
        if j < 8:
            P[po + 8, po] = -1.0
        elif j < 16:
            P[po - 8, po] = 1.0
    c["c_perm"] = P
    invf = np.zeros((128, 1), np.float32)
    half = 8
    inv_freq = (500000.0 ** (-np.arange(half, dtype=np.float32) * 2.0 / 16)).astype(np.float32)
    for p in range(128):
        j = p % 64
        if j < 16:
            invf[p, 0] = inv_freq[j % 8] / (2 * np.pi)
    c["c_invf"] = invf
    pk = np.arange(128)[:, None]
    pq = np.arange(128)[None, :]
    tri0 = (pq >= pk).astype(np.float32)
    c["c_tri0"] = np.tile(tri0, (1, 4))
    c["c_tri4"] = np.tile(1.0 - tri0, (1, 4))
    eb = np.zeros((128, 16, 128), np.float32)
    for j in range(16):
        for p in range(128):
            eb[2 * j + p // 64, j, p] = 1.0
    c["c_eblk"] = eb.reshape(128, 16 * 128)
    n = np.arange(128)[:, None]
    t = np.arange(T)[None, :]
    cm = ((16 * n + 31) <= t).astype(np.float32)
    cm[127, :] = 0
    c["c_cmpm"] = (cm - 1.0) * 30000.0
    keep = np.zeros((128, 8, 32), np.float32)
    add = np.zeros((128, 8, 32), np.float32)
    for i in range(8, 16):
        for p in range(128):
            cur = 2 * i + (1 if p >= 64 else 0)
            for s in range(32):
                valid = s <= cur
                forced = (s == 0) or (s == cur) or (s == cur - 1)
                if not valid:
                    add[p, i - 8, s] = -1.0
                elif forced:
                    add[p, i - 8, s] = 1e4
                else:
                    keep[p, i - 8, s] = 1.0
    c["c_keep"] = keep.reshape(128, 256)
    c["c_add"] = add.reshape(128, 256)
    ovl = np.zeros((128, 32), np.float32)
    for nn in range(127):
        for s in range(32):
            if (16 * nn <= 64 * s + 63) and (16 * nn + 31 >= 64 * s):
                ovl[nn, s] = 1.0
    c["c_ovl"] = ovl
    return c


def _prep_shared(inp):
    f = lambda a: np.ascontiguousarray(a, dtype=np.float32)
    w_in = np.asarray(inp["w_in"][0])
    w3 = w_in.reshape(8, 128, -1)

    def fm_chunk(cols):
        return w3[:, :, cols].transpose(1, 0, 2)
    chunks = []
    for h in range(4):
        chunks.append(fm_chunk(np.arange(h * 128, (h + 1) * 128)))
    for h in range(4):
        chunks.append(fm_chunk(np.arange(512 + h * 128, 512 + (h + 1) * 128)))
    for c in range(4):
        cols = np.concatenate([2048 + c * 64 + np.arange(64), 2048 + (4 + c) * 64 + np.arange(64)])
        chunks.append(fm_chunk(cols))
    for base in (2560, 2688, 2816, 3072):
        chunks.append(fm_chunk(np.arange(base, base + 128)))
    sh = {}
    sh["w_fm"] = f(np.stack(chunks, axis=1).reshape(128, 16, 1024))
    sh["w_tm_i"] = f(w3[:, :, 1024:1536].transpose(1, 0, 2).reshape(128, -1))
    sh["w_tm_g"] = f(w3[:, :, 1536:2048].transpose(1, 0, 2).reshape(128, -1))
    ncols = np.concatenate([np.arange(2944, 3072), np.arange(3200, 3328), np.arange(3328, 3352)])
    sh["w_tm_n"] = f(w3[:, :, ncols].transpose(1, 0, 2).reshape(128, -1))
    sh["w_o"] = f(np.asarray(inp["w_o"][0]).reshape(8, 128, 1024).transpose(1, 0, 2).reshape(128, -1))
    for nm, key in (("w_gate", "ffn_w_gate"), ("w_up", "ffn_w_up")):
        w = np.asarray(inp[key][0]).reshape(8, 128, NF, 128)
        sh[nm] = f(w.transpose(1, 2, 0, 3).reshape(128, NF, 1024))
    sh["w_down"] = f(np.asarray(inp["ffn_w_down"][0]).reshape(NF, 128, 1024).transpose(1, 0, 2))
    for nm, key in (("w1k", "cmp_k_w1"), ("w1v", "cmp_v_w1")):
        w = np.asarray(inp[key][0]).reshape(32, 64, 256).transpose(1, 0, 2)
        sh[nm] = f(np.concatenate([w, w], axis=0).reshape(128, -1))
    for nm, key in (("w2k", "cmp_k_w2"), ("w2v", "cmp_v_w2")):
        sh[nm] = f(np.asarray(inp[key][0]).reshape(2, 128, 64).transpose(1, 0, 2).reshape(128, -1))
    for nm, key in (("pek", "cmp_pe_k"), ("pev", "cmp_pe_v")):
        pe = np.asarray(inp[key][0]).T
        sh[nm] = f(np.concatenate([pe, pe], axis=0))
    sh["g1"] = f(np.asarray(inp["ln1_gain"][0]).reshape(8, 128).T)
    sh["g2"] = f(np.asarray(inp["ln2_gain"][0]).reshape(8, 128).T)
    lbp = np.asarray(inp["hgrn_lb_param"])
    sh["lbp"] = f(lbp.reshape(2, 4, 128).transpose(2, 0, 1).reshape(128, 8))
    sh["hgain"] = f(np.asarray(inp["hgrn_out_gain"][0]).reshape(1, 512))
    sh["ngain"] = f(np.asarray(inp["nsa_out_gain"][0]).reshape(1, 512))
    sh["fgain"] = f(np.asarray(inp["final_gain"]).reshape(1, 1024))
    cw = np.asarray(inp["ffn_conv_w"][0])
    sh["convw"] = f(cw.reshape(3, NF, 128).transpose(2, 1, 0).reshape(128, NF * 3))
    sh["convb"] = f(np.asarray(inp["ffn_conv_b"][0]).reshape(NF, 128).T)
    sh.update(_consts())
    return sh


_NC_CACHE = {}


def kernel(**inputs):
    debug = bool(inputs.pop("_debug", False))
    cores = inputs.pop("_cores", None)
    sh = _prep_shared(inputs)
    x = np.asarray(inputs["x"], dtype=np.float32)
    pos = np.asarray(inputs["positions"]).astype(np.int32)
    core_ids = list(range(NCORES)) if cores is None else list(cores)
    if debug not in _NC_CACHE:
        _NC_CACHE[debug] = build_nc(debug)
    nc = _NC_CACHE[debug]
    in_maps = []
    for b in core_ids:
        m = dict(sh)
        m["x"] = np.ascontiguousarray(x[b])
        m["pos"] = np.ascontiguousarray(pos[b:b + 1])
        in_maps.append(m)
    res = run_bass_kernel_spmd(nc, in_maps, core_ids=core_ids)
    if debug:
        return res
    out = np.stack([np.asarray(r["out"], dtype=np.float32) for r in res.results], axis=0)
    return out
```

```python
import numpy as np
from contextlib import ExitStack
import concourse.bass as bass
import concourse.mybir as mybir
from concourse.bass_utils import run_bass_kernel_spmd

F32 = mybir.dt.float32
BF16 = mybir.dt.bfloat16
I32 = mybir.dt.int32
AF = mybir.ActivationFunctionType
ALU = mybir.AluOpType

T = 2048
D = 1024
NT = 16
DFF = 2816
NF = 22
EPS = 1e-6
PI = float(np.pi)
NCORES = 8
FG = 4


class Res:
    __slots__ = ("w", "r")

    def __init__(self):
        self.w = None
        self.r = []


class Eng:
    def __init__(self, name):
        self.name = name
        self.q = []
        self.count = 0
        self.waited = {}
        self.pending = False


class FW:
    def __init__(self, n_dma_sems=12):
        self.pe = Eng("tensor")
        self.act = Eng("scalar")
        self.dve = Eng("vector")
        self.pool = Eng("gpsimd")
        self.sp = Eng("sync")
        self.engs = [self.pe, self.act, self.dve, self.pool, self.sp]
        self.n_dma_sems = n_dma_sems
        self.dma_state = {q: dict(next=0, issued=[0] * n_dma_sems) for q in ("sync", "gpsimd")}
        self._rec = None

    def rec_begin(self):
        self._rec = []

    def rec_end(self):
        r = self._rec
        self._rec = None
        return r

    def replay_lanes(self, lanes, stagger=0):
        if stagger:
            lanes = [[None] * (k_ * stagger) + list(l) for k_, l in enumerate(lanes)]
        idx = [0] * len(lanes)
        live = True
        while live:
            live = False
            for k, lane in enumerate(lanes):
                if idx[k] < len(lane):
                    ent = lane[idx[k]]
                    idx[k] += 1
                    live = True
                    if ent is None:
                        continue
                    if ent[0] == "op":
                        self.op(ent[1], ent[2], ent[3], ent[4], ent[5])
                    else:
                        self.dma(ent[1], ent[2], ent[3], ent[4])

    def _wait(self, eng, ev):
        if ev is None:
            return
        key, val = ev
        if eng is self.pe and key == "tensor":
            return
        if eng.waited.get(key, 0) >= val:
            return
        eng.waited[key] = val
        eng.q.append(("wait", key, val))

    def _deps(self, eng, reads, writes):
        for r in reads:
            self._wait(eng, r.w)
        for w in writes:
            self._wait(eng, w.w)
            for ev in w.r:
                self._wait(eng, ev)

    def op(self, eng, fn, reads=(), writes=(), signal=True):
        if self._rec is not None:
            self._rec.append(("op", eng, fn, list(reads), list(writes), signal))
            return None
        self._deps(eng, reads, writes)
        if signal:
            eng.count += 1
            ev = (eng.name, eng.count)
            eng.pending = False
        else:
            ev = (eng.name, eng.count + 1)
            eng.pending = True
        eng.q.append(("op", fn, signal))
        for r in reads:
            r.r.append(ev)
        for w in writes:
            w.w = ev
            w.r = []
        return ev

    def dma(self, qeng, fn, reads=(), writes=()):
        if self._rec is not None:
            self._rec.append(("dma", qeng, fn, list(reads), list(writes)))
            return None
        st = self.dma_state[qeng.name]
        k = st["next"]
        st["next"] = (k + 1) % self.n_dma_sems
        key = "dma_%s_%d" % (qeng.name, k)
        if st["issued"][k] > 0:
            self._wait(qeng, (key, 16 * st["issued"][k]))
        self._deps(qeng, reads, writes)
        st["issued"][k] += 1
        ev = (key, 16 * st["issued"][k])
        qeng.q.append(("dma", fn, key))
        for r in reads:
            r.r.append(ev)
        for w in writes:
            w.w = ev
            w.r = []
        return ev

    def barrier(self):
        evs = []
        for e in self.engs:
            assert not e.pending
            if e.count > 0:
                evs.append((e.name, e.count))
        for q, st in self.dma_state.items():
            for k in range(self.n_dma_sems):
                if st["issued"][k] > 0:
                    evs.append(("dma_%s_%d" % (q, k), 16 * st["issued"][k]))
        for e in self.engs:
            for ev in evs:
                self._wait(e, ev)

    def sem_keys(self):
        keys = [e.name for e in self.engs]
        for q in self.dma_state:
            for k in range(self.n_dma_sems):
                keys.append("dma_%s_%d" % (q, k))
        return keys

    def runner(self, sems):
        def run(eng, h):
            own = sems[eng.name]
            pend = []
            for item in eng.q:
                if item[0] == "wait":
                    pend.append((sems[item[1]], item[2]))
                    continue
                for (s, v) in pend:
                    h.wait_ge(s, v)
                pend = []
                ins = item[1](h)
                if item[0] == "op":
                    if item[2]:
                        ins.then_inc(own, 1)
                else:
                    ins.then_inc(sems[item[2]], 16)
            for (s, v) in pend:
                h.wait_ge(s, v)
        return run


def f_mm(out, lhsT, rhs, start=True, stop=True):
    return lambda e: e.matmul(out, lhsT=lhsT, rhs=rhs, start=start, stop=stop)


def f_tr(out, in_, ident):
    return lambda e: e.transpose(out=out, in_=in_, identity=ident)


def f_act(out, in_, func, **kw):
    return lambda e: e.activation(out=out, in_=in_, func=func, **kw)


def f_tt(out, in0, in1, op):
    return lambda e: e.tensor_tensor(out=out, in0=in0, in1=in1, op=op)


def f_ts(out, in0, s1, s2, op0, op1=None):
    if op1 is None:
        return lambda e: e.tensor_scalar(out=out, in0=in0, scalar1=s1, scalar2=None, op0=op0)
    return lambda e: e.tensor_scalar(out=out, in0=in0, scalar1=s1, scalar2=s2, op0=op0, op1=op1)


def f_stt(out, in0, scalar, in1, op0, op1):
    return lambda e: e.scalar_tensor_tensor(out=out, in0=in0, scalar=scalar, in1=in1, op0=op0, op1=op1)


def f_copy(out, in_):
    return lambda e: e.tensor_copy(out=out, in_=in_)


def f_dma(out, in_):
    return lambda e: e.dma_start(out=out, in_=in_)


def f_memset(ap, v):
    return lambda e: e.memset(ap, v)


class Arena:
    def __init__(self, ap, n):
        self.ap = ap
        self.n = n
        self.off = 0

    def reset(self):
        self.off = 0

    def _take(self, nf):
        assert self.off + nf <= self.n, "arena overflow %d + %d > %d" % (self.off, nf, self.n)
        a = self.ap[:, self.off:self.off + nf]
        self.off += nf
        return a

    @staticmethod
    def _shape(a, shape):
        if shape[0] != 128:
            a = a[0:shape[0], :]
        if len(shape) == 2:
            return a
        if len(shape) == 3:
            return a.rearrange("p (a b) -> p a b", b=shape[2])
        return a.rearrange("p (a b c) -> p a b c", b=shape[2], c=shape[3])

    def f32(self, shape):
        n = int(np.prod(shape[1:]))
        return self._shape(self._take(n), shape)

    def i32(self, shape):
        n = int(np.prod(shape[1:]))
        return self._shape(self._take(n).bitcast(I32), shape)

    def bf16(self, shape):
        n = int(np.prod(shape[1:]))
        nf = (n + 1) // 2
        a = self._take(nf).bitcast(BF16)
        if 2 * nf != n:
            a = a[:, 0:n]
        return self._shape(a, shape)


class _Stop(Exception):
    pass


def build_nc(debug=False, stop=None):
    nc = bass.Bass("TRN2", target_bir_lowering=False)
    fw = FW()

    def din(name, shape, dt=F32):
        return nc.dram_tensor(name, list(shape), dt, kind="ExternalInput").ap()

    x_d = din("x", [T, D])
    pos_d = din("pos", [1, T], I32)
    wfm_d = din("w_fm", [128, 16, 1024])
    wti_d = din("w_tm_i", [128, 8 * 512])
    wtg_d = din("w_tm_g", [128, 8 * 512])
    wtn_d = din("w_tm_n", [128, 8 * 280])
    wo_d = din("w_o", [128, 8 * 1024])
    wg_d = din("w_gate", [128, NF, 1024])
    wu_d = din("w_up", [128, NF, 1024])
    wd_d = din("w_down", [128, NF, 1024])
    w1k_d = din("w1k", [128, 32 * 256])
    w1v_d = din("w1v", [128, 32 * 256])
    w2k_d = din("w2k", [128, 2 * 64])
    w2v_d = din("w2v", [128, 2 * 64])
    pek_d = din("pek", [128, 32])
    pev_d = din("pev", [128, 32])
    g1_d = din("g1", [128, 8])
    g2_d = din("g2", [128, 8])
    lbp_d = din("lbp", [128, 8])
    hgain_d = din("hgain", [1, 512])
    ngain_d = din("ngain", [1, 512])
    fgain_d = din("fgain", [1, 1024])
    cw_d = din("convw", [128, NF * 3])
    cb_d = din("convb", [128, NF])
    c_ident = din("c_ident", [128, 128])
    c_perm = din("c_perm", [128, 128])
    c_invf = din("c_invf", [128, 1])
    c_tri0 = din("c_tri0", [128, 512])
    c_tri4 = din("c_tri4", [128, 512])
    c_eblk = din("c_eblk", [128, 16 * 128])
    c_cmpm = din("c_cmpm", [128, T])
    c_keep = din("c_keep", [128, 8 * 32])
    c_add = din("c_add", [128, 8 * 32])
    c_ovl = din("c_ovl", [128, 32])
    out_d = nc.dram_tensor("out", [T, D], F32, kind="ExternalOutput").ap()
    dbg = {}
    if debug:
        dbg["mix"] = nc.dram_tensor("dbg_mix", [128, 8 * T], F32, kind="ExternalOutput").ap()
        dbg["x1"] = nc.dram_tensor("dbg_x1", [T, D], F32, kind="ExternalOutput").ap()

    with ExitStack() as es:
        def sb(name, shape, dt):
            return es.enter_context(nc.sbuf_tensor("s_" + name, shape, dt))

        X1 = sb("X1", [128, NT * D], F32)
        HT = sb("HT", [128, 8, T], BF16)
        MIXT = sb("MIXT", [128, 8 * T // 2], F32)
        MIXTb = MIXT[:, :].bitcast(BF16).rearrange("p (c t) -> p c t", t=T)
        identb = sb("identb", [128, 128], BF16)
        permf = sb("permf", [128, 128], F32)
        invf = sb("invf", [128, 1], F32)
        tri0 = sb("tri0", [128, 512], BF16)
        tri4 = sb("tri4", [128, 512], BF16)
        eblk = sb("eblk", [128, 16, 128], BF16)
        cmpm = sb("cmpm", [128, T], BF16)
        keep_t = sb("keep_t", [128, 8, 32], F32)
        add_t = sb("add_t", [128, 8, 32], F32)
        g1 = sb("g1s", [128, 8], F32)
        g2 = sb("g2s", [128, 8], F32)
        lbp = sb("lbps", [128, 8], F32)
        lb = sb("lb", [128, 4], F32)
        oml = sb("oml", [128, 4], F32)
        hgain = sb("hgain", [128, 512], F32)
        ngain = sb("ngain", [128, 512], F32)
        fgain = sb("fgain", [128, 1024], F32)
        convw = sb("convw", [128, NF, 3], F32)
        convb = sb("convb", [128, NF], F32)
        stat = sb("stat", [128, 64], F32)
        WKN = 13912
        WK = sb("WK", [128, WKN], F32)
        pbs = [es.enter_context(nc.psum_tensor("pb%d" % k, [128, 512], F32)) for k in range(8)]
        pbR = [Res() for _ in range(8)]
        sems = {k: es.enter_context(nc.semaphore(k)) for k in fw.sem_keys()}

        cR = Res()
        bank_i = [0]
        lane_banks = [None]
        lane_pos = {}

        def bank():
            if lane_banks[0] is not None:
                ids = lane_banks[0]
                key = tuple(ids)
                p = lane_pos.get(key, 0)
                lane_pos[key] = (p + 1) % len(ids)
                k = ids[p]
                return pbs[k], pbR[k]
            k = bank_i[0]
            bank_i[0] = (k + 1) % 8
            return pbs[k], pbR[k]

        wk = Arena(WK[:, :], WKN)
        xa = Arena(X1[:, :], NT * D)
        ma = Arena(MIXT[:, :], 8 * T // 2)


        def mmg(outap, outres, pairs, reads):
            n = len(pairs)
            for k, (l, r) in enumerate(pairs):
                fw.op(fw.pe, f_mm(outap, l, r, start=(k == 0), stop=(k == n - 1)), reads=reads, writes=[outres],
                      signal=(k == n - 1))

        wk.reset()
        xt = [wk.f32([128, D]) for _ in range(4)]
        xtR = [Res() for _ in range(4)]
        x_tiles = x_d.rearrange("(n p) d -> n p d", p=128)
        for i in range(4):
            fw.dma(fw.sp, f_dma(xt[i][:, :], x_tiles[i]), writes=[xtR[i]])
        ev_id = fw.dma(fw.pool, f_dma(identb[:], c_ident), writes=[Res()])
        ev_g1 = fw.dma(fw.sp, f_dma(g1[:], g1_d), writes=[Res()])
        epsT = sb("epsT", [128, 1], F32)
        ev_eps = fw.op(fw.pool, f_memset(epsT[:], EPS), writes=[Res()])
        oneT = sb("oneT", [128, 1], F32)
        fw.op(fw.pool, f_memset(oneT[:], 1.0), writes=[Res()])
        fw._wait(fw.pe, ev_id)
        fw._wait(fw.dve, ev_g1)
        fw._wait(fw.act, ev_eps)
        xa.reset()
        _v_tm_pre = xa.bf16([128, NT, 512])
        wtm_pre = [xa.bf16([128, 8, 512]) for _ in range(2)]
        wtmR_pre = [Res(), Res()]
        fw.dma(fw.pool, f_dma(wtm_pre[0][:].rearrange("p a b -> p (a b)").rearrange("p (s e) -> p s e", e=2048),
                              wti_d.rearrange("p (s e) -> p s e", e=2048)), writes=[wtmR_pre[0]])
        mhi_pre = Arena(MIXT[:, 4096:8192], 4096)
        wfm_pre = [mhi_pre.bf16([128, 8, 128]) for _ in range(3)]
        wfmR_pre = [Res() for _ in range(4)]
        fw.dma(fw.pool, f_dma(wfm_pre[0][:].rearrange("p a b -> p (a b)"), wfm_d[:, 0, :]), writes=[wfmR_pre[0]])
        fw.dma(fw.pool, f_dma(wfm_pre[1][:].rearrange("p a b -> p (a b)"), wfm_d[:, 4, :]), writes=[wfmR_pre[1]])
        fw.dma(fw.pool, f_dma(wtm_pre[1][:].rearrange("p a b -> p (a b)").rearrange("p (s e) -> p s e", e=2048),
                              wtg_d.rearrange("p (s e) -> p s e", e=2048)), writes=[wtmR_pre[1]])
        for (dst, src) in [(permf[:], c_perm), (invf[:], c_invf), (keep_t[:].rearrange("p a b -> p (a b)"), c_keep),
                           (add_t[:].rearrange("p a b -> p (a b)"), c_add), (g2[:], g2_d), (lbp[:], lbp_d),
                           (hgain[:], hgain_d.broadcast_to([128, 512])), (ngain[:], ngain_d.broadcast_to([128, 512])),
                           (fgain[:], fgain_d.broadcast_to([128, 1024])),
                           (convw[:].rearrange("p a b -> p (a b)"), cw_d), (convb[:], cb_d)]:
            fw.dma(fw.sp, f_dma(dst, src), writes=[Res()])
        for (dst, src) in [(tri0[:], c_tri0), (tri4[:], c_tri4),
                           (eblk[:].rearrange("p a b -> p (a b)"), c_eblk), (cmpm[:], c_cmpm)]:
            fw.dma(fw.pool, f_dma(dst, src), writes=[Res()])

        HT_R = [Res() for _ in range(NT)]
        MX_R = [Res() for _ in range(NT)]
        X1_R = [Res() for _ in range(NT)]

        def ffn_ring(ar):
            wdn_ = [None] * 12
            for q_ in range(2, 12):
                wdn_[q_] = ar.bf16([128, 1024])
            wgu_ = [ar.bf16([128, 2, 8, 128]) for _ in range(3)]
            wdn_[0] = ar.bf16([128, 1024])
            wdn_[1] = ar.bf16([128, 1024])
            return wgu_, [Res() for _ in range(3)], wdn_, [Res() for _ in range(12)]

        def rstd_from_ss(ss_ap, n_feat, out_ap, res):
            fw.op(fw.act, f_act(out_ap, ss_ap, AF.Sqrt, scale=1.0 / n_feat, bias=epsT[:, 0:1]), reads=[res, cR], writes=[res])
            fw.op(fw.dve, lambda e: e.reciprocal(out_ap, out_ap), reads=[res], writes=[res])

        def norm_to_HT(src_tile_ap, src_res, i, gains, wka):
            st = wka["st"][i % 4]
            stR = wka["stR"][i % 4]
            junk = wka["junk"]
            fw.op(fw.act, f_act(junk[:, :], src_tile_ap, AF.Square, accum_out=st[:, 0:1]), reads=[src_res], writes=[wka["junkR"], stR])
            rstd_from_ss(st[:, 0:1], D, st[:, 1:2], stR)
            xb = wka["xb"][i % 4]
            xbR = wka["xbR"][i % 4]
            fw.op(fw.dve, f_ts(xb[:, :], src_tile_ap, st[:, 1:2], None, ALU.mult), reads=[src_res, stR], writes=[xbR])
            pt, ptR = bank()
            ptb = pt[:, :].bitcast(BF16)
            for c in range(8):
                fw.op(fw.pe, f_tr(ptb[:, c * 128:(c + 1) * 128], xb[:, c * 128:(c + 1) * 128], identb[:]), reads=[xbR, cR], writes=[ptR],
                      signal=(c == 7))
            fw.op(fw.dve, f_tt(HT[:, :, i * 128:(i + 1) * 128], ptb.rearrange("p (c t) -> p c t", t=128),
                               gains[:, 0:8].unsqueeze(2).broadcast_to([128, 8, 128]), ALU.mult),
                  reads=[ptR, cR], writes=[HT_R[i]])

        def chk(name):
            if stop == name:
                fw.barrier()
                raise _Stop()
        try:
            wka = dict(st=[stat[:, 2 * q_:2 * q_ + 2] for q_ in range(4)], stR=[Res() for _ in range(4)], junk=wk.bf16([128, D]), junkR=Res(),
                       xb=[wk.bf16([128, D]) for _ in range(4)], xbR=[Res() for _ in range(4)])
            lanes = [[], [], [], []]
            for i in range(NT):
                lane_banks[0] = [2 * (i % 4), 2 * (i % 4) + 1]
                fw.rec_begin()
                if i >= 4:
                    fw.dma(fw.sp, f_dma(xt[i % 4][:, :], x_tiles[i]), writes=[xtR[i % 4]])
                norm_to_HT(xt[i % 4][:, :], xtR[i % 4], i, g1, wka)
                lanes[i % 4] += fw.rec_end()
            lane_banks[0] = None
            fw.replay_lanes(lanes, stagger=4)
            fw.barrier()
            if stop == "P1":
                raise _Stop()

            wk.reset()
            xa.reset()
            lbR = Res()
            fw.op(fw.dve, f_tt(lb[:], lbp[:, 0:4], lbp[:, 4:8], ALU.subtract), writes=[lbR])
            fw.op(fw.act, f_act(lb[:], lb[:], AF.Sigmoid), reads=[lbR], writes=[lbR])
            fw.op(fw.dve, f_ts(oml[:], lb[:], -1.0, 1.0, ALU.mult, ALU.add), reads=[lbR], writes=[lbR])
            fw.op(fw.dve, f_ts(hgain[:, :], hgain[:, :], -1.0, None, ALU.mult), reads=[cR], writes=[cR])
            mhi = Arena(MIXT[:, 4096:8192], 4096)
            v_tm = xa.bf16([128, NT, 512])
            v_R = [Res() for _ in range(NT)]
            wtm = [xa.bf16([128, 8, 512]) for _ in range(2)]
            wtmR = wtmR_pre
            H2 = T // 4
            tq = [xa.f32([128, H2]) for _ in range(4)]
            tf = [xa.f32([128, H2]) for _ in range(4)]
            tb_ = [xa.f32([128, H2]) for _ in range(4)]
            te = [xa.f32([128, H2]) for _ in range(4)]
            tqR, tfR, tbR, teR = [[Res() for _ in range(4)] for _ in range(4)]
            qdT = wk.bf16([128, 4, T])
            kiT = wk.bf16([128, 4, T])
            kd_tm = wk.bf16([128, 4, NT, 128])
            rmask = wk.f32([128, H2])
            qdR = [Res() for _ in range(4)]
            kiR = [Res() for _ in range(4)]
            kdtR = [Res() for _ in range(4)]
            wfm = [mhi.bf16([128, 8, 128]) for _ in range(3)] + [wk.bf16([128, 8, 128])]
            wfmR = wfmR_pre
            kdT = [mhi.bf16([128, H2]) for _ in range(4)]
            kdR = [Res() for _ in range(4)]
            Sf = mhi.f32([128, 4, 128])
            SfR = [Res() for _ in range(4)]
            dec = mhi.f32([128, 4, NT])
            decR = Res()
            hst = mhi.f32([128, 16])
            fw.op(fw.pool, f_memset(rmask[:, :], 1.0), writes=[cR])
            fw.op(fw.pool, f_memset(rmask[:, :].rearrange("p (n c) -> p n c", c=128)[:, :, 0:1], 0.0), writes=[cR])
            def load_fm(chunk, ws):
                fw.dma(fw.pool, f_dma(wfm[ws][:].rearrange("p a b -> p (a b)"), wfm_d[:, chunk, :]), writes=[wfmR[ws]])
            for i in range(NT):
                pb, pR = bank()
                mmg(pb[:, :], pR, [(HT[:, c, i * 128:(i + 1) * 128], wtm[0][:, c, :]) for c in range(8)], [HT_R[i], wtmR[0]])
                fw.op(fw.act, f_act(v_tm[:, i, :], pb[:, :], AF.Copy), reads=[pR], writes=[v_R[i]])
            def fm_unit(h, u):
                s = u
                c0_ = u * H2
                cur_q, cur_f = 2 * (h % 2), 2 * (h % 2) + 1
                tbk = u
                pb, pR = bank()
                mmg(pb[:, :], pR, [(wfm[cur_f][:, c, :], HT[:, c, tbk * 512:(tbk + 1) * 512]) for c in range(8)],
                    HT_R[4 * tbk:4 * tbk + 4] + [wfmR[cur_f]])
                fw.op(fw.act, f_act(tf[s][:, :], pb[:, :], AF.Exp, scale=-1.0), reads=[pR], writes=[tfR[s]])
                fw.op(fw.act, f_act(tf[s][:, :], tf[s][:, :], AF.Ln, bias=oneT[:, 0:1]), reads=[tfR[s]], writes=[tfR[s]])
                fw.op(fw.act, f_act(tf[s][:, :], tf[s][:, :], AF.Exp, scale=-1.0), reads=[tfR[s]], writes=[tfR[s]])
                fw.op(fw.dve, f_ts(tf[s][:, :], tf[s][:, :], oml[:, h:h + 1], lb[:, h:h + 1], ALU.mult, ALU.add), reads=[tfR[s], lbR], writes=[tfR[s]])
                fw.op(fw.act, f_act(tb_[s][:, :], tf[s][:, :], AF.Ln), reads=[tfR[s]], writes=[tbR[s]])
                fw.op(fw.dve, (lambda s_: lambda e: e.tensor_tensor_scan(out=te[s_][:, :], data0=rmask[:, :], data1=tb_[s_][:, :], initial=0.0,
                                                                         op0=ALU.mult, op1=ALU.add))(s), reads=[tbR[s], cR], writes=[teR[s]])
                fw.op(fw.act, f_act(tb_[s][:, :], te[s][:, :], AF.Exp), reads=[teR[s]], writes=[tbR[s]])
                pq, pqR = bank()
                mmg(pq[:, :], pqR, [(wfm[cur_q][:, c, :], HT[:, c, tbk * 512:(tbk + 1) * 512]) for c in range(8)],
                    HT_R[4 * tbk:4 * tbk + 4] + [wfmR[cur_q]])
                fw.op(fw.dve, f_tt(qdT[:, h, c0_:c0_ + H2], pq[:, :], tb_[s][:, :], ALU.mult), reads=[pqR, tbR[s]], writes=[qdR[h]])
                fw.op(fw.act, f_act(tq[s][:, :], te[s][:, :], AF.Exp, scale=-1.0), reads=[teR[s]], writes=[tqR[s]])
                te3 = te[s][:, :].rearrange("p (n c) -> p n c", c=128)
                fw.op(fw.act, f_act(dec[:, h, 4 * u:4 * u + 4], te3[:, :, 127], AF.Exp), reads=[teR[s]], writes=[decR])
                fw.op(fw.dve, f_stt(kiT[:, h, c0_:c0_ + H2], tf[s][:, :], 1.0, tq[s][:, :], ALU.subtract, ALU.mult), reads=[tfR[s], tqR[s]], writes=[kiR[h]])
                fw.op(fw.dve, f_tt(kdT[s][:, :].rearrange("p (n c) -> p n c", c=128), kiT[:, h, c0_:c0_ + H2].rearrange("p (n c) -> p n c", c=128),
                                   dec[:, h, 4 * u:4 * u + 4].unsqueeze(2).broadcast_to([128, 4, 128]), ALU.mult), reads=[kiR[h], decR], writes=[kdR[s]])
                pt, ptR = bank()
                ptb = pt[:, :].bitcast(BF16)
                for k_ in range(4):
                    fw.op(fw.pe, f_tr(ptb[:, k_ * 128:(k_ + 1) * 128], kdT[s][:, k_ * 128:(k_ + 1) * 128], identb[:]), reads=[kdR[s], cR], writes=[ptR],
                          signal=(k_ == 3))
                fw.op(fw.dve, f_copy(kd_tm[:, h, 4 * u:4 * u + 4, :], ptb[:, 0:512].rearrange("p (k d) -> p k d", d=128)),
                      reads=[ptR], writes=[kdtR[h]])

            lanes = [[], [], [], []]
            fm_tails = [[], [], [], []]
            for h in range(4):
                pre = []
                if h < 3:
                    fw.rec_begin()
                    load_fm(h + 1, 2 * ((h + 1) % 2))
                    load_fm(4 + h + 1, 2 * ((h + 1) % 2) + 1)
                    pre = fw.rec_end()
                for u in range(4):
                    lane_banks[0] = [2 * u, 2 * u + 1]
                    fw.rec_begin()
                    fm_unit(h, u)
                    body = fw.rec_end()
                    if h == 0 and u > 0:
                        lanes[u] += [None] * (u * (len(body) // 4))
                    tail_ = body[-5:]
                    body = body[:-5]
                    mid = len(body) // 2
                    fill = pre if u == 0 else [None] * len(pre)
                    lanes[u] += body[:10] + fm_tails[u] + body[10:mid] + fill + body[mid:]
                    fm_tails[u] = tail_
            lane_banks[0] = None
            for u in range(4):
                lanes[u] += fm_tails[u]
            fw.replay_lanes(lanes)
            fw.barrier()
            wtn = MIXT[:, 4096:4096 + 1120].bitcast(BF16).rearrange("p (a b) -> p a b", b=280)
            wtnR = Res()
            fw.dma(fw.pool, f_dma(wtn[:, :, :], wtn_d.rearrange("p (a b) -> p a b", b=280)), writes=[wtnR])
            xb_ = Arena(X1[:, 8192:16384], 8192)
            atm = [xb_.bf16([128, 512]) for _ in range(2)]
            atmR = [Res(), Res()]
            sg = [xb_.f32([128, 512]) for _ in range(2)]
            sgR = [Res(), Res()]
            sq = [xb_.f32([128, 512]) for _ in range(2)]
            sqR = [Res(), Res()]
            t1 = [xb_.f32([128, 512]) for _ in range(2)]
            t1R = [Res(), Res()]
            mxh = [xb_.bf16([128, 512]) for _ in range(2)]
            mxhR = [Res(), Res()]
            Ssn = xb_.bf16([128, NT, 4, 128])
            SsnR = [Res() for _ in range(NT)]
            hstR = [Res(), Res()]
            fw.op(fw.pool, f_memset(Ssn[:, 0, :, :].rearrange("p a b -> p (a b)"), 0.0), writes=[SsnR[0]])
            fw.op(fw.pool, f_memset(Sf[:, :, :].rearrange("p a b -> p (a b)"), 0.0), writes=SfR)

            def s_step(m):
                pu, puR = bank()
                for h in range(4):
                    fw.op(fw.pe, f_mm(pu[:, h * 128:(h + 1) * 128], kd_tm[:, h, m, :], v_tm[:, m, h * 128:(h + 1) * 128]), reads=[kdtR[h], v_R[m]],
                          writes=[puR], signal=(h == 3))
                for h in range(4):
                    fw.op(fw.dve, f_stt(Sf[:, h, :], Sf[:, h, :], dec[:, h, m:m + 1], pu[:, h * 128:(h + 1) * 128], ALU.mult, ALU.add),
                          reads=[SfR[h], decR, puR], writes=[SfR[h]])
                fw.op(fw.pool, f_copy(Ssn[:, m + 1, :, :], Sf[:, :, :]), reads=SfR, writes=[SsnR[m + 1]])

            def o_tile(n):
                s = n % 2
                pa, paR = bank()
                for h in range(4):
                    fw.op(fw.pe, f_mm(pa[:, h * 128:(h + 1) * 128], kiT[:, h, n * 128:(n + 1) * 128], qdT[:, h, n * 128:(n + 1) * 128]), reads=[kiR[h], qdR[h]],
                          writes=[paR], signal=(h == 3))
                fw.op(fw.dve, f_tt(atm[s][:, :], pa[:, :], tri0[:, :], ALU.mult), reads=[paR, cR], writes=[atmR[s]])
                pg, pgR = bank()
                mmg(pg[:, :], pgR, [(HT[:, c, n * 128:(n + 1) * 128], wtm[1][:, c, :]) for c in range(8)], [HT_R[n], wtmR[1]])
                fw.op(fw.act, f_act(sg[s][:, :], pg[:, :], AF.Silu), reads=[pgR], writes=[sgR[s]])
                po, poR = bank()
                for h in range(4):
                    hs = slice(h * 128, (h + 1) * 128)
                    fw.op(fw.pe, f_mm(po[:, hs], atm[s][:, hs], v_tm[:, n, hs], start=True, stop=False), reads=[atmR[s], v_R[n]], writes=[poR], signal=False)
                    fw.op(fw.pe, f_mm(po[:, hs], qdT[:, h, n * 128:(n + 1) * 128], Ssn[:, n, h, :], start=False, stop=True), reads=[qdR[h], SsnR[n]], writes=[poR],
                          signal=(h == 3))
                fw.op(fw.act, f_act(sq[s][:, :], po[:, :], AF.Square), reads=[poR], writes=[sqR[s]])
                fw.op(fw.dve, lambda e, s_=s: e.tensor_reduce(out=hst[:, 8 * s_:8 * s_ + 4], in_=sq[s_][:, :].rearrange("p (h v) -> p h v", v=128),
                                                            axis=mybir.AxisListType.X, op=ALU.add), reads=[sqR[s]], writes=[hstR[s]])
                fw.op(fw.act, f_act(hst[:, 8 * s + 4:8 * s + 8], hst[:, 8 * s:8 * s + 4], AF.Sqrt, scale=1.0 / 128, bias=epsT[:, 0:1]), reads=[hstR[s], cR], writes=[hstR[s]])
                fw.op(fw.dve, lambda e, s_=s: e.reciprocal(hst[:, 8 * s_ + 4:8 * s_ + 8], hst[:, 8 * s_ + 4:8 * s_ + 8]), reads=[hstR[s]], writes=[hstR[s]])
                fw.op(fw.dve, f_tt(t1[s][:, :].rearrange("p (h v) -> p h v", v=128), po[:, :].rearrange("p (h v) -> p h v", v=128),
                                   hst[:, 8 * s + 4:8 * s + 8].unsqueeze(2).broadcast_to([128, 4, 128]), ALU.mult), reads=[poR, hstR[s]], writes=[t1R[s]])
                fw.op(fw.pool, f_tt(sg[s][:, :], sg[s][:, :], hgain[:, :], ALU.mult), reads=[sgR[s], cR], writes=[sgR[s]])
                fw.op(fw.dve, f_tt(mxh[s][:, :], t1[s][:, :], sg[s][:, :], ALU.mult), reads=[t1R[s], sgR[s]], writes=[mxhR[s]])
                pt, ptR = bank()
                ptb = pt[:, :].bitcast(BF16)
                for h in range(4):
                    fw.op(fw.pe, f_tr(ptb[:, h * 128:(h + 1) * 128], mxh[s][:, h * 128:(h + 1) * 128], identb[:]), reads=[mxhR[s], cR], writes=[ptR], signal=(h == 3))
                fw.op(fw.act, f_act(MIXTb[:, 0:4, n * 128:(n + 1) * 128], ptb[:, 0:512].rearrange("p (c t) -> p c t", t=128), AF.Copy), reads=[ptR], writes=[MX_R[n]])

            lanes = [[], [], []]
            lane_banks[0] = [0, 7]
            fw.rec_begin()
            for m in range(NT - 1):
                s_step(m)
            lanes[0] = fw.rec_end()
            tails = {1: [], 2: []}
            for n in range(NT):
                lane_banks[0] = [1, 2, 3] if n % 2 == 0 else [4, 5, 6]
                fw.rec_begin()
                o_tile(n)
                it_ = fw.rec_end()
                ln_ = 1 + n % 2
                body_, tail_ = it_[:-5], it_[-5:]
                lanes[ln_] += body_[:8] + tails[ln_] + body_[8:]
                tails[ln_] = tail_
            for ln_ in (1, 2):
                lanes[ln_] += tails[ln_]
            lane_banks[0] = None
            lanes[2] = [None] * 20 + lanes[2]
            fw.replay_lanes(lanes)
            fw.barrier()
            if stop == "P2":
                raise _Stop()

            wk.reset()
            xa.reset()
            qT = wk.bf16([128, 4, T])
            kcT = wk.bf16([128, T])
            vcT = wk.bf16([128, T])
            Vs = wk.bf16([128, NT, 2, 66])
            Vw = wk.bf16([128, NT, 2, 66])
            gates = wk.f32([128, NT, 24])
            qTR, kcR, vcR, ksR, kwR, VsR, VwR, gtR = [Res() for _ in range(8)]
            ksZ = [wk.bf16([128, T]) for _ in range(2)]
            kwZ = [wk.bf16([128, T]) for _ in range(2)]
            kzR = Res()
            for g_ in range(2):
                fw.op(fw.pool, f_memset(ksZ[g_][:, :], 0.0), writes=[kzR])
                fw.op(fw.pool, f_memset(kwZ[g_][:, :], 0.0), writes=[kzR])
            cosT = xa.f32([128, T])
            sinT = xa.f32([128, T])
            posi = xa.i32([128, T])
            posf = xa.f32([128, T])
            yy = xa.f32([128, T])
            wfm = [xa.bf16([128, 8, 128]) for _ in range(3)]
            wfmR = [Res() for _ in range(3)]
            qf = [xa.f32([128, 512]) for _ in range(2)]
            qfR = [Res(), Res()]
            qb = [wk.bf16([128, 512]) for _ in range(2)]
            qbR = [Res(), Res()]
            permb = wk.bf16([128, 128])
            fw.op(fw.dve, f_copy(permb[:, :], permf[:, :]), reads=[cR], writes=[cR])
            r1 = [xa.f32([128, 512]) for _ in range(2)]
            r1R = [Res(), Res()]
            r2 = [xa.f32([128, 512]) for _ in range(2)]
            r2R = [Res(), Res()]
            ropeR = Res()
            w1kb = MIXT[:, 4096:8192].bitcast(BF16).rearrange("p (l h) -> p l h", h=256)
            w1kR = Res()
            fw.dma(fw.sp, f_dma(posi[:, :], pos_d.broadcast_to([128, T])), writes=[ropeR])
            fw.op(fw.dve, f_copy(posf[:, :], posi[:, :]), reads=[ropeR], writes=[ropeR])
            fw.op(fw.dve, f_ts(yy[:, :], posf[:, :], invf[:, 0:1], None, ALU.mult), reads=[ropeR, cR], writes=[ropeR])

            def frac_sin(dst, src, add):
                if add != 0.0:
                    fw.op(fw.dve, f_ts(dst, src, add, None, ALU.add), reads=[ropeR], writes=[ropeR])
                    src = dst
                fw.op(fw.dve, f_copy(posi[:, :], src), reads=[ropeR], writes=[ropeR])
                fw.op(fw.dve, f_copy(posf[:, :], posi[:, :]), reads=[ropeR], writes=[ropeR])
                fw.op(fw.dve, f_tt(dst, src, posf[:, :], ALU.subtract), reads=[ropeR], writes=[ropeR])
                fw.op(fw.dve, f_stt(posf[:, :], dst, 0.5, dst, ALU.is_gt, ALU.subtract), reads=[ropeR], writes=[ropeR])
                fw.op(fw.dve, f_stt(dst, posf[:, :], 0.5, posf[:, :], ALU.is_gt, ALU.subtract), reads=[ropeR], writes=[ropeR])
                fw.op(fw.act, f_act(dst, dst, AF.Sin, scale=2 * PI), reads=[ropeR], writes=[ropeR])


            ndma = [0]

            p3lanes = [[], []]

            def fm_proj2(chunk, consume):
                ws = ndma[0] % 3
                ndma[0] += 1
                fw.rec_begin()
                fw.dma(fw.pool, f_dma(wfm[ws][:].rearrange("p a b -> p (a b)"), wfm_d[:, chunk, :]), writes=[wfmR[ws]])
                pre = fw.rec_end()
                p3lanes[0] += pre
                p3lanes[1] += [None] * len(pre)
                for tb in range(4):
                    lane_banks[0] = [4 * (tb % 2) + b_ for b_ in range(4)]
                    fw.rec_begin()
                    pb, pR = bank()
                    mmg(pb[:, :], pR, [(wfm[ws][:, c, :], HT[:, c, tb * 512:(tb + 1) * 512]) for c in range(8)],
                        HT_R[4 * tb:4 * tb + 4] + [wfmR[ws]])
                    consume(tb, pb, pR)
                    p3lanes[tb % 2] += fw.rec_end()
                lane_banks[0] = None

            rope_cnt = [0]

            def rope_consume(dst2d, dstR):
                def consume(tb, pb, pR):
                    s = tb % 2
                    sl = slice(tb * 512, (tb + 1) * 512)
                    fw.op(fw.act, f_act(qf[s][:, :], pb[:, :], AF.Copy), reads=[pR], writes=[qfR[s]])
                    fw.op(fw.act, f_act(qb[s][:, :], pb[:, :], AF.Copy), reads=[pR], writes=[qbR[s]])
                    pr, prR = bank()
                    fw.op(fw.pe, f_mm(pr[:, :], permb[:, :], qb[s][:, :]), reads=[qbR[s], cR], writes=[prR])
                    fw.op(fw.dve, f_tt(r1[s][:, :], qf[s][:, :], cosT[:, sl], ALU.mult), reads=[qfR[s], ropeR], writes=[r1R[s]])
                    fw.op(fw.dve, f_tt(r2[s][:, :], pr[:, :], sinT[:, sl], ALU.mult), reads=[prR, ropeR], writes=[r2R[s]])
                    if isinstance(dst2d, list):
                        for g_ in range(2):
                            rw = slice(g_ * 64, (g_ + 1) * 64)
                            fw.op(fw.dve, f_tt(dst2d[g_][rw, sl], r1[s][rw, :], r2[s][rw, :], ALU.add), reads=[r1R[s], r2R[s]], writes=[dstR])
                    else:
                        fw.op(fw.dve, f_tt(dst2d[:, sl], r1[s][:, :], r2[s][:, :], ALU.add), reads=[r1R[s], r2R[s]], writes=[dstR])
                return consume

            fw.op(fw.dve, f_memset(Vs[:, :, :, :].rearrange('p a b c -> p (a b c)'), 1.0), writes=[VsR])
            fw.op(fw.dve, f_memset(Vw[:, :, :, :].rearrange('p a b c -> p (a b c)'), 1.0), writes=[VwR])
            for i in range(NT):
                pb, pR = bank()
                mmg(pb[:, 0:280], pR, [(HT[:, c, i * 128:(i + 1) * 128], wtn[:, c, :]) for c in range(8)], [HT_R[i], wtnR])
                fw.op(fw.act, f_act(Vs[:, i, :, 0:64], pb[:, 0:128].rearrange("p (g d) -> p g d", d=64), AF.Copy), reads=[pR], writes=[VsR])
                fw.op(fw.act, f_act(Vw[:, i, :, 0:64], pb[:, 128:256].rearrange("p (g d) -> p g d", d=64), AF.Copy), reads=[pR], writes=[VwR])
                fw.op(fw.act, f_act(gates[:, i, :], pb[:, 256:280], AF.Sigmoid), reads=[pR], writes=[gtR])
            frac_sin(sinT[:, :], yy[:, :], 0.0)
            frac_sin(cosT[:, :], yy[:, :], 0.25)
            fm_proj2(13, lambda tb, pb, pR: fw.op(fw.act, f_act(vcT[:, tb * 512:(tb + 1) * 512], pb[:, :], AF.Copy), reads=[pR], writes=[vcR]))
            for c in range(4):
                fm_proj2(8 + c, rope_consume(qT[:, c, :], qTR))
            fm_proj2(12, rope_consume(kcT, kcR))
            fm_proj2(14, rope_consume(ksZ, kzR))
            fm_proj2(15, rope_consume(kwZ, kzR))
            p3lanes[1] = [None] * 8 + p3lanes[1]
            fw.replay_lanes(p3lanes)
            fw.dma(fw.pool, f_dma(w1kb.rearrange("p a b -> p (a b)").rearrange("p (s e) -> p s e", e=2048),
                                  w1k_d.rearrange("p (s e) -> p s e", e=2048)), writes=[w1kR, wtnR])
            chk('P3a3')
            fw.barrier()
            if stop == "P3a":
                raise _Stop()

            xa.reset()
            w1b = xa.bf16([128, 32, 256])
            w1R = Res()
            w2b = xa.bf16([128, 2, 64])
            w2R = Res()
            peb = xa.bf16([128, 32])
            pef = xa.f32([128, 32])
            peR = Res()
            hidT = xa.bf16([128, 2, 2, 128])
            hidR = Res()
            cvec = xa.f32([128, 2])
            cvR = Res()
            kcc = wk.bf16([128, 2, 128])
            kccR = Res()
            vcx = wk.bf16([128, 2, 98])
            vcxR = Res()
            fw.op(fw.pool, f_memset(vcx[:, :, :].rearrange('p a b -> p (a b)'), 1.0), writes=[vcxR])
            fw.op(fw.pool, f_memset(vcx[:, :, 0:64], 0.0), writes=[vcxR])
            ovl_f = xa.f32([128, 32])
            fw.dma(fw.sp, f_dma(ovl_f[:, :], c_ovl), writes=[vcxR])
            for g in range(2):
                fw.op(fw.dve, f_copy(vcx[:, g, 65:97], ovl_f[:, :]), reads=[vcxR], writes=[vcxR])
            fw.op(fw.pool, f_memset(kcc[:, :, :].rearrange("p a b -> p (a b)"), 0.0), writes=[kccR])

            w1v_buf = w1b
            fw.dma(fw.pool, f_dma(w1v_buf[:].rearrange("p a b -> p (a b)").rearrange("p (s e) -> p s e", e=2048),
                                  w1v_d.rearrange("p (s e) -> p s e", e=2048)), writes=[w1R])
            for kv in range(2):
                srcT, srcR = (kcT, kcR) if kv == 0 else (vcT, vcR)
                w1b = w1kb if kv == 0 else w1v_buf
                if kv == 0:
                    w1R_save = w1R
                    w1R = w1kR
                else:
                    w1R = w1R_save
                fw.dma(fw.pool, f_dma(w2b[:].rearrange("p a b -> p (a b)"), w2k_d if kv == 0 else w2v_d), writes=[w2R])
                fw.dma(fw.sp, f_dma(pef[:, :], pek_d if kv == 0 else pev_d), writes=[peR])
                fw.op(fw.dve, f_copy(peb[:, :], pef[:, :]), reads=[peR], writes=[peR])
                src3 = srcT[:, :].rearrange("p (n l) -> p n l", l=16)
                for hc in range(2):
                    pc, pcR = bank()
                    mmg(pc[:, 0:1], pcR, [(w1b[0:64, l, hc * 128:(hc + 1) * 128], peb[0:64, l:l + 1]) for l in range(32)], [w1R, peR])
                    fw.op(fw.dve, f_copy(cvec[:, hc:hc + 1], pc[:, 0:1]), reads=[pcR], writes=[cvR])
                    phs = [bank(), bank()]
                    for l in range(32):
                        for g in range(2):
                            ph, phR = phs[g]
                            rows = slice(g * 64, (g + 1) * 64)
                            rhs = src3[rows, 0:127, l] if l < 16 else src3[rows, 1:128, l - 16]
                            fw.op(fw.pe, f_mm(ph[:, 0:127], w1b[rows, l, hc * 128:(hc + 1) * 128], rhs, start=(l == 0), stop=(l == 31)),
                                  reads=[w1R, srcR], writes=[phR], signal=(l == 31))
                    for g in range(2):
                        ph, phR = phs[g]
                        fw.op(fw.act, f_act(hidT[:, g, hc, 0:127], ph[:, 0:127], AF.Silu, bias=cvec[:, hc:hc + 1]), reads=[phR, cvR], writes=[hidR])
                for g in range(2):
                    po, poR = bank()
                    if kv == 0:
                        mmg(po[g * 64:(g + 1) * 64, 0:127], poR, [(w2b[:, hc, :], hidT[:, g, hc, 0:127]) for hc in range(2)], [w2R, hidR])
                        fw.op(fw.act, f_act(kcc[g * 64:(g + 1) * 64, g, 0:127], po[g * 64:(g + 1) * 64, 0:127], AF.Copy), reads=[poR], writes=[kccR])
                    else:
                        mmg(po[0:127, 0:64], poR, [(hidT[:, g, hc, 0:127], w2b[:, hc, :]) for hc in range(2)], [w2R, hidR])
                        fw.op(fw.act, f_act(vcx[0:127, g, 0:64], po[0:127, 0:64], AF.Copy), reads=[poR], writes=[vcxR])
            fw.barrier()
            if stop == "P3b":
                raise _Stop()

            xa.reset()
            wob_pre = HT[:, 0:4, :].rearrange("p a b -> p (a b)")
            wobpR = Res()
            fw.dma(fw.pool, f_dma(wob_pre.rearrange("p (s e) -> p s e", e=2048), wo_d.rearrange("p (s e) -> p s e", e=2048)), writes=[wobpR])
            NE = 18
            ering = [[xa.bf16([128, 512]) for _ in range(NE)] for _ in range(2)]
            eR = [[Res() for _ in range(NE)] for _ in range(2)]
            e_i = [0, 0]

            def eslot(g):
                k = e_i[g]
                e_i[g] = (k + 1) % NE
                return ering[g][k], eR[g][k]
            psel = xa.f32([128, 2, 32])
            pselR = [Res(), Res()]
            sc = [xa.f32([128, 32]) for _ in range(2)]
            sc2 = [xa.f32([128, 32]) for _ in range(2)]
            m8a = [xa.f32([128, 8]) for _ in range(2)]
            m8b = [xa.f32([128, 8]) for _ in range(2)]
            selb = [xa.bf16([128, 32]) for _ in range(2)]
            selbT = [[xa.bf16([128, 4, 128]) for _ in range(2)] for _ in range(2)]
            selbTR = [[Res(), Res()] for _ in range(2)]
            for g_ in range(2):
                for p_ in range(2):
                    fw.op(fw.pool, f_memset(selbT[g_][p_][:, :, :].rearrange("p a b -> p (a b)"), 0.0), writes=[selbTR[g_][p_]])
            tkR = [Res(), Res()]
            acc = [xa.f32([128, 8, 64]) for _ in range(3)]
            accR = [[Res(), Res()] for _ in range(3)]
            coef = xa.f32([128, 3, 8])
            rs = xa.f32([128, 8])
            coefR = [Res(), Res()]
            nst = xa.f32([128, 16])
            nstR = Res()
            pos_ = [[xa.f32([128, 4, 98]) for _ in range(3)] for _ in range(2)]
            posR = [[Res() for _ in range(3)] for _ in range(2)]
            sqn = xa.f32([128, 8, 64])
            njR = Res()
            mxn = xa.bf16([128, 512])
            mxnR = Res()
            SCALE = 0.125

            def q4(g, i):
                return qT[:, :, i * 128:(i + 1) * 128]

            def f_recip(out, in_):
                return lambda e: e.reciprocal(out, in_)

            def f_max8(out, in_):
                return lambda e: e.max(out=out, in_=in_)

            def f_mrep(out, rep_, vals):
                return lambda e: e.match_replace(out=out, in_to_replace=rep_, in_values=vals, imm_value=-1e30)

            def finish_branch(b, g, i, po, poR, first):
                hs = slice(g * 4, g * 4 + 4)
                ab = acc[i % 3]
                aR = accR[i % 3][g]
                gi_ = gates[:, i, :]
                fw.op(fw.dve, f_ts(rs[:, hs], po[:, :, 64], 1e-30, None, ALU.max), reads=[poR], writes=[coefR[g]])
                fw.op(fw.dve, f_recip(rs[:, hs], rs[:, hs]), reads=[coefR[g]], writes=[coefR[g]])
                fw.op(fw.dve, f_tt(coef[:, b, hs], rs[:, hs], gi_[:, b * 8 + g * 4:b * 8 + g * 4 + 4], ALU.mult), reads=[coefR[g], gtR], writes=[coefR[g]])
                for hp in range(4):
                    h = g * 4 + hp
                    if first:
                        fw.op(fw.dve, f_ts(ab[:, h, :], po[:, hp, 0:64], coef[:, b, h:h + 1], None, ALU.mult), reads=[poR, coefR[g]], writes=[aR])
                    else:
                        fw.op(fw.dve, f_stt(ab[:, h, :], po[:, hp, 0:64], coef[:, b, h:h + 1], ab[:, h, :], ALU.mult, ALU.add),
                              reads=[poR, coefR[g], aR], writes=[aR])

            def cmp_part(i, g):
                ps_, psR = bank()
                ps3 = ps_[:, :].rearrange("p (h q) -> p h q", q=128)
                fw.op(fw.pe, f_mm(ps3, kcc[:, g, :], q4(g, i), start=True, stop=False), reads=[kccR, qTR], writes=[psR], signal=False)
                fw.op(fw.pe, f_mm(ps3, identb[:, :], cmpm[:, i * 128:(i + 1) * 128].unsqueeze(1).broadcast_to([128, 4, 128]), start=False, stop=True),
                      reads=[cR], writes=[psR])
                ec, ecR = eslot(g)
                fw.op(fw.act, f_act(ec[:, :], ps_[:, :], AF.Exp, scale=SCALE), reads=[psR], writes=[ecR])
                po, poR = bank()
                po3 = po[:, :].rearrange("p (h w) -> p h w", w=128)
                for hp in range(4):
                    fw.op(fw.pe, f_mm(po3[:, hp, 0:97], ec[:, hp * 128:(hp + 1) * 128], vcx[:, g, 0:97]), reads=[ecR, vcxR], writes=[poR])
                pst = pos_[g][0]
                fw.op(fw.dve, f_copy(pst[:, :, 0:97], po3[:, :, 0:97]), reads=[poR], writes=[posR[g][0]])
                po3 = pst
                poR = posR[g][0]
                finish_branch(0, g, i, po3, poR, True)
                if i >= 8:
                    for hp in range(4):
                        h = g * 4 + hp
                        if hp == 0:
                            fw.op(fw.dve, f_ts(psel[:, g, :], po3[:, hp, 65:97], rs[:, h:h + 1], None, ALU.mult), reads=[poR, coefR[g]], writes=[pselR[g]])
                        else:
                            fw.op(fw.dve, f_stt(psel[:, g, :], po3[:, hp, 65:97], rs[:, h:h + 1], psel[:, g, :], ALU.mult, ALU.add),
                                  reads=[poR, coefR[g], pselR[g]], writes=[pselR[g]])
                    fw.op(fw.dve, f_tt(sc[g][:, :], psel[:, g, :], keep_t[:, i - 8, :], ALU.mult), reads=[pselR[g], cR], writes=[tkR[g]])
                    fw.op(fw.dve, f_tt(sc[g][:, :], sc[g][:, :], add_t[:, i - 8, :], ALU.add), reads=[tkR[g], cR], writes=[tkR[g]])
                    fw.op(fw.dve, f_max8(m8a[g][:, :], sc[g][:, :]), reads=[tkR[g]], writes=[tkR[g]])
                    fw.op(fw.dve, f_mrep(sc2[g][:, :], m8a[g][:, :], sc[g][:, :]), reads=[tkR[g]], writes=[tkR[g]])
                    fw.op(fw.dve, f_max8(m8b[g][:, :], sc2[g][:, :]), reads=[tkR[g]], writes=[tkR[g]])
                    fw.op(fw.dve, f_ts(sc2[g][:, :], sc[g][:, :], m8b[g][:, 7:8], None, ALU.is_ge), reads=[tkR[g]], writes=[tkR[g]])
                    fw.op(fw.dve, f_ts(selb[g][:, :], sc2[g][:, :], -1.0, 30000.0, ALU.add, ALU.mult), reads=[tkR[g]], writes=[tkR[g]])
                    pt, ptR = bank()
                    ptb = pt[:, :].bitcast(BF16)
                    fw.op(fw.pe, f_tr(ptb[0:32, 0:128], selb[g][:, :], identb[:]), reads=[tkR[g], cR], writes=[ptR])
                    fw.op(fw.act, f_act(selbT[g][i % 2][0:32, :, :], ptb[0:32, 0:128].unsqueeze(1).broadcast_to([32, 4, 128]), AF.Copy),
                          reads=[ptR], writes=[selbTR[g][i % 2]])
            def make_branch(i, g):
                def branch(bidx, js, Kt, KR, Vt, VR, sel):
                    po, poR = bank()
                    po3 = po[:, :].rearrange("p (h w) -> p h w", w=128)
                    n = len(js)
                    ets = []

                    def pv(t):
                        ej, ejR, j = ets[t]
                        for hp in range(4):
                            last = (t == n - 1 and hp == 3)
                            fw.op(fw.pe, lambda e, o=po3[:, hp, 0:65], l=ej[:, hp * 128:(hp + 1) * 128], r=Vt[:, j, g, 0:65], st=(t == 0 and hp == 0), sp=last:
                                  e.matmul(o, lhsT=l, rhs=r, start=st, stop=sp, skip_group_check=True),
                                  reads=[ejR, VR], writes=[poR], signal=(hp == 3))
                    for t, j in enumerate(js):
                        while True:
                            ps_, psR = bank()
                            if ps_ is not po:
                                break
                        ps3 = ps_[:, :].rearrange("p (h q) -> p h q", q=128)
                        if sel:
                            fw.op(fw.pe, f_mm(ps3, Kt[g][:, j * 128:(j + 1) * 128], q4(g, i), start=True, stop=False),
                                  reads=[KR, qTR], writes=[psR], signal=False)
                            fw.op(fw.pe, f_mm(ps3, eblk[:, j, :], selbT[g][i % 2][:, :, :], start=False, stop=True), reads=[cR, selbTR[g][i % 2]], writes=[psR])
                        else:
                            fw.op(fw.pe, f_mm(ps3, Kt[g][:, j * 128:(j + 1) * 128], q4(g, i)), reads=[KR, qTR], writes=[psR])
                        ej, ejR = eslot(g)
                        fw.op(fw.act, f_act(ej[:, :], ps_[:, :], AF.Exp, scale=SCALE), reads=[psR], writes=[ejR])
                        if j == i:
                            fw.op(fw.dve, f_tt(ej[:, :], ej[:, :], tri0[:, :], ALU.mult), reads=[ejR, cR], writes=[ejR])
                        elif bidx == 2 and j == i - 4:
                            fw.op(fw.dve, f_tt(ej[:, :], ej[:, :], tri4[:, :], ALU.mult), reads=[ejR, cR], writes=[ejR])
                        ets.append((ej, ejR, j))
                        if t >= 2:
                            pv(t - 2)
                    for t in range(max(0, n - 2), n):
                        pv(t)
                    pst = pos_[g][bidx]
                    fw.op(fw.dve, f_copy(pst[:, :, 0:65], po3[:, :, 0:65]), reads=[poR], writes=[posR[g][bidx]])
                    finish_branch(bidx, g, i, pst, posR[g][bidx], False)

                return branch

            def win_part(i, g):
                make_branch(i, g)(2, list(range(max(0, i - 4), i + 1)), kwZ, kzR, Vw, VwR, False)

            def slc_part(i, g):
                make_branch(i, g)(1, list(range(i + 1)), ksZ, kzR, Vs, VsR, i >= 8)

            def combine(i):
                ab = acc[i % 3]
                aRs = accR[i % 3]
                fw.op(fw.dve, f_tt(sqn[:, :, :], ab[:, :, :], ab[:, :, :], ALU.mult), reads=aRs, writes=[njR])
                fw.op(fw.dve, lambda e: e.tensor_reduce(out=nst[:, 0:8], in_=sqn[:, :, :], axis=mybir.AxisListType.X, op=ALU.add), reads=[njR], writes=[nstR])
                fw.op(fw.act, f_act(nst[:, 8:16], nst[:, 0:8], AF.Ln, scale=1.0 / 64, bias=epsT[:, 0:1]), reads=[nstR, cR], writes=[nstR])
                fw.op(fw.act, f_act(nst[:, 8:16], nst[:, 8:16], AF.Exp, scale=-0.5), reads=[nstR], writes=[nstR])
                fw.op(fw.dve, f_tt(ab[:, :, :], ab[:, :, :], nst[:, 8:16].unsqueeze(2).broadcast_to([128, 8, 64]), ALU.mult),
                      reads=aRs + [nstR], writes=aRs)
                fw.op(fw.dve, f_tt(mxn[:, :], ab[:, :, :].rearrange("p h d -> p (h d)"), ngain[:, :], ALU.mult), reads=aRs + [cR], writes=[mxnR])
                pt, ptR = bank()
                ptb = pt[:, :].bitcast(BF16)
                for c in range(4):
                    fw.op(fw.pe, f_tr(ptb[:, c * 128:(c + 1) * 128], mxn[:, c * 128:(c + 1) * 128], identb[:]), reads=[mxnR, cR], writes=[ptR],
                          signal=(c == 3))
                fw.op(fw.act, f_act(MIXTb[:, 4:8, i * 128:(i + 1) * 128], ptb[:, 0:512].rearrange("p (c t) -> p c t", t=128), AF.Copy),
                      reads=[ptR], writes=[MX_R[i]])

            lanes = [[], [], []]
            lbanks = [[0, 1, 2, 3], [4, 5, 6]]

            def rec_part(fn, i, g):
                lane_banks[0] = lbanks[g]
                fw.rec_begin()
                fn(i, g)
                return fw.rec_end()
            for g in range(2):
                lanes[g] += rec_part(cmp_part, 0, g)
            lanes[2] += [None] * len(lanes[0])
            prev_cb = []
            for i in range(NT):
                n0 = len(lanes[0])
                for g in range(2):
                    lanes[g] += rec_part(win_part, i, g)
                    tail = []
                    if i + 1 < NT:
                        c_ = rec_part(cmp_part, i + 1, g)
                        if i + 1 >= 8:
                            tail = c_[-2:]
                            c_ = c_[:-2]
                        lanes[g] += c_
                    lanes[g] += rec_part(slc_part, i, g) + tail
                assert len(lanes[0]) == len(lanes[1])
                ntile = len(lanes[0]) - n0
                sp = []
                for e_ in prev_cb:
                    sp += [e_, None, None, None]
                assert len(sp) <= ntile, (len(sp), ntile)
                lanes[2] += sp + [None] * (ntile - len(sp))
                lane_banks[0] = [7]
                fw.rec_begin()
                combine(i)
                prev_cb = fw.rec_end()
            lanes[2] += prev_cb
            lane_banks[0] = None
            fw.replay_lanes(lanes)
            fw.barrier()
            if stop == "P3c":
                raise _Stop()

            if debug:
                wk.reset()
                dtmp = wk.f32([128, 2048])
                dR = Res()
                for c in range(8):
                    fw.op(fw.dve, f_copy(dtmp[:, :], MIXTb[:, c, :]), reads=[dR], writes=[dR])
                    fw.dma(fw.sp, f_dma(dbg["mix"][:, c * T:(c + 1) * T], dtmp[:, :]), reads=[dR], writes=[])
                    fw.barrier()

            wk.reset()
            wob = wk.bf16([128, 8, 1024])
            woR = Res()
            wob2 = wob[:].rearrange("p a b -> p (a b)")
            ev_a = fw.op(fw.act, f_act(wob2[:, 0:4096], wob_pre[:, 0:4096], AF.Copy), reads=[wobpR], writes=[woR])
            ev_b = fw.op(fw.dve, f_copy(wob2[:, 4096:8192], wob_pre[:, 4096:8192]), reads=[wobpR], writes=[woR])
            wka = dict(st=[stat[:, 2 * q_:2 * q_ + 2] for q_ in range(4)], stR=[Res() for _ in range(4)], junk=wk.bf16([128, D]), junkR=Res(),
                       xb=[wk.bf16([128, D]) for _ in range(4)], xbR=[Res() for _ in range(4)])
            assert wk.off <= 4100 + 10 * 512, wk.off
            pre_ar = Arena(WK[:, :], WKN)
            pre_ar.off = 1028 + 3 * 1024
            _wgu, _wguR, _wdn, _wdnR = ffn_ring(pre_ar)
            ffn_pre_R = [_wguR[0], _wguR[1], _wdnR[0], _wdnR[1]]
            for f_ in range(2):
                fw.dma(fw.pool, f_dma(_wgu[f_][:, 0, :, :].rearrange("p a b -> p (a b)"), wg_d[:, f_, :]), writes=[_wguR[f_]])
                fw.dma(fw.pool, f_dma(_wgu[f_][:, 1, :, :].rearrange("p a b -> p (a b)"), wu_d[:, f_, :]), writes=[_wguR[f_]])
                fw.dma(fw.pool, f_dma(_wdn[f_][:, :], wd_d[:, f_, :]), writes=[_wdnR[f_]])
            HT_R = [Res() for _ in range(NT)]
            for r_ in HT_R:
                r_.r = [ev_a, ev_b]
            for i in range(NT):
                fw.dma(fw.sp, f_dma(X1[:, i * D:(i + 1) * D], x_tiles[i]), writes=[X1_R[i]])
            lanes = [[], [], [], []]
            p4_tails = [[], [], [], []]
            for i in range(NT):
                xi = X1[:, i * D:(i + 1) * D]
                lane_banks[0] = [2 * (i % 4), 2 * (i % 4) + 1]
                fw.rec_begin()
                for hf in range(2):
                    pb, pR = bank()
                    mmg(pb[:, :], pR, [(MIXTb[:, c, i * 128:(i + 1) * 128], wob[:, c, hf * 512:(hf + 1) * 512]) for c in range(8)], [MX_R[i], woR])
                    fw.op(fw.dve, f_tt(xi[:, hf * 512:(hf + 1) * 512], xi[:, hf * 512:(hf + 1) * 512], pb[:, :], ALU.add), reads=[pR, X1_R[i]], writes=[X1_R[i]])
                norm_to_HT(xi, X1_R[i], i, g2, wka)
                it_ = fw.rec_end()
                body_, tail_ = it_[:-9], it_[-9:]
                lanes[i % 4] += body_[:9] + p4_tails[i % 4] + body_[9:]
                p4_tails[i % 4] = tail_
            for q_ in range(4):
                lanes[q_] += p4_tails[q_]
            lane_banks[0] = None
            fw.replay_lanes(lanes, stagger=8)
            fw.barrier()
            if stop == "P4":
                raise _Stop()
            if debug:
                for i in range(NT):
                    fw.dma(fw.sp, f_dma(dbg["x1"][i * 128:(i + 1) * 128, :], X1[:, i * D:(i + 1) * D]), reads=[X1_R[i]], writes=[])
                fw.barrier()

            wk.reset()
            ma.reset()
            actb = [ma.bf16([128, FG, T]) for _ in range(2)]
            actR = [[Res() for _ in range(FG)] for _ in range(2)]
            NGU, NWD = 3, 12
            gsb = [wk.f32([128, 514]) for _ in range(2)]
            gsbR = [Res(), Res()]
            c0 = [wk.f32([128, 512]) for _ in range(2)]
            c0R = [Res(), Res()]
            c1 = [wk.f32([128, 512]) for _ in range(2)]
            c1R = [Res(), Res()]
            sl_ = [wk.f32([128, 512]) for _ in range(2)]
            slR = [Res(), Res()]
            wgu, wguR, wdn, wdnR = ffn_ring(wk)
            wguR[0], wguR[1], wdnR[0], wdnR[1] = ffn_pre_R
            groups = [list(range(s, min(s + FG, NF))) for s in range(0, NF, FG)]
            blk = [0]

            def ffn_down(gi):
                fl = groups[gi]
                ab = actb[gi % 2]
                for i in range(NT):
                    for hf in range(2):
                        pb, pR = bank()
                        mmg(pb[:, :], pR, [(ab[:, k, i * 128:(i + 1) * 128], wdn[f % NWD][:, hf * 512:(hf + 1) * 512]) for k, f in enumerate(fl)],
                            [actR[gi % 2][k] for k in range(len(fl))] + [wdnR[f % NWD] for f in fl])
                        xi = X1[:, i * D + hf * 512:i * D + (hf + 1) * 512]
                        fw.op(fw.dve, f_tt(xi, xi, pb[:, :], ALU.add), reads=[pR, X1_R[i]], writes=[X1_R[i]])

            def ffn_load(f):
                if f >= NF:
                    return
                ws = f % NGU
                fw.dma(fw.pool, f_dma(wgu[ws][:, 0, :, :].rearrange("p a b -> p (a b)"), wg_d[:, f, :]), writes=[wguR[ws]])
                fw.dma(fw.pool, f_dma(wgu[ws][:, 1, :, :].rearrange("p a b -> p (a b)"), wu_d[:, f, :]), writes=[wguR[ws]])
                fw.dma(fw.pool, f_dma(wdn[f % NWD][:, :], wd_d[:, f, :]), writes=[wdnR[f % NWD]])

            pending = []

            def flush_tail():
                while pending:
                    s_, ab_, k_, sl_c, pu_, puR_ = pending.pop(0)
                    fw.op(fw.act, f_act(sl_[s_][:, :], c0[s_][:, :], AF.Silu), reads=[c0R[s_]], writes=[slR[s_]])
                    fw.op(fw.dve, f_tt(actb[ab_][:, k_, sl_c], sl_[s_][:, :], pu_[:, :], ALU.mult), reads=[slR[s_], puR_], writes=[actR[ab_][k_]])

            PF = 2
            for gi, fl in enumerate(groups):
                for k, f in enumerate(fl):
                    ws = f % NGU
                    ffn_load(f + PF)
                    for tb in range(4):
                        s = blk[0] % 2
                        blk[0] += 1
                        sl = slice(tb * 512, (tb + 1) * 512)
                        pg, pgR = bank()
                        mmg(pg[:, :], pgR, [(wgu[ws][:, 0, c, :], HT[:, c, sl]) for c in range(8)], HT_R[4 * tb:4 * tb + 4] + [wguR[ws]])
                        pu, puR = bank()
                        mmg(pu[:, :], puR, [(wgu[ws][:, 1, c, :], HT[:, c, sl]) for c in range(8)], HT_R[4 * tb:4 * tb + 4] + [wguR[ws]])
                        if tb == 0:
                            fw.op(fw.dve, f_memset(gsb[s][:, 0:2], 0.0), writes=[gsbR[s]])
                        else:
                            fw.op(fw.act, f_act(gsb[s][:, 0:2], gsb[1 - s][:, 512:514], AF.Copy), reads=[gsbR[1 - s]], writes=[gsbR[s]])
                        fw.op(fw.act, f_act(gsb[s][:, 2:514], pg[:, :], AF.Copy), reads=[pgR], writes=[gsbR[s]])
                        fw.op(fw.act, f_act(c0[s][:, :], pg[:, :], AF.Identity, scale=convw[:, f, 2:3], bias=convb[:, f:f + 1]), reads=[pgR, cR], writes=[c0R[s]])
                        fw.op(fw.dve, f_stt(c1[s][:, :], gsb[s][:, 1:513], convw[:, f, 1:2], c0[s][:, :], ALU.mult, ALU.add),
                              reads=[gsbR[s], c0R[s], cR], writes=[c1R[s]])
                        fw.op(fw.dve, f_stt(c0[s][:, :], gsb[s][:, 0:512], convw[:, f, 0:1], c1[s][:, :], ALU.mult, ALU.add),
                              reads=[gsbR[s], c1R[s], cR], writes=[c0R[s]])
                        flush_tail()
                        pending.append((s, gi % 2, k, sl, pu, puR))
                flush_tail()
                if gi >= 1:
                    ffn_down(gi - 1)
            ffn_down(len(groups) - 1)

            outR = Res()
            out_tiles = out_d.rearrange("(n p) d -> n p d", p=128)
            fjunk = wk.bf16([128, D]) if False else c0[0]
            lanes = [[], [], [], []]
            p6R = [Res() for _ in range(4)]
            for i in range(NT):
                xi = X1[:, i * D:(i + 1) * D]
                s = i % 2
                st = stat[:, 8 + 2 * (i % 4):10 + 2 * (i % 4)]
                stR = p6R[i % 4]
                fw.rec_begin()
                fw.op(fw.act, f_act(sl_[s][:, :].bitcast(BF16), xi, AF.Square, accum_out=st[:, 0:1]), reads=[X1_R[i]], writes=[slR[s], stR])
                rstd_from_ss(st[:, 0:1], D, st[:, 1:2], stR)
                fw.op(fw.dve, f_stt(xi, xi, st[:, 1:2], fgain[:, :], ALU.mult, ALU.mult), reads=[X1_R[i], stR, cR], writes=[X1_R[i]])
                fw.dma(fw.sp, f_dma(out_tiles[i], xi), reads=[X1_R[i]], writes=[])
                lanes[i % 4] += fw.rec_end()
            fw.replay_lanes(lanes, stagger=1)
            fw.barrier()
            if stop == "P6":
                raise _Stop()


        except _Stop:
            fw.barrier()
        run = fw.runner(sems)
        with nc.Block() as block:
            @block.tensor
            def _(e):
                run(fw.pe, e)

            @block.scalar
            def _(e):
                run(fw.act, e)

            @block.vector
            def _(e):
                run(fw.dve, e)

            @block.gpsimd
            def _(e):
                run(fw.pool, e)

            @block.sync
            def _(e):
                run(fw.sp, e)
    return nc


def _consts():
    c = {}
    c["c_ident"] = np.eye(128, dtype=np.float32)
    P = np.zeros((128, 128), np.float32)
    for po in range(128):
        j = po % 64
        if j < 8:
            P[po + 8, po] = -1.0
        elif j < 16:
            P[po - 8, po] = 1.0
    c["c_perm"] = P
    invf = np.zeros((128, 1), np.float32)
    half = 8
    inv_freq = (500000.0 ** (-np.arange(half, dtype=np.float32) * 2.0 / 16)).astype(np.float32)
    for p in range(128):
        j = p % 64
        if j < 16:
            invf[p, 0] = inv_freq[j % 8] / (2 * np.pi)
    c["c_invf"] = invf
    pk = np.arange(128)[:, None]
    pq = np.arange(128)[None, :]
    tri0 = (pq >= pk).astype(np.float32)
    c["c_tri0"] = np.tile(tri0, (1, 4))
    c["c_tri4"] = np.tile(1.0 - tri0, (1, 4))
    eb = np.zeros((128, 16, 128), np.float32)
    for j in range(16):
        for p in range(128):
            eb[2 * j + p // 64, j, p] = 1.0
    c["c_eblk"] = eb.reshape(128, 16 * 128)
    n = np.arange(128)[:, None]
    t = np.arange(T)[None, :]
    cm = ((16 * n + 31) <= t).astype(np.float32)
    cm[127, :] = 0
    c["c_cmpm"] = (cm - 1.0) * 30000.0
    keep = np.zeros((128, 8, 32), np.float32)
    add = np.zeros((128, 8, 32), np.float32)
    for i in range(8, 16):
        for p in range(128):
            cur = 2 * i + (1 if p >= 64 else 0)
            for s in range(32):
                valid = s <= cur
                forced = (s == 0) or (s == cur) or (s == cur - 1)
                if not valid:
                    add[p, i - 8, s] = -1.0
                elif forced:
                    add[p, i - 8, s] = 1e4
                else:
                    keep[p, i - 8, s] = 1.0
    c["c_keep"] = keep.reshape(128, 256)
    c["c_add"] = add.reshape(128, 256)
    ovl = np.zeros((128, 32), np.float32)
    for nn in range(127):
        for s in range(32):
            if (16 * nn <= 64 * s + 63) and (16 * nn + 31 >= 64 * s):
                ovl[nn, s] = 1.0
    c["c_ovl"] = ovl
    return c


def _prep_shared(inp):
    f = lambda a: np.ascontiguousarray(a, dtype=np.float32)
    w_in = np.asarray(inp["w_in"][0])
    w3 = w_in.reshape(8, 128, -1)

    def fm_chunk(cols):
        return w3[:, :, cols].transpose(1, 0, 2)
    chunks = []
    for h in range(4):
        chunks.append(fm_chunk(np.arange(h * 128, (h + 1) * 128)))
    for h in range(4):
        chunks.append(fm_chunk(np.arange(512 + h * 128, 512 + (h + 1) * 128)))
    for c in range(4):
        cols = np.concatenate([2048 + c * 64 + np.arange(64), 2048 + (4 + c) * 64 + np.arange(64)])
        chunks.append(fm_chunk(cols))
    for base in (2560, 2688, 2816, 3072):
        chunks.append(fm_chunk(np.arange(base, base + 128)))
    sh = {}
    sh["w_fm"] = f(np.stack(chunks, axis=1).reshape(128, 16, 1024))
    sh["w_tm_i"] = f(w3[:, :, 1024:1536].transpose(1, 0, 2).reshape(128, -1))
    sh["w_tm_g"] = f(w3[:, :, 1536:2048].transpose(1, 0, 2).reshape(128, -1))
    ncols = np.concatenate([np.arange(2944, 3072), np.arange(3200, 3328), np.arange(3328, 3352)])
    sh["w_tm_n"] = f(w3[:, :, ncols].transpose(1, 0, 2).reshape(128, -1))
    sh["w_o"] = f(np.asarray(inp["w_o"][0]).reshape(8, 128, 1024).transpose(1, 0, 2).reshape(128, -1))
    for nm, key in (("w_gate", "ffn_w_gate"), ("w_up", "ffn_w_up")):
        w = np.asarray(inp[key][0]).reshape(8, 128, NF, 128)
        sh[nm] = f(w.transpose(1, 2, 0, 3).reshape(128, NF, 1024))
    sh["w_down"] = f(np.asarray(inp["ffn_w_down"][0]).reshape(NF, 128, 1024).transpose(1, 0, 2))
    for nm, key in (("w1k", "cmp_k_w1"), ("w1v", "cmp_v_w1")):
        w = np.asarray(inp[key][0]).reshape(32, 64, 256).transpose(1, 0, 2)
        sh[nm] = f(np.concatenate([w, w], axis=0).reshape(128, -1))
    for nm, key in (("w2k", "cmp_k_w2"), ("w2v", "cmp_v_w2")):
        sh[nm] = f(np.asarray(inp[key][0]).reshape(2, 128, 64).transpose(1, 0, 2).reshape(128, -1))
    for nm, key in (("pek", "cmp_pe_k"), ("pev", "cmp_pe_v")):
        pe = np.asarray(inp[key][0]).T
        sh[nm] = f(np.concatenate([pe, pe], axis=0))
    sh["g1"] = f(np.asarray(inp["ln1_gain"][0]).reshape(8, 128).T)
    sh["g2"] = f(np.asarray(inp["ln2_gain"][0]).reshape(8, 128).T)
    lbp = np.asarray(inp["hgrn_lb_param"])
    sh["lbp"] = f(lbp.reshape(2, 4, 128).transpose(2, 0, 1).reshape(128, 8))
    sh["hgain"] = f(np.asarray(inp["hgrn_out_gain"][0]).reshape(1, 512))
    sh["ngain"] = f(np.asarray(inp["nsa_out_gain"][0]).reshape(1, 512))
    sh["fgain"] = f(np.asarray(inp["final_gain"]).reshape(1, 1024))
    cw = np.asarray(inp["ffn_conv_w"][0])
    sh["convw"] = f(cw.reshape(3, NF, 128).transpose(2, 1, 0).reshape(128, NF * 3))
    sh["convb"] = f(np.asarray(inp["ffn_conv_b"][0]).reshape(NF, 128).T)
    sh.update(_consts())
    return sh


_NC_CACHE = {}


def kernel(**inputs):
    debug = bool(inputs.pop("_debug", False))
    cores = inputs.pop("_cores", None)
    sh = _prep_shared(inputs)
    x = np.asarray(inputs["x"], dtype=np.float32)
    pos = np.asarray(inputs["positions"]).astype(np.int32)
    core_ids = list(range(NCORES)) if cores is None else list(cores)
    if debug not in _NC_CACHE:
        _NC_CACHE[debug] = build_nc(debug)
    nc = _NC_CACHE[debug]
    in_maps = []
    for b in core_ids:
        m = dict(sh)
        m["x"] = np.ascontiguousarray(x[b])
        m["pos"] = np.ascontiguousarray(pos[b:b + 1])
        in_maps.append(m)
    res = run_bass_kernel_spmd(nc, in_maps, core_ids=core_ids)
    if debug:
        return res
    out = np.stack([np.asarray(r["out"], dtype=np.float32) for r in res.results], axis=0)
    return out
```
